# Optimizing a Trainium2 kernel written in Bass

```python
import jax
import jax.numpy as jnp
from jax import lax
import numpy as np

D_MODEL = 1024
BATCH = 8
SEQ = 8192
DEPTH = 2

HEAD_DIM = 64
A_HEADS = 8
A_KV_HEADS = 2
A_HALF_WINDOW = 128
B_HEADS_PER_GROUP = 4
B_CONFIGS = ((128, 1), (512, 4), (2048, 16))
C_HEADS = 8
GRID_W = 64
NA_KH = 8
NA_KW = 16
NUM_BUCKETS = 32
REL_MAX_DIST = 2048
N_GROUPS = 4
EXPERTS_PER_GROUP = 8
N_EXPERTS = N_GROUPS * EXPERTS_PER_GROUP
TOP_K = 2
D_EXPERT = 512
MOE_BLOCK = 256
BLK = 128
LN_EPS = 1e-5
NEG_INF = -1e30
DEEPNORM_ALPHA = (2 * DEPTH) ** 0.25
DEEPNORM_BETA = (8 * DEPTH) ** -0.25

A_Q = A_HEADS * HEAD_DIM
A_KV = A_KV_HEADS * HEAD_DIM
B_W = B_HEADS_PER_GROUP * HEAD_DIM
C_W = C_HEADS * HEAD_DIM
N_BRANCH = 3
IN_SIZES = (A_Q, A_KV, A_KV) + (B_W,) * (3 * len(B_CONFIGS)) + (C_W,) * 3 + (D_MODEL,) * N_BRANCH
W_IN_COLS = sum(IN_SIZES)
N_BIAS_HEADS = A_HEADS + B_HEADS_PER_GROUP * len(B_CONFIGS)

kernel_name = 'hybrid_gated_mixers_hmoe_encoder'


def layer_norm(x, g, b):
    xf = x.astype(jnp.float32)
    mu = xf.mean(-1, keepdims=True)
    var = jnp.square(xf - mu).mean(-1, keepdims=True)
    return ((xf - mu) * lax.rsqrt(var + LN_EPS) * g + b).astype(x.dtype)


def t5_bucket(rel):
    half = NUM_BUCKETS // 2
    max_exact = half // 2
    ret = np.where(rel > 0, half, 0)
    n = np.abs(rel)
    large = max_exact + (np.log(np.maximum(n, max_exact) / max_exact)
                         / np.log(REL_MAX_DIST / max_exact) * (half - max_exact)).astype(np.int32)
    large = np.minimum(large, half - 1)
    return (ret + np.where(n < max_exact, n, large)).astype(np.int32)


def banded_attention(q, k, v, half, dist_scale, bias_tab, sink, with_lse):
    N, L, Hk, G, Dh = q.shape
    nb = -(-L // BLK)
    Lp = nb * BLK
    KL = BLK + 2 * half
    qp = jnp.pad(q, ((0, 0), (0, Lp - L), (0, 0), (0, 0), (0, 0)))
    kv_pad = ((0, 0), (half, Lp - L + half), (0, 0), (0, 0))
    kp = jnp.pad(k, kv_pad)
    vp = jnp.pad(v, kv_pad)
    off = np.arange(KL)[None, :] - half - np.arange(BLK)[:, None]
    band = jnp.asarray(np.abs(off) <= half)
    bucket = jnp.asarray(t5_bucket(off * dist_scale))
    bias = bias_tab[bucket].astype(jnp.float32).reshape(BLK, KL, Hk, G).transpose(2, 3, 0, 1)
    scale = Dh ** -0.5

    def block(b):
        start = b * BLK
        qb = lax.dynamic_slice_in_dim(qp, start, BLK, axis=1)
        kb = lax.dynamic_slice_in_dim(kp, start, KL, axis=1)
        vb = lax.dynamic_slice_in_dim(vp, start, KL, axis=1)
        kpos = start - half + jnp.arange(KL)
        valid = band & ((kpos >= 0) & (kpos < L))[None, :]
        s = jnp.einsum('nqhgd,nkhd->nhgqk', qb, kb, preferred_element_type=jnp.float32) * scale + bias
        s = jnp.where(valid, s, NEG_INF)
        m = s.max(-1)
        if sink is not None:
            sk = sink.astype(jnp.float32)[None, :, :, None]
            m = jnp.maximum(m, sk)
        p = jnp.exp(s - m[..., None])
        denom = p.sum(-1)
        if sink is not None:
            denom = denom + jnp.exp(sk - m)
        o = jnp.einsum('nhgqk,nkhd->nqhgd', p, vb.astype(jnp.float32)) / denom.transpose(0, 3, 1, 2)[..., None]
        if with_lse:
            return o.astype(q.dtype), m + jnp.log(denom)
        return o.astype(q.dtype)

    res = lax.map(block, jnp.arange(nb))
    if with_lse:
        o, lse = res
        lse = lse.transpose(1, 0, 4, 2, 3).reshape(N, Lp, Hk, G)[:, :L]
    else:
        o = res
    o = jnp.moveaxis(o, 0, 1).reshape(N, Lp, Hk, G, Dh)[:, :L]
    if with_lse:
        return o, lse
    return o


def dilated_attention(q, k, v, window, dilation, bias_tab):
    Bn, L, H, Dh = q.shape
    Ls = L // dilation

    def to_sub(t):
        return t.reshape(Bn, Ls, dilation, H, Dh).transpose(0, 2, 1, 3, 4).reshape(Bn * dilation, Ls, H, Dh)

    o, lse = banded_attention(to_sub(q)[:, :, :, None], to_sub(k), to_sub(v),
                              window // (2 * dilation), dilation, bias_tab, None, True)
    o = o[:, :, :, 0].reshape(Bn, dilation, Ls, H, Dh).transpose(0, 2, 1, 3, 4).reshape(Bn, L, H, Dh)
    lse = lse[..., 0].reshape(Bn, dilation, Ls, H).transpose(0, 2, 1, 3).reshape(Bn, L, H)
    return o, lse


def neighborhood_attention(q, k, v, rpb):
    Bn, L, H, Dh = q.shape
    rows = L // GRID_W
    kh = min(NA_KH, rows)
    n_cb = GRID_W // NA_KW
    kcw = 2 * NA_KW
    qcol = np.arange(GRID_W).reshape(n_cb, NA_KW)
    cb_start = np.clip(np.arange(n_cb) * NA_KW - NA_KW // 2, 0, GRID_W - kcw)
    kcol = cb_start[:, None] + np.arange(kcw)
    qstart = np.clip(qcol - NA_KW // 2, 0, GRID_W - NA_KW)
    col_valid = (kcol[:, None, :] >= qstart[:, :, None]) & (kcol[:, None, :] < qstart[:, :, None] + NA_KW)
    cidx = np.clip(kcol[:, None, :] - qcol[:, :, None] + NA_KW - 1, 0, 2 * NA_KW - 2)
    mask = jnp.asarray(np.broadcast_to(col_valid[:, :, None, :], (n_cb, NA_KW, kh, kcw)).reshape(n_cb, NA_KW, kh * kcw))
    qg = q.reshape(Bn, rows, GRID_W, H, Dh)
    kg = k.reshape(Bn, rows, GRID_W, H, Dh)
    vg = v.reshape(Bn, rows, GRID_W, H, Dh)
    rpb32 = rpb.astype(jnp.float32)
    scale = Dh ** -0.5

    def row(i):
        rstart = jnp.clip(i - kh // 2, 0, rows - kh)
        qr = lax.dynamic_index_in_dim(qg, i, axis=1, keepdims=False).reshape(Bn, n_cb, NA_KW, H, Dh)
        kr = lax.dynamic_slice_in_dim(kg, rstart, kh, axis=1)[:, :, kcol]
        vr = lax.dynamic_slice_in_dim(vg, rstart, kh, axis=1)[:, :, kcol]
        kr = kr.transpose(0, 2, 1, 3, 4, 5).reshape(Bn, n_cb, kh * kcw, H, Dh)
        vr = vr.transpose(0, 2, 1, 3, 4, 5).reshape(Bn, n_cb, kh * kcw, H, Dh)
        ridx = rstart + jnp.arange(kh) - i + NA_KH - 1
        bias = rpb32[:, ridx[None, None, :, None], cidx[:, :, None, :]]
        bias = bias.reshape(H, n_cb, NA_KW, kh * kcw).transpose(1, 0, 2, 3)
        s = jnp.einsum('ncqhd,nckhd->nchqk', qr, kr, preferred_element_type=jnp.float32) * scale + bias
        s = jnp.where(mask[:, None], s, NEG_INF)
        p = jax.nn.softmax(s, axis=-1)
        o = jnp.einsum('nchqk,nckhd->ncqhd', p, vr.astype(jnp.float32))
        return o.reshape(Bn, GRID_W, H, Dh).astype(q.dtype)

    out = lax.map(row, jnp.arange(rows))
    return jnp.moveaxis(out, 0, 1).reshape(Bn, L, H, Dh)


def gated_mixer(h, rel_bias, w_in, b_gate, sink_a, rpb_c, w_br_a, w_br_b, w_br_c, w_out):
    Bn, L, _ = h.shape
    n_b = len(B_CONFIGS)
    proj = h @ w_in
    parts = jnp.split(proj, [int(c) for c in np.cumsum(IN_SIZES)[:-1]], axis=-1)
    qa = parts[0].reshape(Bn, L, A_KV_HEADS, A_HEADS // A_KV_HEADS, HEAD_DIM)
    ka = parts[1].reshape(Bn, L, A_KV_HEADS, HEAD_DIM)
    va = parts[2].reshape(Bn, L, A_KV_HEADS, HEAD_DIM)
    o_a = banded_attention(qa, ka, va, A_HALF_WINDOW, 1, rel_bias[:, :A_HEADS],
                           sink_a.reshape(A_KV_HEADS, A_HEADS // A_KV_HEADS), False).reshape(Bn, L, A_Q)
    outs, lses = [], []
    for g, (window, dilation) in enumerate(B_CONFIGS):
        qb, kb, vb = [t.reshape(Bn, L, B_HEADS_PER_GROUP, HEAD_DIM) for t in parts[3 + 3 * g: 6 + 3 * g]]
        c0 = A_HEADS + g * B_HEADS_PER_GROUP
        o, lse = dilated_attention(qb, kb, vb, window, dilation, rel_bias[:, c0:c0 + B_HEADS_PER_GROUP])
        outs.append(o)
        lses.append(lse)
    wts = jax.nn.softmax(jnp.stack(lses), axis=0)
    o_b = jnp.einsum('gblh,gblhd->blhd', wts, jnp.stack(outs).astype(jnp.float32)).astype(h.dtype).reshape(Bn, L, B_W)
    qc, kc, vc = [t.reshape(Bn, L, C_HEADS, HEAD_DIM) for t in parts[3 + 3 * n_b: 6 + 3 * n_b]]
    o_c = neighborhood_attention(qc, kc, vc, rpb_c).reshape(Bn, L, C_W)
    g_a, g_b, g_c = [jax.nn.sigmoid(parts[6 + 3 * n_b + i] + b_gate[i]) for i in range(N_BRANCH)]
    merged = g_a * (o_a @ w_br_a) + g_b * (o_b @ w_br_b) + g_c * (o_c @ w_br_c)
    return merged @ w_out


def hier_moe(h, w_rg, b_rg, w_re, b_re, w_eg, w_eu, w_ed):
    Bn, L, D = h.shape
    T = Bn * L
    xf = h.reshape(T, D)
    f32 = jnp.float32
    g_logits = jnp.dot(xf, w_rg, preferred_element_type=f32) + b_rg.astype(f32)
    g_prob = jax.nn.softmax(g_logits, axis=-1)
    g_idx = lax.top_k(g_logits, 1)[1][:, 0]
    g_w = jnp.take_along_axis(g_prob, g_idx[:, None], axis=-1)[:, 0]
    e_logits_all = jnp.einsum('td,gde->tge', xf, w_re, preferred_element_type=f32) + b_re.astype(f32)
    e_logits = jnp.take_along_axis(e_logits_all, g_idx[:, None, None], axis=1)[:, 0]
    e_top, e_idx = lax.top_k(e_logits, TOP_K)
    gate = g_w[:, None] * jax.nn.softmax(e_top, axis=-1)
    eid = g_idx[:, None] * EXPERTS_PER_GROUP + e_idx
    n_assign = T * TOP_K
    e_flat = eid.reshape(n_assign)
    w_flat = gate.reshape(n_assign)
    tok_flat = jnp.arange(n_assign) // TOP_K
    order = jnp.argsort(e_flat)
    e_sorted = e_flat[order]
    counts = jnp.bincount(e_flat, length=N_EXPERTS)
    offsets = jnp.cumsum(counts) - counts
    padded = (counts + MOE_BLOCK - 1) // MOE_BLOCK * MOE_BLOCK
    pad_end = jnp.cumsum(padded)
    pad_off = pad_end - padded
    dest = pad_off[e_sorted] + (jnp.arange(n_assign) - offsets[e_sorted])
    nblk = -(-n_assign // MOE_BLOCK) + N_EXPERTS
    P = nblk * MOE_BLOCK
    buf_tok = jnp.full((P,), T, jnp.int32).at[dest].set(tok_flat[order].astype(jnp.int32))
    buf_w = jnp.zeros((P,), f32).at[dest].set(w_flat[order])
    blk_e = jnp.minimum(jnp.searchsorted(pad_end, jnp.arange(nblk) * MOE_BLOCK, side='right'), N_EXPERTS - 1)
    x_pad = jnp.concatenate([xf, jnp.zeros((1, D), xf.dtype)], axis=0)

    def run(b):
        tok = lax.dynamic_slice_in_dim(buf_tok, b * MOE_BLOCK, MOE_BLOCK)
        wb = lax.dynamic_slice_in_dim(buf_w, b * MOE_BLOCK, MOE_BLOCK)
        e = blk_e[b]
        xb = x_pad[tok]
        hid = jax.nn.silu(xb @ w_eg[e]) * (xb @ w_eu[e])
        return ((hid @ w_ed[e]) * wb[:, None]).astype(h.dtype)

    yb = lax.map(run, jnp.arange(nblk))
    out = jnp.zeros((T + 1, D), h.dtype).at[buf_tok].add(yb.reshape(P, D))[:T]
    return out.reshape(Bn, L, D)


def setup_inputs(seed: int = 0) -> dict:
    key = jax.random.key(seed)
    ks = jax.random.split(key, 26)
    f32 = jnp.float32

    def nrm(k, shape, scale):
        return jax.random.normal(k, shape, f32) * scale

    return {
        'x': nrm(ks[0], (BATCH, SEQ, D_MODEL), 1.0),
        'ln0_g': 1.0 + nrm(ks[1], (D_MODEL,), 0.05),
        'ln0_b': nrm(ks[2], (D_MODEL,), 0.02),
        'rel_bias': nrm(ks[3], (NUM_BUCKETS, N_BIAS_HEADS), 0.1),
        'w_in': nrm(ks[4], (DEPTH, D_MODEL, W_IN_COLS), D_MODEL ** -0.5),
        'b_gate': nrm(ks[5], (DEPTH, N_BRANCH, D_MODEL), 0.02),
        'sink_a': nrm(ks[6], (DEPTH, A_HEADS), 0.5),
        'rpb_c': nrm(ks[7], (DEPTH, C_HEADS, 2 * NA_KH - 1, 2 * NA_KW - 1), 0.1),
        'w_br_a': nrm(ks[8], (DEPTH, A_Q, D_MODEL), A_Q ** -0.5),
        'w_br_b': nrm(ks[9], (DEPTH, B_W, D_MODEL), B_W ** -0.5),
        'w_br_c': nrm(ks[10], (DEPTH, C_W, D_MODEL), C_W ** -0.5),
        'w_out': nrm(ks[11], (DEPTH, D_MODEL, D_MODEL), D_MODEL ** -0.5 * DEEPNORM_BETA),
        'ln1_g': 1.0 + nrm(ks[12], (DEPTH, D_MODEL), 0.05),
        'ln1_b': nrm(ks[13], (DEPTH, D_MODEL), 0.02),
        'w_rg': nrm(ks[14], (DEPTH, D_MODEL, N_GROUPS), D_MODEL ** -0.5),
        'b_rg': nrm(ks[15], (DEPTH, N_GROUPS), 0.01),
        'w_re': nrm(ks[16], (DEPTH, N_GROUPS, D_MODEL, EXPERTS_PER_GROUP), D_MODEL ** -0.5),
        'b_re': nrm(ks[17], (DEPTH, N_GROUPS, EXPERTS_PER_GROUP), 0.01),
        'w_eg': nrm(ks[18], (DEPTH, N_EXPERTS, D_MODEL, D_EXPERT), D_MODEL ** -0.5),
        'w_eu': nrm(ks[19], (DEPTH, N_EXPERTS, D_MODEL, D_EXPERT), D_MODEL ** -0.5),
        'w_ed': nrm(ks[20], (DEPTH, N_EXPERTS, D_EXPERT, D_MODEL), D_EXPERT ** -0.5 * DEEPNORM_BETA),
        'ln2_g': 1.0 + nrm(ks[21], (DEPTH, D_MODEL), 0.05),
        'ln2_b': nrm(ks[22], (DEPTH, D_MODEL), 0.02),
    }


def reference(x, ln0_g, ln0_b, rel_bias, w_in, b_gate, sink_a, rpb_c, w_br_a, w_br_b, w_br_c, w_out,
              ln1_g, ln1_b, w_rg, b_rg, w_re, b_re, w_eg, w_eu, w_ed, ln2_g, ln2_b):
    h = layer_norm(x, ln0_g, ln0_b)
    for l in range(DEPTH):
        y = gated_mixer(h, rel_bias, w_in[l], b_gate[l], sink_a[l], rpb_c[l],
                        w_br_a[l], w_br_b[l], w_br_c[l], w_out[l])
        h = layer_norm(DEEPNORM_ALPHA * h + y, ln1_g[l], ln1_b[l])
        y = hier_moe(h, w_rg[l], b_rg[l], w_re[l], b_re[l], w_eg[l], w_eu[l], w_ed[l])
        h = layer_norm(DEEPNORM_ALPHA * h + y, ln2_g[l], ln2_b[l])
    return h
```

```python
import numpy as np
import concourse.bass as bass
import concourse.mybir as mybir
from concourse.bass_utils import run_bass_kernel_spmd

F32 = mybir.dt.float32
BF16 = mybir.dt.bfloat16
I32 = mybir.dt.int32
U32 = mybir.dt.uint32
ALU = mybir.AluOpType
AF = mybir.ActivationFunctionType
AX = mybir.AxisListType

ENGS = ("pe", "act", "dve", "pool", "sp")


class Buf:
    __slots__ = ("name", "last_w", "readers")

    def __init__(self, name=""):
        self.name = name
        self.last_w = None
        self.readers = []


class Op:
    __slots__ = ("eng", "fn", "deps", "is_dma", "sem", "count", "has_dep", "prev_dma", "idx")

    def __init__(self, eng, fn, is_dma):
        self.eng = eng
        self.fn = fn
        self.deps = []
        self.is_dma = is_dma
        self.sem = None
        self.count = 0
        self.has_dep = False
        self.prev_dma = None


class Sched:
    NS = 4
    ND = 8

    def __init__(self, nc, stack):
        self.nc = nc
        self.esem = {e: [stack.enter_context(nc.semaphore(f"s_{e}{i}")) for i in range(self.NS)] for e in ENGS}
        self.ecnt = {e: [0] * self.NS for e in ENGS}
        self.ek = {e: 0 for e in ENGS}
        self.dsem = {e: [stack.enter_context(nc.semaphore(f"d_{e}{i}")) for i in range(self.ND)] for e in ("sp", "pool", "act")}
        self.dcnt = {e: [0] * self.ND for e in self.dsem}
        self.dk = {e: 0 for e in self.dsem}
        self.dlast = {e: [None] * self.ND for e in self.dsem}
        self.waited = {e: {} for e in ENGS}


class Phase:
    def __init__(self, sched, name):
        self.s = sched
        self.nc = sched.nc
        self.name = name
        self.ops = []
        self.bufs = []

    def buf(self, name=""):
        b = Buf(name)
        self.bufs.append(b)
        return b

    def _add(self, op, reads, writes):
        deps = []
        for b in reads:
            if b.last_w is not None:
                deps.append(b.last_w)
        for b in writes:
            if b.last_w is not None:
                deps.append(b.last_w)
            deps.extend(b.readers)
        for b in reads:
            b.readers.append(op)
        for b in writes:
            b.last_w = op
            b.readers = []
        seen = set()
        for d in deps:
            if d is op or id(d) in seen:
                continue
            seen.add(id(d))
            if d.eng == "pe" and op.eng == "pe" and not d.is_dma and not op.is_dma:
                continue
            op.deps.append(d)
            d.has_dep = True
        self.ops.append(op)
        return op

    def op(self, eng, fn, reads=(), writes=()):
        return self._add(Op(eng, fn, False), reads, writes)

    def dma(self, q, out, in_, reads=(), writes=(), **kw):
        def fn(e):
            return e.dma_start(out=out, in_=in_, **kw)
        return self._add(Op(q, fn, True), reads, writes)

    def dma_fn(self, q, fn, reads=(), writes=()):
        return self._add(Op(q, fn, True), reads, writes)

    def emit(self):
        s = self.s
        nc = self.nc
        for op in self.ops:
            e = op.eng
            if op.is_dma:
                k = s.dk[e] % s.ND
                s.dk[e] += 1
                op.prev_dma = s.dlast[e][k]
                s.dcnt[e][k] += 16
                op.sem = s.dsem[e][k]
                op.count = s.dcnt[e][k]
                s.dlast[e][k] = (op.sem, op.count)
            elif op.has_dep:
                k = s.ek[e] % s.NS
                s.ek[e] += 1
                s.ecnt[e][k] += 1
                op.sem = s.esem[e][k]
                op.count = s.ecnt[e][k]
        per = {e: [o for o in self.ops if o.eng == e] for e in ENGS}

        def run(e, eng):
            waited = s.waited[e]
            for op in per[e]:
                need = {}
                for d in op.deps:
                    key = id(d.sem)
                    if need.get(key, (None, 0))[1] < d.count:
                        need[key] = (d.sem, d.count)
                if op.prev_dma is not None:
                    sem, cnt = op.prev_dma
                    key = id(sem)
                    if need.get(key, (None, 0))[1] < cnt:
                        need[key] = (sem, cnt)
                for key, (sem, cnt) in need.items():
                    if waited.get(key, 0) < cnt:
                        eng.wait_ge(sem, cnt)
                        waited[key] = cnt
                ins = op.fn(eng)
                if op.sem is not None:
                    ins.then_inc(op.sem, 16 if op.is_dma else 1)
            if e in s.dsem:
                for k in range(s.ND):
                    if s.dlast[e][k] is not None:
                        sem, cnt = s.dlast[e][k]
                        if waited.get(id(sem), 0) < cnt:
                            eng.wait_ge(sem, cnt)
                            waited[id(sem)] = cnt

        with nc.Block() as block:
            @block.sync
            def _(eng):
                run("sp", eng)

            @block.tensor
            def _(eng):
                run("pe", eng)

            @block.scalar
            def _(eng):
                run("act", eng)

            @block.vector
            def _(eng):
                run("dve", eng)

            @block.gpsimd
            def _(eng):
                run("pool", eng)
        self.ops = []


T = 8192
D = 1024
NT = T // 128
PADR = 1024
ROWS = PADR + T + PADR
DEPTH = 2
ALPHA = (2 * DEPTH) ** 0.25
LN_EPS = 1e-5
CAP = 768
NSLOT = 32 * CAP
NEGB = -30000.0
B_CFG = ((128, 1), (512, 4), (2048, 16))
QK_AQ, QK_AK = 0, 512
QK_BQ = (640, 1152, 1664)
QK_BK = (896, 1408, 1920)
QK_CQ, QK_CK = 2176, 2688
QK_W = 3200
SEGS = [
    (0, 512, "qk", QK_AQ), (512, 128, "qk", QK_AK), (640, 128, "va", 0),
    (768, 512, "qk", QK_BQ[0]), (1280, 256, "vb", 0),
    (1536, 512, "qk", QK_BQ[1]), (2048, 256, "vb", 1),
    (2304, 512, "qk", QK_BQ[2]), (2816, 256, "vb", 2),
    (3072, 512, "qk", QK_CQ), (3584, 512, "qk", QK_CK), (4096, 512, "vc", 0),
]
GATE0 = 4608


def _t5_bucket(rel):
    half, max_exact = 16, 8
    ret = np.where(rel > 0, half, 0)
    n = np.abs(rel)
    large = max_exact + (np.log(np.maximum(n, max_exact) / max_exact) / np.log(2048 / max_exact) * (half - max_exact)).astype(np.int32)
    large = np.minimum(large, half - 1)
    return (ret + np.where(n < max_exact, n, large)).astype(np.int32)


def host_bias_tables(rel_bias, rpb_c):
    kk = np.arange(128)[:, None]
    qq = np.arange(128)[None, :]
    ba = np.full((128, 2, 3, 4, 128), NEGB, np.float32)
    for c in range(3):
        off = (c - 1) * 128 + kk - qq
        band = np.abs(off) <= 128
        bk = _t5_bucket(off)
        for h in range(8):
            ba[:, h // 4, c, h % 4, :] = np.where(band, rel_bias[bk, h], NEGB)
    bb = np.full((3, 128, 2, 4, 128), NEGB, np.float32)
    for g, (_, dil) in enumerate(B_CFG):
        for jp in range(2):
            off = jp * 128 + kk - 64 - qq
            band = np.abs(off) <= 64
            bk = _t5_bucket(off * dil)
            for h in range(4):
                bb[g, :, jp, h, :] = np.where(band, rel_bias[bk, 8 + 4 * g + h], NEGB)
    bc = np.full((rpb_c.shape[0], 5, 128, 5, 8, 128), NEGB, np.float32)
    for pi, j in enumerate((0, 1, 2, 62, 63)):
        cb0 = min(max(j - 2, 0), 59)
        qtok = j * 128 + np.arange(128)
        qi, qc = qtok // 64, qtok % 64
        rstart = np.clip(qi - 4, 0, 120)
        qstart = np.clip(qc - 8, 0, 48)
        for c in range(5):
            ktok = (cb0 + c) * 128 + np.arange(128)
            kr, kc = ktok // 64, ktok % 64
            valid = ((kr[:, None] >= rstart[None, :]) & (kr[:, None] < rstart[None, :] + 8)
                     & (kc[:, None] >= qstart[None, :]) & (kc[:, None] < qstart[None, :] + 16))
            ridx = np.clip(kr[:, None] - qi[None, :] + 7, 0, 14)
            cidx = np.clip(kc[:, None] - qc[None, :] + 15, 0, 30)
            for l in range(rpb_c.shape[0]):
                for h in range(8):
                    bc[l, pi, :, c, h, :] = np.where(valid, rpb_c[l, h][ridx, cidx], NEGB)
    return ba, bb, bc


def c_pattern(j):
    return {0: 0, 1: 1, 62: 3, 63: 4}.get(j, 2)


def o_mm(ph, out, lhsT, rhs, start, stop, reads, writes):
    return ph.op("pe", lambda e: e.matmul(out, lhsT=lhsT, rhs=rhs, start=start, stop=stop), reads, writes)


def o_tp(ph, out, in_, ident, reads, writes):
    return ph.op("pe", lambda e: e.transpose(out=out, in_=in_, identity=ident), reads, writes)


def o_act(ph, out, in_, func, reads, writes, scale=None, bias=None):
    kw = {}
    if scale is not None:
        kw["scale"] = scale
    if bias is not None:
        kw["bias"] = bias
    return ph.op("act", lambda e: e.activation(out=out, in_=in_, func=func, **kw), reads, writes)


def o_cp(ph, eng, out, in_, reads, writes):
    if eng == "act":
        return ph.op("act", lambda e: e.copy(out=out, in_=in_), reads, writes)
    return ph.op(eng, lambda e: e.tensor_copy(out=out, in_=in_), reads, writes)


def o_tt(ph, eng, out, in0, in1, op, reads, writes):
    return ph.op(eng, lambda e: e.tensor_tensor(out=out, in0=in0, in1=in1, op=op), reads, writes)


def o_ts(ph, eng, out, in0, s1, s2, op0, op1, reads, writes):
    if s2 is None:
        return ph.op(eng, lambda e: e.tensor_scalar(out=out, in0=in0, scalar1=s1, scalar2=None, op0=op0), reads, writes)
    return ph.op(eng, lambda e: e.tensor_scalar(out=out, in0=in0, scalar1=s1, scalar2=s2, op0=op0, op1=op1), reads, writes)


def o_stt(ph, out, in0, scalar, in1, op0, op1, reads, writes):
    return ph.op("dve", lambda e: e.scalar_tensor_tensor(out=out, in0=in0, scalar=scalar, in1=in1, op0=op0, op1=op1), reads, writes)


def o_red(ph, out, in_, op, reads, writes):
    return ph.op("dve", lambda e: e.tensor_reduce(out=out, in_=in_, axis=AX.X, op=op), reads, writes)


def o_rcp(ph, out, in_, reads, writes):
    return ph.op("dve", lambda e: e.reciprocal(out=out, in_=in_), reads, writes)


def o_memset(ph, eng, ap, val, writes):
    return ph.op(eng, lambda e: e.memset(ap, val), (), writes)


class Ring:
    def __init__(self, ph, tiles, name):
        self.tiles = tiles
        self.bufs = [ph.buf(f"{name}{i}") for i in range(len(tiles))]
        self.k = 0

    def next(self):
        i = self.k % len(self.tiles)
        self.k += 1
        return self.tiles[i], self.bufs[i]


class Ctx:
    pass


def ln_s1(ph, C, z, zb):
    st, stb = C.ln_st.next()
    mv, mvb = C.ln_mv.next()
    ph.op("dve", lambda e: e.bn_stats(out=st[:, 0, :], in_=z[:, 0:512]), [zb], [stb])
    ph.op("dve", lambda e: e.bn_stats(out=st[:, 1, :], in_=z[:, 512:1024]), [zb, stb], [stb])
    ph.op("dve", lambda e: e.bn_aggr(out=mv[:, 0:2], in_=st[:].rearrange("p a s -> p (a s)")), [stb], [mvb])
    return mv, mvb


def ln_s2(ph, C, mv, mvb):
    o_act(ph, mv[:, 2:3], mv[:, 1:2], AF.Sqrt, [mvb], [mvb], bias=C.eps[:, 0:1], scale=1.0)
    o_rcp(ph, mv[:, 3:4], mv[:, 2:3], [mvb], [mvb])
    o_ts(ph, "dve", mv[:, 4:5], mv[:, 0:1], mv[:, 3:4], -1.0, ALU.mult, ALU.mult, [mvb], [mvb])


def ln_s3(ph, C, z, zb, mv, mvb, out, outb, g, b, cb, geng="pool", beng="pool"):
    o_act(ph, out, z, AF.Identity, [zb, mvb], [outb], scale=mv[:, 3:4], bias=mv[:, 4:5])
    o_tt(ph, geng, out, out, g, ALU.mult, [outb, cb], [outb])
    o_tt(ph, beng, out, out, b, ALU.add, [outb, cb], [outb])


def ln_tile(ph, C, z, zb, out, outb, g, b, cb, geng="pool", beng="pool"):
    mv, mvb = ln_s1(ph, C, z, zb)
    ln_s2(ph, C, mv, mvb)
    ln_s3(ph, C, z, zb, mv, mvb, out, outb, g, b, cb, geng, beng)


def ln_setup(ph, C, A):
    C.ln_st = Ring(ph, [A(f"lnst{i}", [128, 2, 6], F32) for i in range(6)], "lnst")
    C.ln_mv = Ring(ph, [A(f"lnmv{i}", [128, 8], F32) for i in range(6)], "lnmv")


def load_gb(ph, A, gsrc, bsrc, name):
    g = A(name + "g", [128, D], F32)
    b = A(name + "b", [128, D], F32)
    cb = ph.buf(name)
    ph.dma("sp", g[:], gsrc.to_broadcast([128, D]), writes=[cb])
    ph.dma("sp", b[:], bsrc.to_broadcast([128, D]), writes=[cb])
    return g, b, cb


def mk_alloc(nc, st, prefix):
    def A(name, shape, dt):
        return st.enter_context(nc.sbuf_tensor(prefix + name, list(shape), dt))

    def P(name, shape, dt):
        return st.enter_context(nc.psum_tensor(prefix + name, list(shape), dt))
    return A, P


def phase_ln0(C):
    from contextlib import ExitStack
    nc = C.nc
    with ExitStack() as st:
        A, P = mk_alloc(nc, st, "p0_")
        ph = Phase(C.S, "ln0")
        ln_setup(ph, C, A)
        g, b, cb = load_gb(ph, A, C.ln0g, C.ln0b, "ln0")
        zr = Ring(ph, [A(f"z{i}", [128, D], F32) for i in range(3)], "z")
        orr = Ring(ph, [A(f"o{i}", [128, D], F32) for i in range(3)], "o")
        nxt = zr.next()
        ph.dma("sp", nxt[0][:], C.x[0:128, :], writes=[nxt[1]])
        for i in range(NT):
            z, zb = nxt
            if i + 1 < NT:
                nxt = zr.next()
                ph.dma("sp", nxt[0][:], C.x[(i + 1) * 128:(i + 2) * 128, :], writes=[nxt[1]])
            o, ob = orr.next()
            ln_tile(ph, C, z[:], zb, o[:], ob, g[:], b[:], cb, geng="dve", beng="pool")
            ph.dma("sp", C.H[i * 128:(i + 1) * 128, :], o[:], reads=[ob])
        ph.emit()


def phase_proj(C, l):
    from contextlib import ExitStack
    nc = C.nc
    with ExitStack() as st:
        A, P = mk_alloc(nc, st, f"p1_{l}_")
        ph = Phase(C.S, f"proj{l}")
        w = A("w", [128, 8, 7680], BF16)
        wb = ph.buf("w")
        wstr = Ring(ph, [A(f"wst{i}", [128, 1536], F32) for i in range(2)], "wst")
        wbs = []
        for k in range(8):
            for cbk in range(5):
                src = C.w_in[l, k * 128:(k + 1) * 128, cbk * 1536:(cbk + 1) * 1536]
                wbc = ph.buf(f"w{k}_{cbk}")
                wbs.append(wbc)
                if (k * 5 + cbk) % 3 == 2:
                    wst, wstb = wstr.next()
                    ph.dma("sp", wst[:], src, writes=[wstb])
                    o_cp(ph, "pool", w[:, k, cbk * 1536:(cbk + 1) * 1536], wst[:], [wstb], [wbc])
                else:
                    ph.dma("pool", w[:, k, cbk * 1536:(cbk + 1) * 1536], src, writes=[wbc])
        bg = A("bg", [128, 24], F32)
        ph.dma("sp", bg[:], C.bgT[l], writes=[wb])
        hin = Ring(ph, [A(f"hin{i}", [128, D], F32) for i in range(2)], "hin")
        hT_t = [A(f"hT{i}", [128, 8, 512], BF16) for i in range(2)]
        hT_b = [[ph.buf(f"hT{i}_{s_}") for s_ in range(4)] for i in range(2)]
        qks = Ring(ph, [A(f"qks{i}", [128, QK_W], BF16) for i in range(2)], "qks")
        vas_t = [A(f"vas{i}", [128, 2, 65], BF16) for i in range(2)]
        vbs_t = [A(f"vbs{i}", [128, 3, 4, 65], BF16) for i in range(2)]
        vcs_t = [A(f"vcs{i}", [128, 8, 65], BF16) for i in range(2)]
        vas = Ring(ph, vas_t, "vas")
        vbs = Ring(ph, vbs_t, "vbs")
        vcs = Ring(ph, vcs_t, "vcs")
        for i in range(2):
            o_memset(ph, "pool", vas_t[i][:], 1.0, [vas.bufs[i]])
            o_memset(ph, "pool", vbs_t[i][:], 1.0, [vbs.bufs[i]])
            o_memset(ph, "pool", vcs_t[i][:], 1.0, [vcs.bufs[i]])
        gst = Ring(ph, [A(f"gst{i}", [128, 6, 512], BF16) for i in range(2)], "gst")
        tpr = Ring(ph, [P(f"tp{i}", [128, 8, 128], F32) for i in range(2)], "tp")
        mmr = Ring(ph, [P(f"mm{i}", [128, 512], F32) for i in range(4)], "mm")
        GTv = C.GT
        ev = 0
        nxt = hin.next()
        ph.dma("sp", nxt[0][:], C.H[0:128, :], writes=[nxt[1]])
        for n in range(T // 512):
            ht, htbs = hT_t[n % 2], hT_b[n % 2]
            for sub in range(4):
                htb = htbs[sub]
                i = 4 * n + sub
                h_in, hib = nxt
                if i + 1 < NT:
                    nxt = hin.next()
                    ph.dma("sp", nxt[0][:], C.H[(i + 1) * 128:(i + 2) * 128, :], writes=[nxt[1]])
                tp, tpb = tpr.next()
                for k in range(8):
                    o_tp(ph, tp[:, k, :], h_in[:, k * 128:(k + 1) * 128], C.identf[:], [hib], [tpb])
                o_cp(ph, "dve", ht[:, :, sub * 128:(sub + 1) * 128], tp[:], [tpb], [htb])
                qk, qkb = qks.next()
                va, vab = vas.next()
                vb, vbb = vbs.next()
                vc, vcb = vcs.next()
                for (c0, wd, kind, dst) in SEGS:
                    mm, mmb = mmr.next()
                    for k in range(8):
                        o_mm(ph, mm[:, 0:wd], ht[:, k, sub * 128:(sub + 1) * 128], w[:, k, c0:c0 + wd],
                             k == 0, k == 7, [htb, wb] + wbs, [mmb])
                    eng = "dve" if ev % 2 == 0 else "act"
                    ev += 1
                    if kind == "qk":
                        o_cp(ph, eng, qk[:, dst:dst + wd], mm[:, 0:wd], [mmb], [qkb])
                    elif kind == "va":
                        o_cp(ph, eng, va[:, :, 0:64], mm[:, 0:128].rearrange("p (h d) -> p h d", h=2), [mmb], [vab])
                    elif kind == "vb":
                        o_cp(ph, eng, vb[:, dst, :, 0:64], mm[:, 0:256].rearrange("p (h d) -> p h d", h=4), [mmb], [vbb])
                    else:
                        o_cp(ph, eng, vc[:, :, 0:64], mm[:, 0:512].rearrange("p (h d) -> p h d", h=8), [mmb], [vcb])
                r0 = PADR + i * 128
                ph.dma("sp", C.QK[r0:r0 + 128, :], qk[:], reads=[qkb])
                ph.dma("sp", C.VA[r0:r0 + 128, :], va[:].rearrange("p h d -> p (h d)"), reads=[vab])
                for gi in range(3):
                    ph.dma("sp", C.VB[gi][r0:r0 + 128, :], vb[:, gi, :, :].rearrange("p h d -> p (h d)"), reads=[vbb])
                ph.dma("sp", C.VC[r0:r0 + 128, :], vc[:].rearrange("p h d -> p (h d)"), reads=[vcb])
            for cg in range(4):
                gs, gsb = gst.next()
                for a in range(6):
                    c = cg * 6 + a
                    mm, mmb = mmr.next()
                    for k in range(8):
                        o_mm(ph, mm[:], w[:, k, GATE0 + c * 128:GATE0 + (c + 1) * 128], ht[:, k, :],
                             k == 0, k == 7, htbs + [wb] + wbs, [mmb])
                    o_act(ph, gs[:, a, :], mm[:], AF.Sigmoid, [mmb, wb], [gsb], bias=bg[:, c:c + 1], scale=1.0)
                ph.dma("sp", GTv[cg * 768:(cg + 1) * 768, n * 512:(n + 1) * 512].rearrange("(a p) t -> p a t", p=128),
                       gs[:], reads=[gsb])
        ph.emit()


def phase_attn_a(C, l):
    from contextlib import ExitStack
    nc = C.nc
    with ExitStack() as st:
        A, P = mk_alloc(nc, st, f"pa_{l}_")
        ph = Phase(C.S, f"attnA{l}")
        es = A("es", [128, 8], F32)
        esb = ph.buf("es")
        ph.dma("sp", es[:], C.sink[l].to_broadcast([128, 8]), writes=[esb])
        o_act(ph, es[:], es[:], AF.Exp, [esb], [esb])
        qr = Ring(ph, [A(f"q{i}", [128, 512], BF16) for i in range(2)], "q")
        kdr = Ring(ph, [A(f"kd{i}", [128, 3, 2, 2, 64], BF16) for i in range(2)], "kd")
        vr = Ring(ph, [A(f"v{i}", [128, 3, 130], BF16) for i in range(2)], "v")
        qTr = Ring(ph, [A(f"qT{i}", [128, 4, 128], BF16) for i in range(2)], "qT")
        kl_t = [A(f"kTl{i}", [128, 6, 128], BF16) for i in range(2)]
        kh_t = [A(f"kTh{i}", [128, 6, 128], BF16) for i in range(2)]
        klr = Ring(ph, kl_t, "kTl")
        khr = Ring(ph, kh_t, "kTh")
        for i in range(2):
            o_memset(ph, "pool", kl_t[i][:], 0.0, [klr.bufs[i]])
            o_memset(ph, "pool", kh_t[i][:], 0.0, [khr.bufs[i]])
        ptr = Ring(ph, [A(f"pt{i}", [128, 1536], BF16) for i in range(2)], "pt")
        oar = Ring(ph, [A(f"oa{i}", [128, 512], BF16) for i in range(2)], "oa")
        dnr = Ring(ph, [A(f"dn{i}", [128, 8], F32) for i in range(2)], "dn")
        otr = Ring(ph, [A(f"ot{i}", [128, 4, 512], BF16) for i in range(2)], "ot")
        tpq = Ring(ph, [P("tpq", [128, 1024], BF16)], "tpq")
        tpk = Ring(ph, [P("tpk", [128, 1024], BF16)], "tpk")
        psr = Ring(ph, [P("ps", [128, 1536], F32)], "ps")
        por = Ring(ph, [P(f"po{i}", [128, 512], F32) for i in range(2)], "po")
        OTv = C.OT[0:512, :].rearrange("(i p) t -> p i t", p=128)

        def loads(b):
            q, qb = qr.next()
            kd, kdb = kdr.next()
            v, vb = vr.next()
            r0 = PADR + 128 * b
            ph.dma("sp", q[:], C.QK[r0:r0 + 128, QK_AQ:QK_AQ + 512], writes=[qb])
            for g in range(2):
                ksrc = C.QK[r0 - 128:r0 + 256, QK_AK + 64 * g:QK_AK + 64 * g + 64].rearrange("(c p) d -> p c d", p=128)
                for a in range(2):
                    ph.dma("sp", kd[:, :, g, a, :], ksrc, writes=[kdb])
            ph.dma("sp", v[:], C.VA[r0 - 128:r0 + 256, :].rearrange("(c p) d -> p c d", p=128), writes=[vb])
            return (q, qb, kd, kdb, v, vb)

        nxt = loads(0)
        ot, otb = None, None
        for b in range(NT):
            q, qb, kd, kdb, v, vb = nxt
            if b + 1 < NT:
                nxt = loads(b + 1)
            tq, tqb = tpq.next()
            for i in range(4):
                o_tp(ph, tq[:, i * 128:(i + 1) * 128], q[:, i * 128:(i + 1) * 128], C.identb[:], [qb], [tqb])
            qT, qTb = qTr.next()
            o_cp(ph, "dve", qT[:].rearrange("p i t -> p (i t)"), tq[:, 0:512], [tqb], [qTb])
            tk, tkb = tpk.next()
            for c in range(3):
                for g in range(2):
                    o_tp(ph, tk[:, (c * 2 + g) * 128:(c * 2 + g + 1) * 128],
                         kd[:, c, g, :, :].rearrange("p a d -> p (a d)"), C.identb[:], [kdb], [tkb])
            kl, klb = klr.next()
            kh, khb = khr.next()
            o_cp(ph, "act", kl[0:64, :, :].rearrange("p s t -> p (s t)"), tk[0:64, 0:768], [tkb], [klb])
            o_cp(ph, "dve", kh[64:128, :, :].rearrange("p s t -> p (s t)"), tk[64:128, 0:768], [tkb], [khb])
            oa, oab = oar.next()
            dn, dnb = dnr.next()
            for g in range(2):
                ps, psb = psr.next()
                for c in range(3):
                    for j in range(4):
                        h = 4 * g + j
                        i, par = h // 2, h % 2
                        kk_, kkb = (kl, klb) if par == 0 else (kh, khb)
                        o_mm(ph, ps[:, (c * 4 + j) * 128:(c * 4 + j + 1) * 128], kk_[:, c * 2 + g, :],
                             qT[:, i, :], True, True, [kkb, qTb], [psb])
                pt, ptb = ptr.next()
                for c in range(3):
                    o_act(ph, pt[:, c * 512:(c + 1) * 512], ps[:, c * 512:(c + 1) * 512], AF.Exp, [psb], [ptb], scale=0.125)
                o_tt(ph, "dve", pt[:], pt[:], C.EA[:, g * 1536:(g + 1) * 1536], ALU.mult, [ptb], [ptb])
                po, pob = por.next()
                for j in range(4):
                    for c in range(3):
                        o_mm(ph, po[:, j * 65:(j + 1) * 65], pt[:, (c * 4 + j) * 128:(c * 4 + j + 1) * 128],
                             v[:, c, g * 65:(g + 1) * 65], c == 0, c == 2, [ptb, vb], [pob])
                pov = po[:, 0:260].rearrange("p (j d) -> p j d", j=4)
                o_tt(ph, "dve", dn[:, g * 4:(g + 1) * 4], pov[:, :, 64], es[:, g * 4:(g + 1) * 4], ALU.add, [pob, esb], [dnb])
                o_rcp(ph, dn[:, g * 4:(g + 1) * 4], dn[:, g * 4:(g + 1) * 4], [dnb], [dnb])
                o_tt(ph, "dve", oa[:, g * 256:(g + 1) * 256].rearrange("p (j d) -> p j d", j=4), pov[:, :, 0:64],
                     dn[:, g * 4:(g + 1) * 4].unsqueeze(2).to_broadcast([128, 4, 64]), ALU.mult, [pob, dnb], [oab])
            to, tob = tpq.next()
            for i in range(4):
                o_tp(ph, to[:, 512 + i * 128:512 + (i + 1) * 128], oa[:, i * 128:(i + 1) * 128], C.identb[:], [oab], [tob])
            if b % 4 == 0:
                ot, otb = otr.next()
            o_cp(ph, "act", ot[:, :, (b % 4) * 128:(b % 4 + 1) * 128], to[:, 512:1024].rearrange("p (i t) -> p i t", i=4), [tob], [otb])
            if b % 4 == 3:
                ph.dma("sp", OTv[:, :, (b // 4) * 512:(b // 4 + 1) * 512], ot[:], reads=[otb])
        ph.emit()


def phase_attn_b(C, l, gi):
    from contextlib import ExitStack
    nc = C.nc
    dil = B_CFG[gi][1]
    Ls = T // dil
    nb = Ls // 128
    NBK = 4
    with ExitStack() as st:
        A, P = mk_alloc(nc, st, f"pb_{l}_{gi}_")
        q_t = [A(f"q{i}", [128, 256], BF16) for i in range(NBK)]
        k_t = [A(f"k{i}", [128, 2, 256], BF16) for i in range(NBK)]
        v_t = [A(f"v{i}", [128, 2, 260], BF16) for i in range(NBK)]
        qT_t = [A(f"qT{i}", [128, 2, 128], BF16) for i in range(2)]
        kl_t = [A(f"kTl{i}", [128, 4, 128], BF16) for i in range(2)]
        kh_t = [A(f"kTh{i}", [128, 4, 128], BF16) for i in range(2)]
        pt_t = [A(f"pt{i}", [128, 1024], BF16) for i in range(NBK)]
        sb_t = [A(f"sb{i}", [128, 1024], F32) for i in range(2)]
        nb_t = [A(f"nb{i}", [128, 260], F32) for i in range(NBK)]
        tp_t = [P(f"tp{i}", [128, 1024], BF16) for i in range(2)]
        ps_t = [P("ps", [128, 1024], F32)]
        po_t = [P(f"po{i}", [128, 512], F32) for i in range(NBK)]
        QKv = C.QK.rearrange("(s d) c -> d s c", d=dil)
        VBv = C.VB[gi].rearrange("(s d) c -> d s c", d=dil)
        NBv = C.NB[gi].rearrange("(s d) c -> d s c", d=dil)
        sp0 = PADR // dil
        ph = Phase(C.S, f"attnB{l}_{gi}_init")
        zb = ph.buf("z")
        for i in range(2):
            o_memset(ph, "pool", kl_t[i][:], 0.0, [zb])
            o_memset(ph, "pool", kh_t[i][:], 0.0, [zb])
        ph.emit()
        blocks = [(r, b) for r in range(dil) for b in range(nb)]
        for g0 in range(0, len(blocks), NBK):
            grp = blocks[g0:g0 + NBK]
            ph = Phase(C.S, f"attnB{l}_{gi}_{g0}")
            tpr = Ring(ph, tp_t, "tp")
            psr = Ring(ph, ps_t, "ps")
            qTr = Ring(ph, qT_t, "qT")
            klr = Ring(ph, kl_t, "kl")
            khr = Ring(ph, kh_t, "kh")
            sbr = Ring(ph, sb_t, "sb")
            ld = []
            for i, (r, b) in enumerate(grp):
                qb, kb, vb = ph.buf(), ph.buf(), ph.buf()
                s0 = sp0 + 128 * b
                ph.dma("sp", q_t[i][:], QKv[r, s0:s0 + 128, QK_BQ[gi]:QK_BQ[gi] + 256], writes=[qb])
                ph.dma("sp", k_t[i][:], QKv[r, s0 - 64:s0 + 192, QK_BK[gi]:QK_BK[gi] + 256].rearrange("(j p) d -> p j d", p=128), writes=[kb])
                ph.dma("sp", v_t[i][:], VBv[r, s0 - 64:s0 + 192, :].rearrange("(j p) d -> p j d", p=128), writes=[vb])
                ld.append((qb, kb, vb))
            ptbs = []
            for i, (r, b) in enumerate(grp):
                q, k = q_t[i], k_t[i]
                qb, kb, vb = ld[i]
                tp, tpb = tpr.next()
                for a in range(2):
                    o_tp(ph, tp[:, a * 128:(a + 1) * 128], q[:, a * 128:(a + 1) * 128], C.identb[:], [qb], [tpb])
                for jp in range(2):
                    for a in range(2):
                        sl = 2 + 2 * jp + a
                        o_tp(ph, tp[:, sl * 128:(sl + 1) * 128], k[:, jp, a * 128:(a + 1) * 128], C.identb[:], [kb], [tpb])
                qT, qTb = qTr.next()
                kl, klb = klr.next()
                kh, khb = khr.next()
                o_cp(ph, "dve", qT[:].rearrange("p s t -> p (s t)"), tp[:, 0:256], [tpb], [qTb])
                o_cp(ph, "act", kl[0:64, :, :].rearrange("p s t -> p (s t)"), tp[0:64, 256:768], [tpb], [klb])
                o_cp(ph, "dve", kh[64:128, :, :].rearrange("p s t -> p (s t)"), tp[64:128, 256:768], [tpb], [khb])
                ps, psb = psr.next()
                for jp in range(2):
                    for h in range(4):
                        a, par = h // 2, h % 2
                        kk_, kkb = (kl, klb) if par == 0 else (kh, khb)
                        o_mm(ph, ps[:, (jp * 4 + h) * 128:(jp * 4 + h + 1) * 128], kk_[:, 2 * jp + a, :],
                             qT[:, a, :], True, True, [kkb, qTb], [psb])
                sb_, sbb = sbr.next()
                for jp in range(2):
                    o_stt(ph, sb_[:, jp * 512:(jp + 1) * 512], ps[:, jp * 512:(jp + 1) * 512], 0.125,
                          C.BB[:, gi * 1024 + jp * 512:gi * 1024 + (jp + 1) * 512], ALU.mult, ALU.add, [psb], [sbb])
                ptb = ph.buf()
                o_act(ph, pt_t[i][:], sb_[:], AF.Exp, [sbb], [ptb])
                ptbs.append(ptb)
            pobs = []
            for i, (r, b) in enumerate(grp):
                pt, v, po = pt_t[i], v_t[i], po_t[i]
                pob = ph.buf()
                for h in range(4):
                    for jp in range(2):
                        o_mm(ph, po[:, h * 65:(h + 1) * 65], pt[:, (jp * 4 + h) * 128:(jp * 4 + h + 1) * 128],
                             v[:, jp, h * 65:(h + 1) * 65], jp == 0, jp == 1, [ptbs[i], ld[i][2]], [pob])
                pobs.append(pob)
            for i, (r, b) in enumerate(grp):
                nbb = ph.buf()
                o_cp(ph, "act" if i % 2 == 0 else "dve", nb_t[i][:], po_t[i][:, 0:260], [pobs[i]], [nbb])
                ph.dma("sp", NBv[r, 128 * b:128 * (b + 1), :], nb_t[i][:], reads=[nbb])
            ph.emit()


def phase_attn_bc(C, l):
    from contextlib import ExitStack
    nc = C.nc
    with ExitStack() as st:
        A, P = mk_alloc(nc, st, f"pbc_{l}_")
        ph = Phase(C.S, f"attnBc{l}")
        nr = [Ring(ph, [A(f"n{g}_{i}", [128, 260], F32) for i in range(2)], f"n{g}") for g in range(3)]
        dnr = Ring(ph, [A(f"dn{i}", [128, 4], F32) for i in range(2)], "dn")
        obr = Ring(ph, [A(f"ob{i}", [128, 256], BF16) for i in range(2)], "ob")
        otr = Ring(ph, [A(f"ot{i}", [128, 2, 512], BF16) for i in range(2)], "ot")
        tpr = Ring(ph, [P(f"tp{i}", [128, 1024], BF16) for i in range(2)], "tp")
        OTv = C.OT[512:768, :].rearrange("(i p) t -> p i t", p=128)

        def loads(i):
            res = []
            for g in range(3):
                n, nb_ = nr[g].next()
                ph.dma("sp", n[:], C.NB[g][i * 128:(i + 1) * 128, :], writes=[nb_])
                res.append((n, nb_))
            return res

        nxt = loads(0)
        ot, otb = None, None
        for i in range(NT):
            (n0, b0), (n1, b1), (n2, b2) = nxt
            if i + 1 < NT:
                nxt = loads(i + 1)
            o_tt(ph, "pool", n0[:], n0[:], n1[:], ALU.add, [b0, b1], [b0])
            o_tt(ph, "pool", n0[:], n0[:], n2[:], ALU.add, [b0, b2], [b0])
            nv = n0[:].rearrange("p (h d) -> p h d", h=4)
            dn, dnb = dnr.next()
            o_rcp(ph, dn[:], nv[:, :, 64], [b0], [dnb])
            ob, obb = obr.next()
            o_tt(ph, "dve", ob[:].rearrange("p (h d) -> p h d", h=4), nv[:, :, 0:64],
                 dn[:].unsqueeze(2).to_broadcast([128, 4, 64]), ALU.mult, [b0, dnb], [obb])
            tp, tpb = tpr.next()
            for a in range(2):
                o_tp(ph, tp[:, a * 128:(a + 1) * 128], ob[:, a * 128:(a + 1) * 128], C.identb[:], [obb], [tpb])
            if i % 4 == 0:
                ot, otb = otr.next()
            o_cp(ph, "act", ot[:, :, (i % 4) * 128:(i % 4 + 1) * 128], tp[:, 0:256].rearrange("p (a t) -> p a t", a=2), [tpb], [otb])
            if i % 4 == 3:
                ph.dma("sp", OTv[:, :, (i // 4) * 512:(i // 4 + 1) * 512], ot[:], reads=[otb])
        ph.emit()


def phase_attn_c(C, l):
    from contextlib import ExitStack
    nc = C.nc
    NTL = 2
    with ExitStack() as st:
        A, P = mk_alloc(nc, st, f"pc_{l}_")
        EC = A("EC", [128, 5, 5120], BF16)
        tmp = A("ect", [128, 5120], F32)
        q_t = [A(f"q{i}", [128, 512], BF16) for i in range(NTL)]
        k_t = [A(f"k{i}", [128, 5, 512], BF16) for i in range(NTL)]
        v_t = [A(f"v{i}", [128, 5, 520], BF16) for i in range(NTL)]
        qT_t = [A(f"qT{i}", [128, 4, 128], BF16) for i in range(NTL)]
        kl_t = [A(f"kTl{i}", [128, 5, 4, 128], BF16) for i in range(NTL)]
        kh_t = [A(f"kTh{i}", [128, 5, 4, 128], BF16) for i in range(NTL)]
        C_pt = [A(f"pt{i}", [128, 640], BF16) for i in range(8 * NTL)]
        oc_t = [A(f"oc{i}", [128, 512], BF16) for i in range(NTL)]
        dn_t = [A(f"dn{i}", [128, 8], F32) for i in range(NTL)]
        tps = [P("tp0", [128, 1024], BF16)]
        psx = P("psx", [128, 1536], F32)
        po_t = [P(f"po{i}", [128, 512], F32) for i in range(2 * NTL)]
        OTv = C.OT[768:1280, :].rearrange("(i p) t -> p i t", p=128)
        ph = Phase(C.S, f"attnC{l}_init")
        zb = ph.buf("z")
        for i in range(NTL):
            o_memset(ph, "pool", kl_t[i][:], 0.0, [zb])
            o_memset(ph, "pool", kh_t[i][:], 0.0, [zb])
        ecb = ph.buf("EC")
        tb = ph.buf("tmp")
        for pi in range(5):
            ph.dma("sp", tmp[:], C.biasC[l, pi], writes=[tb])
            o_act(ph, EC[:, pi, :], tmp[:], AF.Exp, [tb], [ecb])
        ph.emit()
        for j0 in range(0, NT, NTL):
            ph = Phase(C.S, f"attnC{l}_{j0}")
            tpr = Ring(ph, tps, "tp")
            ps_b = [ph.buf("psA"), ph.buf("psB")]
            ps_v = [psx[:, 0:640], psx[:, 768:1408]]
            psk = 0
            info = []
            for t in range(NTL):
                j = j0 + t
                qb, kb, vb = ph.buf(), ph.buf(), ph.buf()
                cb0 = min(max(j - 2, 0), 59)
                r0 = PADR + 128 * j
                k0 = PADR + 128 * cb0
                ph.dma("sp", q_t[t][:], C.QK[r0:r0 + 128, QK_CQ:QK_CQ + 512], writes=[qb])
                ph.dma("sp", k_t[t][:], C.QK[k0:k0 + 640, QK_CK:QK_CK + 512].rearrange("(c p) d -> p c d", p=128), writes=[kb])
                ph.dma("sp", v_t[t][:], C.VC[k0:k0 + 640, :].rearrange("(c p) d -> p c d", p=128), writes=[vb])
                info.append((j, qb, kb, vb))
            ptbs = {}
            for t in range(NTL):
                j, qb, kb, vb = info[t]
                q, k, qT, kl, kh = q_t[t], k_t[t], qT_t[t], kl_t[t], kh_t[t]
                qTb, klb, khb = ph.buf(), ph.buf(), ph.buf()
                pat = c_pattern(j)
                tp, tpb = tpr.next()
                for i in range(4):
                    o_tp(ph, tp[:, i * 128:(i + 1) * 128], q[:, i * 128:(i + 1) * 128], C.identb[:], [qb], [tpb])
                    o_tp(ph, tp[:, (4 + i) * 128:(5 + i) * 128], k[:, 0, i * 128:(i + 1) * 128], C.identb[:], [kb], [tpb])
                o_cp(ph, "dve", qT[:].rearrange("p i t -> p (i t)"), tp[:, 0:512], [tpb], [qTb])
                o_cp(ph, "act", kl[0:64, 0, :, :].rearrange("p i t -> p (i t)"), tp[0:64, 512:1024], [tpb], [klb])
                o_cp(ph, "dve", kh[64:128, 0, :, :].rearrange("p i t -> p (i t)"), tp[64:128, 512:1024], [tpb], [khb])
                for f in range(2):
                    tp, tpb = tpr.next()
                    for cc in range(2):
                        c = 1 + 2 * f + cc
                        for i in range(4):
                            o_tp(ph, tp[:, (cc * 4 + i) * 128:(cc * 4 + i + 1) * 128], k[:, c, i * 128:(i + 1) * 128], C.identb[:], [kb], [tpb])
                    o_cp(ph, "act", kl[0:64, 1 + 2 * f:3 + 2 * f, :, :].rearrange("p c i t -> p (c i t)"), tp[0:64, :], [tpb], [klb])
                    o_cp(ph, "dve", kh[64:128, 1 + 2 * f:3 + 2 * f, :, :].rearrange("p c i t -> p (c i t)"), tp[64:128, :], [tpb], [khb])
                for h in range(8):
                    i, par = h // 2, h % 2
                    kk_, kkb = (kl, klb) if par == 0 else (kh, khb)
                    ps, psb = ps_v[psk % 2], ps_b[psk % 2]
                    psk += 1
                    for c in range(5):
                        o_mm(ph, ps[:, c * 128:(c + 1) * 128], kk_[:, c, i, :], qT[:, i, :], True, True, [kkb, qTb], [psb])
                    pt = C_pt[t * 8 + h]
                    ptb = ph.buf()
                    ptbs[(t, h)] = ptb
                    o_act(ph, pt[:], ps, AF.Exp, [psb], [ptb], scale=0.125)
                    o_tt(ph, "dve", pt[:].rearrange("p (c t) -> p c t", c=5), pt[:].rearrange("p (c t) -> p c t", c=5),
                         EC[:, pat, :].rearrange("p (c h t) -> p c h t", c=5, h=8)[:, :, h, :], ALU.mult, [ptb], [ptb])
            po_b = {}
            for t in range(NTL):
                j, qb, kb, vb = info[t]
                for h in range(8):
                    pt, ptb = C_pt[t * 8 + h], ptbs[(t, h)]
                    a = t * 2 + h // 4
                    if a not in po_b:
                        po_b[a] = ph.buf()
                    po, pob = po_t[a], po_b[a]
                    for c in range(5):
                        o_mm(ph, po[:, (h % 4) * 65:(h % 4 + 1) * 65], pt[:, c * 128:(c + 1) * 128],
                             v_t[t][:, c, h * 65:(h + 1) * 65], c == 0, c == 4, [ptb, vb], [pob])
            for t in range(NTL):
                j = info[t][0]
                oc, dn = oc_t[t], dn_t[t]
                ocb, dnb = ph.buf(), ph.buf()
                for a2 in range(2):
                    a = t * 2 + a2
                    pov = po_t[a][:, 0:260].rearrange("p (j d) -> p j d", j=4)
                    o_rcp(ph, dn[:, a2 * 4:(a2 + 1) * 4], pov[:, :, 64], [po_b[a]], [dnb])
                    o_tt(ph, "dve", oc[:, a2 * 256:(a2 + 1) * 256].rearrange("p (j d) -> p j d", j=4), pov[:, :, 0:64],
                         dn[:, a2 * 4:(a2 + 1) * 4].unsqueeze(2).to_broadcast([128, 4, 64]), ALU.mult, [po_b[a], dnb], [ocb])
                ph.dma("sp", C.OC[j * 128:(j + 1) * 128, :], oc[:], reads=[ocb])
            ph.emit()
        ph = Phase(C.S, f"attnC{l}_tr")
        ocr = Ring(ph, [A(f"oc2_{i}", [128, 512], BF16) for i in range(2)], "oc2")
        otr = Ring(ph, [A(f"ot2_{i}", [128, 4, 512], BF16) for i in range(2)], "ot2")
        tpr = Ring(ph, [tps[0]], "tp")
        nxt = ocr.next()
        ph.dma("sp", nxt[0][:], C.OC[0:128, :], writes=[nxt[1]])
        ot2, ot2b = None, None
        for j in range(NT):
            oc2, oc2b = nxt
            if j + 1 < NT:
                nxt = ocr.next()
                ph.dma("sp", nxt[0][:], C.OC[(j + 1) * 128:(j + 2) * 128, :], writes=[nxt[1]])
            tp, tpb = tpr.next()
            for i in range(4):
                o_tp(ph, tp[:, i * 128:(i + 1) * 128], oc2[:, i * 128:(i + 1) * 128], C.identb[:], [oc2b], [tpb])
            if j % 4 == 0:
                ot2, ot2b = otr.next()
            o_cp(ph, "act", ot2[:, :, (j % 4) * 128:(j % 4 + 1) * 128], tp[:, 0:512].rearrange("p (i t) -> p i t", i=4), [tpb], [ot2b])
            if j % 4 == 3:
                ph.dma("sp", OTv[:, :, (j // 4) * 512:(j // 4 + 1) * 512], ot2[:], reads=[ot2b])
        ph.emit()


def phase_merge(C, l):
    from contextlib import ExitStack
    nc = C.nc
    with ExitStack() as st:
        A, P = mk_alloc(nc, st, f"p3_{l}_")
        ph = Phase(C.S, f"merge{l}")
        ln_setup(ph, C, A)
        g, b, cb = load_gb(ph, A, C.ln1g[l], C.ln1b[l], "ln1")
        wbr = A("wbr", [128, 10, 1024], BF16)
        wo = A("wo", [128, 8, 1024], BF16)
        wb = ph.buf("w")
        srcs = [(C.w_bra[l], 4), (C.w_brb[l], 2), (C.w_brc[l], 4)]
        kk = 0
        wbs = []
        for src, nk in srcs:
            for k in range(nk):
                wbc = ph.buf()
                wbs.append(wbc)
                ph.dma("pool", wbr[:, kk, :], src[k * 128:(k + 1) * 128, :], writes=[wbc])
                kk += 1
        for k in range(8):
            wbc = ph.buf()
            wbs.append(wbc)
            ph.dma("pool", wo[:, k, :], C.w_out[l, k * 128:(k + 1) * 128, :], writes=[wbc])
        otr = Ring(ph, [A(f"ot{i}", [128, 10, 512], BF16) for i in range(2)], "ot")
        gtr = Ring(ph, [A(f"gt{i}", [128, 24, 512], BF16) for i in range(2)], "gt")
        t1r = Ring(ph, [A(f"t1_{i}", [128, 512], F32) for i in range(2)], "t1")
        t2r = Ring(ph, [A(f"t2_{i}", [128, 512], F32) for i in range(2)], "t2")
        t3r = Ring(ph, [A(f"t3_{i}", [128, 512], F32) for i in range(2)], "t3")
        mgr = Ring(ph, [A(f"mg{i}", [128, 8, 512], BF16) for i in range(2)], "mg")
        hr = Ring(ph, [A(f"h{i}", [128, D], F32) for i in range(2)], "h")
        zr = Ring(ph, [A(f"z{i}", [128, D], F32) for i in range(5)], "z")
        orr = Ring(ph, [A(f"o{i}", [128, D], F32) for i in range(3)], "o")
        obr = Ring(ph, [A(f"ob{i}", [128, D], BF16) for i in range(2)], "ob")
        par = Ring(ph, [P(f"pa{i}", [128, 512], F32) for i in range(2)], "pa")
        pbr = Ring(ph, [P(f"pb{i}", [128, 512], F32) for i in range(2)], "pb")
        pcr = Ring(ph, [P(f"pc{i}", [128, 512], F32) for i in range(2)], "pc")
        pyr = Ring(ph, [P("py", [128, 1024], F32)], "py")
        OTv = C.OT.rearrange("(i p) t -> p i t", p=128)
        GTv = C.GT.rearrange("(i p) t -> p i t", p=128)

        def loads(n):
            ot, otb = otr.next()
            gt, gtb = gtr.next()
            ph.dma("sp", ot[:], OTv[:, :, n * 512:(n + 1) * 512], writes=[otb])
            for a in range(3):
                ph.dma("sp", gt[:, a * 8:(a + 1) * 8, :], GTv[:, a * 8:(a + 1) * 8, n * 512:(n + 1) * 512], writes=[gtb])
            return ot, otb, gt, gtb

        nxt = loads(0)
        for n in range(T // 512):
            ot, otb, gt, gtb = nxt
            if n + 1 < T // 512:
                nxt = loads(n + 1)
            mg, mgb = mgr.next()
            for mc in range(8):
                pa, pab = par.next()
                pb, pbb = pbr.next()
                pc, pcb = pcr.next()
                for (pp, ppb, k0, nk) in ((pa, pab, 0, 4), (pb, pbb, 4, 2), (pc, pcb, 6, 4)):
                    for k in range(nk):
                        o_mm(ph, pp[:], wbr[:, k0 + k, mc * 128:(mc + 1) * 128], ot[:, k0 + k, :], k == 0, k == nk - 1, wbs + [otb], [ppb])
                t1, t1b = t1r.next()
                t2, t2b = t2r.next()
                t3, t3b = t3r.next()
                o_tt(ph, "dve", t1[:], pa[:], gt[:, mc, :], ALU.mult, [pab, gtb], [t1b])
                o_tt(ph, "dve", t2[:], pb[:], gt[:, 8 + mc, :], ALU.mult, [pbb, gtb], [t2b])
                o_tt(ph, "dve", t3[:], pc[:], gt[:, 16 + mc, :], ALU.mult, [pcb, gtb], [t3b])
                o_tt(ph, "pool", t1[:], t1[:], t2[:], ALU.add, [t1b, t2b], [t1b])
                o_tt(ph, "pool", mg[:, mc, :], t1[:], t3[:], ALU.add, [t1b, t3b], [mgb])
            subs = []
            for sub in range(4):
                i = 4 * n + sub
                h, hb = hr.next()
                ph.dma("sp", h[:], C.H[i * 128:(i + 1) * 128, :], writes=[hb])
                py, pyb = pyr.next()
                for half in range(2):
                    for k in range(8):
                        o_mm(ph, py[:, half * 512:(half + 1) * 512], mg[:, k, sub * 128:(sub + 1) * 128],
                             wo[:, k, half * 512:(half + 1) * 512], k == 0, k == 7, [mgb] + wbs, [pyb])
                z, zb = zr.next()
                o_stt(ph, z[:], h[:], ALPHA, py[:], ALU.mult, ALU.add, [hb, pyb], [zb])
                mv, mvb = ln_s1(ph, C, z[:], zb)
                subs.append((i, z, zb, mv, mvb))
            for (i, z, zb, mv, mvb) in subs:
                ln_s2(ph, C, mv, mvb)
            for (i, z, zb, mv, mvb) in subs:
                o, ob = orr.next()
                ln_s3(ph, C, z[:], zb, mv, mvb, o[:], ob, g[:], b[:], cb)
                ph.dma("sp", C.H1[i * 128:(i + 1) * 128, :], o[:], reads=[ob])
                o16, o16b = obr.next()
                o_cp(ph, "act", o16[:], o[:], [ob], [o16b])
                ph.dma("sp", C.H1b[i * 128:(i + 1) * 128, :], o16[:], reads=[o16b])
        ph.emit()


def phase_route(C, l):
    from contextlib import ExitStack
    nc = C.nc
    with ExitStack() as st:
        A, P = mk_alloc(nc, st, f"p3b_{l}_")
        ph = Phase(C.S, f"route{l}")
        wr = A("wr", [128, 8, 36], F32)
        brr = A("brr", [128, 36], F32)
        wb = ph.buf("w")
        ph.dma("sp", wr[:], C.wr[l].rearrange("(k p) e -> p k e", p=128), writes=[wb])
        ph.dma("sp", brr[:], C.br[l].to_broadcast([128, 36]), writes=[wb])
        LG = A("LG", [128, NT, 36], F32)
        lgb = ph.buf("LG")
        hr = Ring(ph, [A(f"h{i}", [128, D], F32) for i in range(2)], "h")
        hTr = Ring(ph, [A(f"hT{i}", [128, 8, 128], F32) for i in range(2)], "hT")
        tpr = Ring(ph, [P("tp", [128, 8, 128], F32)], "tp")
        plr = Ring(ph, [P("pl", [128, 512], F32)], "pl")
        ppr = Ring(ph, [P(f"pp{i}", [128, 512], F32) for i in range(2)], "pp")
        pcr = Ring(ph, [P(f"pcs{i}", [128, 512], F32) for i in range(2)], "pcs")
        nxt = hr.next()
        ph.dma("sp", nxt[0][:], C.H1[0:128, :], writes=[nxt[1]])
        for i in range(NT):
            h, hb = nxt
            if i + 1 < NT:
                nxt = hr.next()
                ph.dma("sp", nxt[0][:], C.H1[(i + 1) * 128:(i + 2) * 128, :], writes=[nxt[1]])
            tp, tpb = tpr.next()
            for k in range(8):
                o_tp(ph, tp[:, k, :], h[:, k * 128:(k + 1) * 128], C.identf[:], [hb], [tpb])
            hT, hTb = hTr.next()
            o_cp(ph, "act", hT[:], tp[:], [tpb], [hTb])
            pl, plb = plr.next()
            for k in range(8):
                o_mm(ph, pl[:, 0:36], hT[:, k, :], wr[:, k, :], k == 0, k == 7, [hTb, wb], [plb])
            o_tt(ph, "dve", LG[:, i, :], pl[:, 0:36], brr[:], ALU.add, [plb, wb], [lgb])
        def S_(name, shape, dt=F32):
            return A(name, shape, dt), ph.buf(name)
        lg = LG[:, :, 0:4]
        le = LG[:, :, 4:36].rearrange("p n (g e) -> p n g e", g=4)
        gmax, gmb = S_("gmax", [128, NT])
        o_red(ph, gmax[:], lg, ALU.max, [lgb], [gmb])
        goh, gohb = S_("goh", [128, NT, 4])
        o_tt(ph, "dve", goh[:], lg, gmax[:].unsqueeze(2).to_broadcast([128, NT, 4]), ALU.is_equal, [lgb, gmb], [gohb])
        gex, gexb = S_("gex", [128, NT, 4])
        o_tt(ph, "dve", gex[:], lg, gmax[:].unsqueeze(2).to_broadcast([128, NT, 4]), ALU.subtract, [lgb, gmb], [gexb])
        o_act(ph, gex[:], gex[:], AF.Exp, [gexb], [gexb])
        gw, gwb = S_("gw", [128, NT])
        o_red(ph, gw[:], gex[:], ALU.add, [gexb], [gwb])
        o_rcp(ph, gw[:], gw[:], [gwb], [gwb])
        esel, eselb = S_("esel", [128, NT, 8])
        etmp, etmpb = S_("etmp", [128, NT, 8])
        for g in range(4):
            dst, dstb = (esel, eselb) if g == 0 else (etmp, etmpb)
            o_tt(ph, "dve", dst[:], le[:, :, g, :], goh[:, :, g].unsqueeze(2).to_broadcast([128, NT, 8]), ALU.mult, [lgb, gohb], [dstb])
            if g > 0:
                o_tt(ph, "dve", esel[:], esel[:], etmp[:], ALU.add, [eselb, etmpb], [eselb])
        m1, m1b = S_("m1", [128, NT])
        o_red(ph, m1[:], esel[:], ALU.max, [eselb], [m1b])
        oh1, oh1b = S_("oh1", [128, NT, 8])
        o_tt(ph, "dve", oh1[:], esel[:], m1[:].unsqueeze(2).to_broadcast([128, NT, 8]), ALU.is_equal, [eselb, m1b], [oh1b])
        e2, e2b = S_("e2", [128, NT, 8])
        o_ts(ph, "dve", e2[:], oh1[:], -1e30, None, ALU.mult, None, [oh1b], [e2b])
        o_tt(ph, "dve", e2[:], e2[:], esel[:], ALU.add, [e2b, eselb], [e2b])
        m2, m2b = S_("m2", [128, NT])
        o_red(ph, m2[:], e2[:], ALU.max, [e2b], [m2b])
        oh2, oh2b = S_("oh2", [128, NT, 8])
        o_tt(ph, "dve", oh2[:], e2[:], m2[:].unsqueeze(2).to_broadcast([128, NT, 8]), ALU.is_equal, [e2b, m2b], [oh2b])
        ee, eeb = S_("ee", [128, NT])
        o_tt(ph, "dve", ee[:], m2[:], m1[:], ALU.subtract, [m1b, m2b], [eeb])
        o_act(ph, ee[:], ee[:], AF.Exp, [eeb], [eeb])
        p1, p1b = S_("p1", [128, NT])
        o_ts(ph, "dve", p1[:], ee[:], 1.0, None, ALU.add, None, [eeb], [p1b])
        o_rcp(ph, p1[:], p1[:], [p1b], [p1b])
        gwt = C.GW
        gwtb = ph.buf("GW")
        o_tt(ph, "dve", gwt[:, :, 1], ee[:], p1[:], ALU.mult, [eeb, p1b], [gwtb])
        o_tt(ph, "dve", gwt[:, :, 1], gwt[:, :, 1], gw[:], ALU.mult, [gwtb, gwb], [gwtb])
        o_tt(ph, "dve", gwt[:, :, 0], p1[:], gw[:], ALU.mult, [p1b, gwb], [gwtb])
        M1, M1b = S_("M1", [128, NT, 4, 8])
        M2, M2b = S_("M2", [128, NT, 4, 8])
        for g in range(4):
            o_tt(ph, "dve", M1[:, :, g, :], oh1[:], goh[:, :, g].unsqueeze(2).to_broadcast([128, NT, 8]), ALU.mult, [oh1b, gohb], [M1b])
            o_tt(ph, "dve", M2[:, :, g, :], oh2[:], goh[:, :, g].unsqueeze(2).to_broadcast([128, NT, 8]), ALU.mult, [oh2b, gohb], [M2b])
        Mb16, Mb16b = S_("Mb16", [128, NT * 32], BF16)
        o_tt(ph, "dve", Mb16[:], M1[:].rearrange("p n g e -> p (n g e)"), M2[:].rearrange("p n g e -> p (n g e)"), ALU.add, [M1b, M2b], [Mb16b])
        POS, POSb = S_("POS", [128, NT * 32])
        CS, CSb = S_("CS", [128, NT * 32])
        for ch in range(4):
            pp, ppb = ppr.next()
            o_mm(ph, pp[:], C.Umat[:], Mb16[:, ch * 512:(ch + 1) * 512], True, True, [Mb16b], [ppb])
            o_cp(ph, "dve", POS[:, ch * 512:(ch + 1) * 512], pp[:], [ppb], [POSb])
            pcs, pcsb = pcr.next()
            o_mm(ph, pcs[:], C.ones[:], Mb16[:, ch * 512:(ch + 1) * 512], True, True, [Mb16b], [pcsb])
            o_cp(ph, "act", CS[:, ch * 512:(ch + 1) * 512], pcs[:], [pcsb], [CSb])
        SA, SAb = S_("SA", [128, NT * 32])
        SB, SBb = S_("SB", [128, NT * 32])
        o_cp(ph, "dve", SA[:], CS[:], [CSb], [SAb])
        cur, curb, oth, othb = SA, SAb, SB, SBb
        s_ = 1
        while s_ < NT:
            o_cp(ph, "dve", oth[:, 0:s_ * 32], cur[:, 0:s_ * 32], [curb], [othb])
            o_tt(ph, "dve", oth[:, s_ * 32:], cur[:, s_ * 32:], cur[:, 0:(NT - s_) * 32], ALU.add, [curb], [othb])
            cur, curb, oth, othb = oth, othb, cur, curb
            s_ *= 2
        o_tt(ph, "dve", POS[:], POS[:], cur[:], ALU.add, [POSb, curb], [POSb])
        o_tt(ph, "dve", POS[:], POS[:], CS[:], ALU.subtract, [POSb, CSb], [POSb])
        o_ts(ph, "dve", POS[:], POS[:], float(CAP - 1), None, ALU.min, None, [POSb], [POSb])
        o_tt(ph, "dve", POS[:].rearrange("p (n e) -> p n e", e=32), POS[:].rearrange("p (n e) -> p n e", e=32),
             C.ecap[:].unsqueeze(1).to_broadcast([128, NT, 32]), ALU.add, [POSb], [POSb])
        DF, DFb = S_("DF", [128, NT, 2])
        for kx, (Mx, Mxb) in enumerate(((M1, M1b), (M2, M2b))):
            o_tt(ph, "dve", Mx[:].rearrange("p n g e -> p (n g e)"), Mx[:].rearrange("p n g e -> p (n g e)"), POS[:], ALU.mult, [Mxb, POSb], [Mxb])
            o_red(ph, DF[:, :, kx], Mx[:].rearrange("p n g e -> p n (g e)"), ALU.add, [Mxb], [DFb])
        dib = ph.buf("DI")
        o_cp(ph, "dve", C.DI[:], DF[:], [DFb], [dib])
        xr = Ring(ph, [A(f"x{i}", [128, D], BF16) for i in range(3)], "x")
        for i in range(NT):
            xt, xb = xr.next()
            ph.dma("sp", xt[:], C.H1b[i * 128:(i + 1) * 128, :], writes=[xb])
            for kx in range(2):
                ph.dma_fn("pool", (lambda e, xt=xt, i=i, kx=kx: e.indirect_dma_start(
                    out=C.XS, out_offset=bass.IndirectOffsetOnAxis(ap=C.DI[:, i, kx:kx + 1], axis=0), in_=xt[:], in_offset=None)),
                    reads=[xb, dib])
        ph.emit()


def phase_experts(C, l):
    from contextlib import ExitStack
    nc = C.nc
    NST = CAP // 128
    HN = CAP // 2
    with ExitStack() as st:
        A, P = mk_alloc(nc, st, f"p4_{l}_")
        ph = Phase(C.S, f"experts{l}")
        wgr = Ring(ph, [A(f"wg{i}", [128, 8, 512], BF16) for i in range(2)], "wg")
        wur = Ring(ph, [A(f"wu{i}", [128, 8, 512], BF16) for i in range(2)], "wu")
        wdr = Ring(ph, [A(f"wd{i}", [128, 4, 1024], BF16) for i in range(2)], "wd")
        wdsr = Ring(ph, [A(f"wds{i}", [128, 4, 1024], F32) for i in range(2)], "wds")
        xsr = Ring(ph, [A(f"xs{i}", [128, NST, D], BF16) for i in range(2)], "xs")
        xTr = Ring(ph, [A(f"xT{i}", [128, 8, CAP], BF16) for i in range(2)], "xT")
        sgr = Ring(ph, [A(f"sg{i}", [128, HN], F32) for i in range(2)], "sg")
        hdr = Ring(ph, [A(f"hd{i}", [128, 4, HN], BF16) for i in range(2)], "hd")
        yor = Ring(ph, [A(f"yo{i}", [128, D], BF16) for i in range(3)], "yo")
        tpr = Ring(ph, [P(f"tp{i}", [128, 1024], BF16) for i in range(2)], "tp")
        pgr = Ring(ph, [P(f"pg{i}", [128, 512], F32) for i in range(2)], "pg")
        pur = Ring(ph, [P(f"pu{i}", [128, 512], F32) for i in range(2)], "pu")
        pyr = Ring(ph, [P("py", [128, 1024], F32)], "py")

        def loads(e):
            wg, wgb = wgr.next()
            wu, wub = wur.next()
            wd, wdb = wdr.next()
            xs, xsb = xsr.next()
            ph.dma("pool", wg[:], C.w_eg[l, e].rearrange("(k p) f -> p k f", p=128), writes=[wgb])
            ph.dma("pool", wu[:], C.w_eu[l, e].rearrange("(k p) f -> p k f", p=128), writes=[wub])
            wds, wdsb = wdsr.next()
            ph.dma("sp", wds[:], C.w_ed[l, e].rearrange("(k p) f -> p k f", p=128), writes=[wdsb])
            o_cp(ph, "pool", wd[:], wds[:], [wdsb], [wdb])
            ph.dma("sp", xs[:], C.XS[e * CAP:(e + 1) * CAP, :].rearrange("(s p) d -> p s d", p=128), writes=[xsb])
            return wg, wgb, wu, wub, wd, wdb, xs, xsb

        nxt = loads(0)
        for e in range(32):
            wg, wgb, wu, wub, wd, wdb, xs, xsb = nxt
            if e + 1 < 32:
                nxt = loads(e + 1)
            xT, xTb = xTr.next()
            for s_ in range(NST):
                tp, tpb = tpr.next()
                for k in range(8):
                    o_tp(ph, tp[:, k * 128:(k + 1) * 128], xs[:, s_, k * 128:(k + 1) * 128], C.identb[:], [xsb], [tpb])
                o_cp(ph, "dve" if s_ % 2 == 0 else "act", xT[:, :, s_ * 128:(s_ + 1) * 128],
                     tp[:].rearrange("p (k t) -> p k t", k=8), [tpb], [xTb])
            for hh in range(2):
                hd, hdb = hdr.next()
                for fc in range(4):
                    pg, pgb = pgr.next()
                    pu, pub = pur.next()
                    for k in range(8):
                        o_mm(ph, pg[:, 0:HN], wg[:, k, fc * 128:(fc + 1) * 128], xT[:, k, hh * HN:(hh + 1) * HN], k == 0, k == 7, [wgb, xTb], [pgb])
                    for k in range(8):
                        o_mm(ph, pu[:, 0:HN], wu[:, k, fc * 128:(fc + 1) * 128], xT[:, k, hh * HN:(hh + 1) * HN], k == 0, k == 7, [wub, xTb], [pub])
                    sg, sgb = sgr.next()
                    o_act(ph, sg[:], pg[:, 0:HN], AF.Silu, [pgb], [sgb])
                    o_tt(ph, "dve", hd[:, fc, :], sg[:], pu[:, 0:HN], ALU.mult, [sgb, pub], [hdb])
                for s_ in range(NST // 2):
                    py, pyb = pyr.next()
                    for half in range(2):
                        for k in range(4):
                            o_mm(ph, py[:, half * 512:(half + 1) * 512], hd[:, k, s_ * 128:(s_ + 1) * 128],
                                 wd[:, k, half * 512:(half + 1) * 512], k == 0, k == 3, [hdb, wdb], [pyb])
                    yo, yob = yor.next()
                    o_cp(ph, "dve" if s_ % 2 == 0 else "act", yo[:], py[:], [pyb], [yob])
                    r0 = e * CAP + hh * HN + s_ * 128
                    ph.dma("sp", C.R[r0:r0 + 128, :], yo[:], reads=[yob])
        ph.emit()


def phase_combine(C, l, dst):
    from contextlib import ExitStack
    nc = C.nc
    with ExitStack() as st:
        A, P = mk_alloc(nc, st, f"p5_{l}_")
        ph = Phase(C.S, f"combine{l}")
        ln_setup(ph, C, A)
        g, b, cb = load_gb(ph, A, C.ln2g[l], C.ln2b[l], "ln2")
        r1r = Ring(ph, [A(f"r1_{i}", [128, D], BF16) for i in range(2)], "r1")
        r2r = Ring(ph, [A(f"r2_{i}", [128, D], BF16) for i in range(2)], "r2")
        hr = Ring(ph, [A(f"h{i}", [128, D], F32) for i in range(2)], "h")
        yr = Ring(ph, [A(f"y{i}", [128, D], F32) for i in range(2)], "y")
        zr = Ring(ph, [A(f"z{i}", [128, D], F32) for i in range(5)], "z")
        orr = Ring(ph, [A(f"o{i}", [128, D], F32) for i in range(3)], "o")

        def loads(i):
            r1, r1b = r1r.next()
            r2, r2b = r2r.next()
            h, hb = hr.next()
            for kx, (rt, rb) in enumerate(((r1, r1b), (r2, r2b))):
                ph.dma_fn("pool", (lambda e, rt=rt, i=i, kx=kx: e.indirect_dma_start(
                    out=rt[:], out_offset=None, in_=C.R, in_offset=bass.IndirectOffsetOnAxis(ap=C.DI[:, i, kx:kx + 1], axis=0))),
                    writes=[rb])
            ph.dma("sp", h[:], C.H1[i * 128:(i + 1) * 128, :], writes=[hb])
            return r1, r1b, r2, r2b, h, hb

        nxt = loads(0)
        grp = []
        for i in range(NT):
            r1, r1b, r2, r2b, h, hb = nxt
            if i + 1 < NT:
                nxt = loads(i + 1)
            y, yb = yr.next()
            o_act(ph, y[:], r1[:], AF.Copy, [r1b], [yb], scale=C.GW[:, i, 0:1])
            o_stt(ph, y[:], r2[:], C.GW[:, i, 1:2], y[:], ALU.mult, ALU.add, [r2b, yb], [yb])
            z, zb = zr.next()
            o_stt(ph, z[:], h[:], ALPHA, y[:], ALU.mult, ALU.add, [hb, yb], [zb])
            mv, mvb = ln_s1(ph, C, z[:], zb)
            grp.append((i, z, zb, mv, mvb))
            if len(grp) == 4 or i == NT - 1:
                for (ii, z_, zb_, mv_, mvb_) in grp:
                    ln_s2(ph, C, mv_, mvb_)
                for (ii, z_, zb_, mv_, mvb_) in grp:
                    o, ob = orr.next()
                    ln_s3(ph, C, z_[:], zb_, mv_, mvb_, o[:], ob, g[:], b[:], cb, geng="dve", beng="pool")
                    ph.dma("sp", dst[ii * 128:(ii + 1) * 128, :], o[:], reads=[ob])
                grp = []
        ph.emit()


def phase_pre(C):
    from contextlib import ExitStack
    nc = C.nc
    with ExitStack() as st:
        A, P = mk_alloc(nc, st, "pre_")
        ph = Phase(C.S, "pre")
        cb = ph.buf("const")
        o_memset(ph, "pool", C.identb[:], 0.0, [cb])
        ph.op("pool", lambda e: e.affine_select(out=C.identb[:], in_=C.identb[:], pattern=[[-1, 128]], compare_op=ALU.not_equal,
                                                 fill=1.0, base=0, channel_multiplier=1), [cb], [cb])
        o_memset(ph, "pool", C.identf[:], 0.0, [cb])
        ph.op("pool", lambda e: e.affine_select(out=C.identf[:], in_=C.identf[:], pattern=[[-1, 128]], compare_op=ALU.not_equal,
                                                 fill=1.0, base=0, channel_multiplier=1), [cb], [cb])
        o_memset(ph, "pool", C.Umat[:], 1.0, [cb])
        ph.op("pool", lambda e: e.affine_select(out=C.Umat[:], in_=C.Umat[:], pattern=[[1, 128]], compare_op=ALU.is_gt,
                                                 fill=0.0, base=0, channel_multiplier=-1), [cb], [cb])
        o_memset(ph, "pool", C.ones[:], 1.0, [cb])
        o_memset(ph, "pool", C.eps[:], LN_EPS, [cb])
        o_memset(ph, "pool", C.fence[:], 0.0, [cb])
        eci = A("eci", [128, 32], I32)
        ph.op("pool", lambda e: e.iota(eci[:], pattern=[[CAP, 32]], base=0, channel_multiplier=0), [cb], [cb])
        o_cp(ph, "pool", C.ecap[:], eci[:], [cb], [cb])
        tmpr = Ring(ph, [A(f"bt{i}", [128, 3072], F32) for i in range(2)], "bt")
        tmp, tb = tmpr.next()
        ph.dma("sp", tmp[:], C.biasA, writes=[tb])
        o_act(ph, C.EA[:], tmp[:], AF.Exp, [tb], [cb])
        ph.dma("sp", C.BB[:].rearrange("p (g c) -> p g c", g=3), C.biasB.rearrange("g p c -> p g c"), writes=[cb])
        zt = A("zt", [128, 8 * 520], BF16)
        zb = ph.buf("zt")
        o_memset(ph, "pool", zt[:], 0.0, [zb])
        na = PADR // 128
        for base in (0, PADR + T):
            for a in range(na):
                r0 = base + a * 128
                ph.dma("sp", C.QK[r0:r0 + 128, :], zt[:, 0:QK_W], reads=[zb])
            for arr, wd_ in [(C.VA, 130), (C.VB[0], 260), (C.VB[1], 260), (C.VB[2], 260), (C.VC, 520)]:
                ph.dma("sp", arr[base:base + PADR, :].rearrange("(p a) c -> p (a c)", a=na), zt[:, 0:na * wd_], reads=[zb])
        ph.emit()


def run_b(C, l):
    import os
    groups = [int(g) for g in os.environ.get("DBG_BG", "0,1,2").split(",") if g != ""]
    for gi in groups:
        phase_attn_b(C, l, gi)
    if os.environ.get("DBG_BC", "1") == "1":
        phase_attn_bc(C, l)


INPUT_SPECS = [
    ("x", [T, D]), ("ln0_g", [1, D]), ("ln0_b", [1, D]),
    ("biasA", [128, 3072]), ("biasB", [3, 128, 1024]), ("biasC", [DEPTH, 5, 128, 5120]),
    ("w_in", [DEPTH, D, 7680]), ("bgT", [DEPTH, 128, 24]), ("sink", [DEPTH, 1, 8]),
    ("w_bra", [DEPTH, 512, D]), ("w_brb", [DEPTH, 256, D]), ("w_brc", [DEPTH, 512, D]), ("w_out", [DEPTH, D, D]),
    ("ln1g", [DEPTH, 1, D]), ("ln1b", [DEPTH, 1, D]), ("wr", [DEPTH, D, 36]), ("br", [DEPTH, 1, 36]),
    ("w_eg", [DEPTH, 32, D, 512]), ("w_eu", [DEPTH, 32, D, 512]), ("w_ed", [DEPTH, 32, 512, D]),
    ("ln2g", [DEPTH, 1, D]), ("ln2b", [DEPTH, 1, D]),
]


def build(debug=(), upto=None, only_inputs=None, only_steps=None):
    from contextlib import ExitStack
    nc = bass.Bass("TRN2", target_bir_lowering=False)
    C = Ctx()
    C.nc = nc
    for name, shape in INPUT_SPECS:
        if only_inputs is not None and name not in only_inputs:
            continue
        ap = nc.dram_tensor(name, list(shape), F32, kind="ExternalInput").ap()
        setattr(C, {"ln0_g": "ln0g", "ln0_b": "ln0b"}.get(name, name), ap)

    def scr(name, shape, dt):
        kind = "ExternalOutput" if name in debug else "Internal"
        return nc.dram_tensor(name, list(shape), dt, kind=kind).ap()

    C.out = nc.dram_tensor("out", [T, D], F32, kind="ExternalOutput").ap()
    C.H = scr("H", [T, D], F32)
    C.QK = scr("QK", [ROWS, QK_W], BF16)
    C.VA = scr("VA", [ROWS, 130], BF16)
    C.VB = [scr(f"VB{g}", [ROWS, 260], BF16) for g in range(3)]
    C.VC = scr("VC", [ROWS, 520], BF16)
    C.GT = scr("GT", [3072, T], BF16)
    C.NB = [scr(f"NB{g}", [T, 260], F32) for g in range(3)]
    C.OT = scr("OT", [1280, T], BF16)
    C.OC = scr("OC", [T, 512], BF16)
    C.H1 = scr("H1", [T, D], F32)
    C.H1b = scr("H1b", [T, D], BF16)
    C.XS = scr("XS", [NSLOT, D], BF16)
    C.R = scr("R", [NSLOT, D], BF16)
    with ExitStack() as st:
        C.S = Sched(nc, st)
        A, _ = mk_alloc(nc, st, "g_")
        C.identb = A("identb", [128, 128], BF16)
        C.identf = A("identf", [128, 128], F32)
        C.Umat = A("Umat", [128, 128], BF16)
        C.ones = A("ones", [128, 128], BF16)
        C.eps = A("eps", [128, 1], F32)
        C.ecap = A("ecap", [128, 32], F32)
        C.EA = A("EA", [128, 3072], BF16)
        C.BB = A("BB", [128, 3072], F32)
        C.fence = A("fence", [128, 2], F32)
        C.DI = A("DI", [128, NT, 2], I32)
        C.GW = A("GW", [128, NT, 2], F32)
        steps = [("pre", lambda: phase_pre(C)), ("ln0", lambda: phase_ln0(C))]
        for l in range(DEPTH):
            dst = C.H if l + 1 < DEPTH else C.out
            steps += [
                (f"proj{l}", lambda l=l: phase_proj(C, l)),
                (f"attnA{l}", lambda l=l: phase_attn_a(C, l)),
                (f"attnB{l}", lambda l=l: run_b(C, l)),
                (f"attnC{l}", lambda l=l: phase_attn_c(C, l)),
                (f"merge{l}", lambda l=l: phase_merge(C, l)),
                (f"route{l}", lambda l=l: phase_route(C, l)),
                (f"experts{l}", lambda l=l: phase_experts(C, l)),
                (f"combine{l}", lambda l=l, dst=dst: phase_combine(C, l, dst)),
            ]
        for name, fn in steps:
            if only_steps is not None and name not in only_steps:
                continue
            fn()
            if upto is not None and name == upto:
                break
    return nc


def host_inputs(inp):
    f = lambda a: np.ascontiguousarray(np.asarray(a), dtype=np.float32)
    rel_bias = f(inp["rel_bias"])
    rpb_c = f(inp["rpb_c"])
    ba, bb, bc = host_bias_tables(rel_bias, rpb_c)
    b_gate = f(inp["b_gate"])
    w_rg, w_re = f(inp["w_rg"]), f(inp["w_re"])
    wr = np.concatenate([w_rg, w_re.transpose(0, 2, 1, 3).reshape(DEPTH, D, 32)], axis=2)
    br = np.concatenate([f(inp["b_rg"]), f(inp["b_re"]).reshape(DEPTH, 32)], axis=1).reshape(DEPTH, 1, 36)
    shared = {
        "ln0_g": f(inp["ln0_g"]).reshape(1, D), "ln0_b": f(inp["ln0_b"]).reshape(1, D),
        "biasA": np.ascontiguousarray(ba.reshape(128, 3072)),
        "biasB": np.ascontiguousarray(bb.reshape(3, 128, 1024)),
        "biasC": np.ascontiguousarray(bc.reshape(DEPTH, 5, 128, 5120)),
        "w_in": f(inp["w_in"]),
        "bgT": np.ascontiguousarray(b_gate.reshape(DEPTH, 24, 128).transpose(0, 2, 1)),
        "sink": f(inp["sink_a"]).reshape(DEPTH, 1, 8),
        "w_bra": f(inp["w_br_a"]), "w_brb": f(inp["w_br_b"]), "w_brc": f(inp["w_br_c"]), "w_out": f(inp["w_out"]),
        "ln1g": f(inp["ln1_g"]).reshape(DEPTH, 1, D), "ln1b": f(inp["ln1_b"]).reshape(DEPTH, 1, D),
        "wr": np.ascontiguousarray(wr), "br": np.ascontiguousarray(br),
        "w_eg": f(inp["w_eg"]), "w_eu": f(inp["w_eu"]), "w_ed": f(inp["w_ed"]),
        "ln2g": f(inp["ln2_g"]).reshape(DEPTH, 1, D), "ln2b": f(inp["ln2_b"]).reshape(DEPTH, 1, D),
    }
    return shared


_NC_CACHE = {}


def kernel(**inputs):
    x = np.ascontiguousarray(np.asarray(inputs["x"]), dtype=np.float32)
    shared = host_inputs(inputs)
    if "nc" not in _NC_CACHE:
        _NC_CACHE["nc"] = build()
    nc = _NC_CACHE["nc"]
    in_maps = []
    for c in range(8):
        m = dict(shared)
        m["x"] = x[c]
        in_maps.append(m)
    res = run_bass_kernel_spmd(nc, in_maps, core_ids=list(range(8)))
    return np.stack([np.asarray(r["out"], dtype=np.float32) for r in res.results], axis=0)
```

```python
import numpy as np
import concourse.bass as bass
import concourse.mybir as mybir
from concourse.bass_utils import run_bass_kernel_spmd

F32 = mybir.dt.float32
BF16 = mybir.dt.bfloat16
I32 = mybir.dt.int32
U32 = mybir.dt.uint32
ALU = mybir.AluOpType
AF = mybir.ActivationFunctionType
AX = mybir.AxisListType

ENGS = ("pe", "act", "dve", "pool", "sp")


class Buf:
    __slots__ = ("name", "last_w", "readers")

    def __init__(self, name=""):
        self.name = name
        self.last_w = None
        self.readers = []


class Op:
    __slots__ = ("eng", "fn", "deps", "is_dma", "sem", "count", "has_dep", "prev_dma", "idx")

    def __init__(self, eng, fn, is_dma):
        self.eng = eng
        self.fn = fn
        self.deps = []
        self.is_dma = is_dma
        self.sem = None
        self.count = 0
        self.has_dep = False
        self.prev_dma = None


class Sched:
    NS = 4
    ND = 8

    def __init__(self, nc, stack):
        self.nc = nc
        self.esem = {e: [stack.enter_context(nc.semaphore(f"s_{e}{i}")) for i in range(self.NS)] for e in ENGS}
        self.ecnt = {e: [0] * self.NS for e in ENGS}
        self.ek = {e: 0 for e in ENGS}
        self.dsem = {e: [stack.enter_context(nc.semaphore(f"d_{e}{i}")) for i in range(self.ND)] for e in ("sp", "pool", "act")}
        self.dcnt = {e: [0] * self.ND for e in self.dsem}
        self.dk = {e: 0 for e in self.dsem}
        self.dlast = {e: [None] * self.ND for e in self.dsem}
        self.waited = {e: {} for e in ENGS}


class Phase:
    def __init__(self, sched, name):
        self.s = sched
        self.nc = sched.nc
        self.name = name
        self.ops = []
        self.bufs = []

    def buf(self, name=""):
        b = Buf(name)
        self.bufs.append(b)
        return b

    def _add(self, op, reads, writes):
        deps = []
        for b in reads:
            if b.last_w is not None:
                deps.append(b.last_w)
        for b in writes:
            if b.last_w is not None:
                deps.append(b.last_w)
            deps.extend(b.readers)
        for b in reads:
            b.readers.append(op)
        for b in writes:
            b.last_w = op
            b.readers = []
        seen = set()
        for d in deps:
            if d is op or id(d) in seen:
                continue
            seen.add(id(d))
            if d.eng == "pe" and op.eng == "pe" and not d.is_dma and not op.is_dma:
                continue
            op.deps.append(d)
            d.has_dep = True
        self.ops.append(op)
        return op

    def op(self, eng, fn, reads=(), writes=()):
        return self._add(Op(eng, fn, False), reads, writes)

    def dma(self, q, out, in_, reads=(), writes=(), **kw):
        def fn(e):
            return e.dma_start(out=out, in_=in_, **kw)
        return self._add(Op(q, fn, True), reads, writes)

    def dma_fn(self, q, fn, reads=(), writes=()):
        return self._add(Op(q, fn, True), reads, writes)

    def emit(self):
        s = self.s
        nc = self.nc
        for op in self.ops:
            e = op.eng
            if op.is_dma:
                k = s.dk[e] % s.ND
                s.dk[e] += 1
                op.prev_dma = s.dlast[e][k]
                s.dcnt[e][k] += 16
                op.sem = s.dsem[e][k]
                op.count = s.dcnt[e][k]
                s.dlast[e][k] = (op.sem, op.count)
            elif op.has_dep:
                k = s.ek[e] % s.NS
                s.ek[e] += 1
                s.ecnt[e][k] += 1
                op.sem = s.esem[e][k]
                op.count = s.ecnt[e][k]
        per = {e: [o for o in self.ops if o.eng == e] for e in ENGS}

        def run(e, eng):
            waited = s.waited[e]
            for op in per[e]:
                need = {}
                for d in op.deps:
                    key = id(d.sem)
                    if need.get(key, (None, 0))[1] < d.count:
                        need[key] = (d.sem, d.count)
                if op.prev_dma is not None:
                    sem, cnt = op.prev_dma
                    key = id(sem)
                    if need.get(key, (None, 0))[1] < cnt:
                        need[key] = (sem, cnt)
                for key, (sem, cnt) in need.items():
                    if waited.get(key, 0) < cnt:
                        eng.wait_ge(sem, cnt)
                        waited[key] = cnt
                ins = op.fn(eng)
                if op.sem is not None:
                    ins.then_inc(op.sem, 16 if op.is_dma else 1)
            if e in s.dsem:
                for k in range(s.ND):
                    if s.dlast[e][k] is not None:
                        sem, cnt = s.dlast[e][k]
                        if waited.get(id(sem), 0) < cnt:
                            eng.wait_ge(sem, cnt)
                            waited[id(sem)] = cnt

        with nc.Block() as block:
            @block.sync
            def _(eng):
                run("sp", eng)

            @block.tensor
            def _(eng):
                run("pe", eng)

            @block.scalar
            def _(eng):
                run("act", eng)

            @block.vector
            def _(eng):
                run("dve", eng)

            @block.gpsimd
            def _(eng):
                run("pool", eng)
        self.ops = []


T = 8192
D = 1024
NT = T // 128
PADR = 1024
ROWS = PADR + T + PADR
DEPTH = 2
ALPHA = (2 * DEPTH) ** 0.25
LN_EPS = 1e-5
CAP = 768
NSLOT = 32 * CAP
NEGB = -30000.0
B_CFG = ((128, 1), (512, 4), (2048, 16))
QK_AQ, QK_AK = 0, 512
QK_BQ = (640, 1152, 1664)
QK_BK = (896, 1408, 1920)
QK_CQ, QK_CK = 2176, 2688
QK_W = 3200
SEGS = [
    (0, 512, "qk", QK_AQ), (512, 128, "qk", QK_AK), (640, 128, "va", 0),
    (768, 512, "qk", QK_BQ[0]), (1280, 256, "vb", 0),
    (1536, 512, "qk", QK_BQ[1]), (2048, 256, "vb", 1),
    (2304, 512, "qk", QK_BQ[2]), (2816, 256, "vb", 2),
    (3072, 512, "qk", QK_CQ), (3584, 512, "qk", QK_CK), (4096, 512, "vc", 0),
]
GATE0 = 4608


def _t5_bucket(rel):
    half, max_exact = 16, 8
    ret = np.where(rel > 0, half, 0)
    n = np.abs(rel)
    large = max_exact + (np.log(np.maximum(n, max_exact) / max_exact) / np.log(2048 / max_exact) * (half - max_exact)).astype(np.int32)
    large = np.minimum(large, half - 1)
    return (ret + np.where(n < max_exact, n, large)).astype(np.int32)


def host_bias_tables(rel_bias, rpb_c):
    kk = np.arange(128)[:, None]
    qq = np.arange(128)[None, :]
    ba = np.full((128, 2, 3, 4, 128), NEGB, np.float32)
    for c in range(3):
        off = (c - 1) * 128 + kk - qq
        band = np.abs(off) <= 128
        bk = _t5_bucket(off)
        for h in range(8):
            ba[:, h // 4, c, h % 4, :] = np.where(band, rel_bias[bk, h], NEGB)
    bb = np.full((3, 128, 2, 4, 128), NEGB, np.float32)
    for g, (_, dil) in enumerate(B_CFG):
        for jp in range(2):
            off = jp * 128 + kk - 64 - qq
            band = np.abs(off) <= 64
            bk = _t5_bucket(off * dil)
            for h in range(4):
                bb[g, :, jp, h, :] = np.where(band, rel_bias[bk, 8 + 4 * g + h], NEGB)
    bc = np.full((rpb_c.shape[0], 5, 128, 5, 8, 128), NEGB, np.float32)
    for pi, j in enumerate((0, 1, 2, 62, 63)):
        cb0 = min(max(j - 2, 0), 59)
        qtok = j * 128 + np.arange(128)
        qi, qc = qtok // 64, qtok % 64
        rstart = np.clip(qi - 4, 0, 120)
        qstart = np.clip(qc - 8, 0, 48)
        for c in range(5):
            ktok = (cb0 + c) * 128 + np.arange(128)
            kr, kc = ktok // 64, ktok % 64
            valid = ((kr[:, None] >= rstart[None, :]) & (kr[:, None] < rstart[None, :] + 8)
                     & (kc[:, None] >= qstart[None, :]) & (kc[:, None] < qstart[None, :] + 16))
            ridx = np.clip(kr[:, None] - qi[None, :] + 7, 0, 14)
            cidx = np.clip(kc[:, None] - qc[None, :] + 15, 0, 30)
            for l in range(rpb_c.shape[0]):
                for h in range(8):
                    bc[l, pi, :, c, h, :] = np.where(valid, rpb_c[l, h][ridx, cidx], NEGB)
    return ba, bb, bc


def c_pattern(j):
    return {0: 0, 1: 1, 62: 3, 63: 4}.get(j, 2)


def o_mm(ph, out, lhsT, rhs, start, stop, reads, writes):
    return ph.op("pe", lambda e: e.matmul(out, lhsT=lhsT, rhs=rhs, start=start, stop=stop), reads, writes)


def o_tp(ph, out, in_, ident, reads, writes):
    return ph.op("pe", lambda e: e.transpose(out=out, in_=in_, identity=ident), reads, writes)


def o_act(ph, out, in_, func, reads, writes, scale=None, bias=None):
    kw = {}
    if scale is not None:
        kw["scale"] = scale
    if bias is not None:
        kw["bias"] = bias
    return ph.op("act", lambda e: e.activation(out=out, in_=in_, func=func, **kw), reads, writes)


def o_cp(ph, eng, out, in_, reads, writes):
    if eng == "act":
        return ph.op("act", lambda e: e.copy(out=out, in_=in_), reads, writes)
    return ph.op(eng, lambda e: e.tensor_copy(out=out, in_=in_), reads, writes)


def o_tt(ph, eng, out, in0, in1, op, reads, writes):
    return ph.op(eng, lambda e: e.tensor_tensor(out=out, in0=in0, in1=in1, op=op), reads, writes)


def o_ts(ph, eng, out, in0, s1, s2, op0, op1, reads, writes):
    if s2 is None:
        return ph.op(eng, lambda e: e.tensor_scalar(out=out, in0=in0, scalar1=s1, scalar2=None, op0=op0), reads, writes)
    return ph.op(eng, lambda e: e.tensor_scalar(out=out, in0=in0, scalar1=s1, scalar2=s2, op0=op0, op1=op1), reads, writes)


def o_stt(ph, out, in0, scalar, in1, op0, op1, reads, writes):
    return ph.op("dve", lambda e: e.scalar_tensor_tensor(out=out, in0=in0, scalar=scalar, in1=in1, op0=op0, op1=op1), reads, writes)


def o_red(ph, out, in_, op, reads, writes):
    return ph.op("dve", lambda e: e.tensor_reduce(out=out, in_=in_, axis=AX.X, op=op), reads, writes)


def o_rcp(ph, out, in_, reads, writes):
    return ph.op("dve", lambda e: e.reciprocal(out=out, in_=in_), reads, writes)


def o_memset(ph, eng, ap, val, writes):
    return ph.op(eng, lambda e: e.memset(ap, val), (), writes)


class Ring:
    def __init__(self, ph, tiles, name):
        self.tiles = tiles
        self.bufs = [ph.buf(f"{name}{i}") for i in range(len(tiles))]
        self.k = 0

    def next(self):
        i = self.k % len(self.tiles)
        self.k += 1
        return self.tiles[i], self.bufs[i]


class Ctx:
    pass


def ln_s1(ph, C, z, zb):
    st, stb = C.ln_st.next()
    mv, mvb = C.ln_mv.next()
    ph.op("dve", lambda e: e.bn_stats(out=st[:, 0, :], in_=z[:, 0:512]), [zb], [stb])
    ph.op("dve", lambda e: e.bn_stats(out=st[:, 1, :], in_=z[:, 512:1024]), [zb, stb], [stb])
    ph.op("dve", lambda e: e.bn_aggr(out=mv[:, 0:2], in_=st[:].rearrange("p a s -> p (a s)")), [stb], [mvb])
    return mv, mvb


def ln_s2(ph, C, mv, mvb):
    o_act(ph, mv[:, 2:3], mv[:, 1:2], AF.Sqrt, [mvb], [mvb], bias=C.eps[:, 0:1], scale=1.0)
    o_rcp(ph, mv[:, 3:4], mv[:, 2:3], [mvb], [mvb])
    o_ts(ph, "dve", mv[:, 4:5], mv[:, 0:1], mv[:, 3:4], -1.0, ALU.mult, ALU.mult, [mvb], [mvb])


def ln_s3(ph, C, z, zb, mv, mvb, out, outb, g, b, cb, geng="pool", beng="pool"):
    o_act(ph, out, z, AF.Identity, [zb, mvb], [outb], scale=mv[:, 3:4], bias=mv[:, 4:5])
    o_tt(ph, geng, out, out, g, ALU.mult, [outb, cb], [outb])
    o_tt(ph, beng, out, out, b, ALU.add, [outb, cb], [outb])


def ln_tile(ph, C, z, zb, out, outb, g, b, cb, geng="pool", beng="pool"):
    mv, mvb = ln_s1(ph, C, z, zb)
    ln_s2(ph, C, mv, mvb)
    ln_s3(ph, C, z, zb, mv, mvb, out, outb, g, b, cb, geng, beng)


def ln_setup(ph, C, A):
    C.ln_st = Ring(ph, [A(f"lnst{i}", [128, 2, 6], F32) for i in range(6)], "lnst")
    C.ln_mv = Ring(ph, [A(f"lnmv{i}", [128, 8], F32) for i in range(6)], "lnmv")


def load_gb(ph, A, gsrc, bsrc, name):
    g = A(name + "g", [128, D], F32)
    b = A(name + "b", [128, D], F32)
    cb = ph.buf(name)
    ph.dma("sp", g[:], gsrc.to_broadcast([128, D]), writes=[cb])
    ph.dma("sp", b[:], bsrc.to_broadcast([128, D]), writes=[cb])
    return g, b, cb


def mk_alloc(nc, st, prefix):
    def A(name, shape, dt):
        return st.enter_context(nc.sbuf_tensor(prefix + name, list(shape), dt))

    def P(name, shape, dt):
        return st.enter_context(nc.psum_tensor(prefix + name, list(shape), dt))
    return A, P


def phase_ln0(C):
    from contextlib import ExitStack
    nc = C.nc
    with ExitStack() as st:
        A, P = mk_alloc(nc, st, "p0_")
        ph = Phase(C.S, "ln0")
        ln_setup(ph, C, A)
        g, b, cb = load_gb(ph, A, C.ln0g, C.ln0b, "ln0")
        zr = Ring(ph, [A(f"z{i}", [128, D], F32) for i in range(3)], "z")
        orr = Ring(ph, [A(f"o{i}", [128, D], F32) for i in range(3)], "o")
        nxt = zr.next()
        ph.dma("sp", nxt[0][:], C.x[0:128, :], writes=[nxt[1]])
        for i in range(NT):
            z, zb = nxt
            if i + 1 < NT:
                nxt = zr.next()
                ph.dma("sp", nxt[0][:], C.x[(i + 1) * 128:(i + 2) * 128, :], writes=[nxt[1]])
            o, ob = orr.next()
            ln_tile(ph, C, z[:], zb, o[:], ob, g[:], b[:], cb, geng="dve", beng="pool")
            ph.dma("sp", C.H[i * 128:(i + 1) * 128, :], o[:], reads=[ob])
        ph.emit()


def phase_proj(C, l):
    from contextlib import ExitStack
    nc = C.nc
    with ExitStack() as st:
        A, P = mk_alloc(nc, st, f"p1_{l}_")
        ph = Phase(C.S, f"proj{l}")
        w = A("w", [128, 8, 7680], BF16)
        wb = ph.buf("w")
        wstr = Ring(ph, [A(f"wst{i}", [128, 1536], F32) for i in range(2)], "wst")
        wbs = []
        for k in range(8):
            for cbk in range(5):
                src = C.w_in[l, k * 128:(k + 1) * 128, cbk * 1536:(cbk + 1) * 1536]
                wbc = ph.buf(f"w{k}_{cbk}")
                wbs.append(wbc)
                if (k * 5 + cbk) % 3 == 2:
                    wst, wstb = wstr.next()
                    ph.dma("sp", wst[:], src, writes=[wstb])
                    o_cp(ph, "pool", w[:, k, cbk * 1536:(cbk + 1) * 1536], wst[:], [wstb], [wbc])
                else:
                    ph.dma("pool", w[:, k, cbk * 1536:(cbk + 1) * 1536], src, writes=[wbc])
        bg = A("bg", [128, 24], F32)
        ph.dma("sp", bg[:], C.bgT[l], writes=[wb])
        hin = Ring(ph, [A(f"hin{i}", [128, D], F32) for i in range(2)], "hin")
        hT_t = [A(f"hT{i}", [128, 8, 512], BF16) for i in range(2)]
        hT_b = [[ph.buf(f"hT{i}_{s_}") for s_ in range(4)] for i in range(2)]
        qks = Ring(ph, [A(f"qks{i}", [128, QK_W], BF16) for i in range(2)], "qks")
        vas_t = [A(f"vas{i}", [128, 2, 65], BF16) for i in range(2)]
        vbs_t = [A(f"vbs{i}", [128, 3, 4, 65], BF16) for i in range(2)]
        vcs_t = [A(f"vcs{i}", [128, 8, 65], BF16) for i in range(2)]
        vas = Ring(ph, vas_t, "vas")
        vbs = Ring(ph, vbs_t, "vbs")
        vcs = Ring(ph, vcs_t, "vcs")
        for i in range(2):
            o_memset(ph, "pool", vas_t[i][:], 1.0, [vas.bufs[i]])
            o_memset(ph, "pool", vbs_t[i][:], 1.0, [vbs.bufs[i]])
            o_memset(ph, "pool", vcs_t[i][:], 1.0, [vcs.bufs[i]])
        gst = Ring(ph, [A(f"gst{i}", [128, 6, 512], BF16) for i in range(2)], "gst")
        tpr = Ring(ph, [P(f"tp{i}", [128, 8, 128], F32) for i in range(2)], "tp")
        mmr = Ring(ph, [P(f"mm{i}", [128, 512], F32) for i in range(4)], "mm")
        GTv = C.GT
        ev = 0
        nxt = hin.next()
        ph.dma("sp", nxt[0][:], C.H[0:128, :], writes=[nxt[1]])
        for n in range(T // 512):
            ht, htbs = hT_t[n % 2], hT_b[n % 2]
            for sub in range(4):
                htb = htbs[sub]
                i = 4 * n + sub
                h_in, hib = nxt
                if i + 1 < NT:
                    nxt = hin.next()
                    ph.dma("sp", nxt[0][:], C.H[(i + 1) * 128:(i + 2) * 128, :], writes=[nxt[1]])
                tp, tpb = tpr.next()
                for k in range(8):
                    o_tp(ph, tp[:, k, :], h_in[:, k * 128:(k + 1) * 128], C.identf[:], [hib], [tpb])
                o_cp(ph, "dve", ht[:, :, sub * 128:(sub + 1) * 128], tp[:], [tpb], [htb])
                qk, qkb = qks.next()
                va, vab = vas.next()
                vb, vbb = vbs.next()
                vc, vcb = vcs.next()
                for (c0, wd, kind, dst) in SEGS:
                    mm, mmb = mmr.next()
                    for k in range(8):
                        o_mm(ph, mm[:, 0:wd], ht[:, k, sub * 128:(sub + 1) * 128], w[:, k, c0:c0 + wd],
                             k == 0, k == 7, [htb, wb] + wbs, [mmb])
                    eng = "dve" if ev % 2 == 0 else "act"
                    ev += 1
                    if kind == "qk":
                        o_cp(ph, eng, qk[:, dst:dst + wd], mm[:, 0:wd], [mmb], [qkb])
                    elif kind == "va":
                        o_cp(ph, eng, va[:, :, 0:64], mm[:, 0:128].rearrange("p (h d) -> p h d", h=2), [mmb], [vab])
                    elif kind == "vb":
                        o_cp(ph, eng, vb[:, dst, :, 0:64], mm[:, 0:256].rearrange("p (h d) -> p h d", h=4), [mmb], [vbb])
                    else:
                        o_cp(ph, eng, vc[:, :, 0:64], mm[:, 0:512].rearrange("p (h d) -> p h d", h=8), [mmb], [vcb])
                r0 = PADR + i * 128
                ph.dma("sp", C.QK[r0:r0 + 128, :], qk[:], reads=[qkb])
                ph.dma("sp", C.VA[r0:r0 + 128, :], va[:].rearrange("p h d -> p (h d)"), reads=[vab])
                for gi in range(3):
                    ph.dma("sp", C.VB[gi][r0:r0 + 128, :], vb[:, gi, :, :].rearrange("p h d -> p (h d)"), reads=[vbb])
                ph.dma("sp", C.VC[r0:r0 + 128, :], vc[:].rearrange("p h d -> p (h d)"), reads=[vcb])
            for cg in range(4):
                gs, gsb = gst.next()
                for a in range(6):
                    c = cg * 6 + a
                    mm, mmb = mmr.next()
                    for k in range(8):
                        o_mm(ph, mm[:], w[:, k, GATE0 + c * 128:GATE0 + (c + 1) * 128], ht[:, k, :],
                             k == 0, k == 7, htbs + [wb] + wbs, [mmb])
                    o_act(ph, gs[:, a, :], mm[:], AF.Sigmoid, [mmb, wb], [gsb], bias=bg[:, c:c + 1], scale=1.0)
                ph.dma("sp", GTv[cg * 768:(cg + 1) * 768, n * 512:(n + 1) * 512].rearrange("(a p) t -> p a t", p=128),
                       gs[:], reads=[gsb])
        ph.emit()


def phase_attn_a(C, l):
    from contextlib import ExitStack
    nc = C.nc
    with ExitStack() as st:
        A, P = mk_alloc(nc, st, f"pa_{l}_")
        ph = Phase(C.S, f"attnA{l}")
        es = A("es", [128, 8], F32)
        esb = ph.buf("es")
        ph.dma("sp", es[:], C.sink[l].to_broadcast([128, 8]), writes=[esb])
        o_act(ph, es[:], es[:], AF.Exp, [esb], [esb])
        qr = Ring(ph, [A(f"q{i}", [128, 512], BF16) for i in range(2)], "q")
        kdr = Ring(ph, [A(f"kd{i}", [128, 3, 2, 2, 64], BF16) for i in range(2)], "kd")
        vr = Ring(ph, [A(f"v{i}", [128, 3, 130], BF16) for i in range(2)], "v")
        qTr = Ring(ph, [A(f"qT{i}", [128, 4, 128], BF16) for i in range(2)], "qT")
        kl_t = [A(f"kTl{i}", [128, 6, 128], BF16) for i in range(2)]
        kh_t = [A(f"kTh{i}", [128, 6, 128], BF16) for i in range(2)]
        klr = Ring(ph, kl_t, "kTl")
        khr = Ring(ph, kh_t, "kTh")
        for i in range(2):
            o_memset(ph, "pool", kl_t[i][:], 0.0, [klr.bufs[i]])
            o_memset(ph, "pool", kh_t[i][:], 0.0, [khr.bufs[i]])
        ptr = Ring(ph, [A(f"pt{i}", [128, 1536], BF16) for i in range(2)], "pt")
        oar = Ring(ph, [A(f"oa{i}", [128, 512], BF16) for i in range(2)], "oa")
        dnr = Ring(ph, [A(f"dn{i}", [128, 8], F32) for i in range(2)], "dn")
        otr = Ring(ph, [A(f"ot{i}", [128, 4, 512], BF16) for i in range(2)], "ot")
        tpq = Ring(ph, [P("tpq", [128, 1024], BF16)], "tpq")
        tpk = Ring(ph, [P("tpk", [128, 1024], BF16)], "tpk")
        psr = Ring(ph, [P("ps", [128, 1536], F32)], "ps")
        por = Ring(ph, [P(f"po{i}", [128, 512], F32) for i in range(2)], "po")
        OTv = C.OT[0:512, :].rearrange("(i p) t -> p i t", p=128)

        def loads(b):
            q, qb = qr.next()
            kd, kdb = kdr.next()
            v, vb = vr.next()
            r0 = PADR + 128 * b
            ph.dma("sp", q[:], C.QK[r0:r0 + 128, QK_AQ:QK_AQ + 512], writes=[qb])
            for g in range(2):
                ksrc = C.QK[r0 - 128:r0 + 256, QK_AK + 64 * g:QK_AK + 64 * g + 64].rearrange("(c p) d -> p c d", p=128)
                for a in range(2):
                    ph.dma("sp", kd[:, :, g, a, :], ksrc, writes=[kdb])
            ph.dma("sp", v[:], C.VA[r0 - 128:r0 + 256, :].rearrange("(c p) d -> p c d", p=128), writes=[vb])
            return (q, qb, kd, kdb, v, vb)

        nxt = loads(0)
        ot, otb = None, None
        for b in range(NT):
            q, qb, kd, kdb, v, vb = nxt
            if b + 1 < NT:
                nxt = loads(b + 1)
            tq, tqb = tpq.next()
            for i in range(4):
                o_tp(ph, tq[:, i * 128:(i + 1) * 128], q[:, i * 128:(i + 1) * 128], C.identb[:], [qb], [tqb])
            qT, qTb = qTr.next()
            o_cp(ph, "dve", qT[:].rearrange("p i t -> p (i t)"), tq[:, 0:512], [tqb], [qTb])
            tk, tkb = tpk.next()
            for c in range(3):
                for g in range(2):
                    o_tp(ph, tk[:, (c * 2 + g) * 128:(c * 2 + g + 1) * 128],
                         kd[:, c, g, :, :].rearrange("p a d -> p (a d)"), C.identb[:], [kdb], [tkb])
            kl, klb = klr.next()
            kh, khb = khr.next()
            o_cp(ph, "act", kl[0:64, :, :].rearrange("p s t -> p (s t)"), tk[0:64, 0:768], [tkb], [klb])
            o_cp(ph, "dve", kh[64:128, :, :].rearrange("p s t -> p (s t)"), tk[64:128, 0:768], [tkb], [khb])
            oa, oab = oar.next()
            dn, dnb = dnr.next()
            for g in range(2):
                ps, psb = psr.next()
                for c in range(3):
                    for j in range(4):
                        h = 4 * g + j
                        i, par = h // 2, h % 2
                        kk_, kkb = (kl, klb) if par == 0 else (kh, khb)
                        o_mm(ph, ps[:, (c * 4 + j) * 128:(c * 4 + j + 1) * 128], kk_[:, c * 2 + g, :],
                             qT[:, i, :], True, True, [kkb, qTb], [psb])
                pt, ptb = ptr.next()
                for c in range(3):
                    o_act(ph, pt[:, c * 512:(c + 1) * 512], ps[:, c * 512:(c + 1) * 512], AF.Exp, [psb], [ptb], scale=0.125)
                o_tt(ph, "dve", pt[:], pt[:], C.EA[:, g * 1536:(g + 1) * 1536], ALU.mult, [ptb], [ptb])
                po, pob = por.next()
                for j in range(4):
                    for c in range(3):
                        o_mm(ph, po[:, j * 65:(j + 1) * 65], pt[:, (c * 4 + j) * 128:(c * 4 + j + 1) * 128],
                             v[:, c, g * 65:(g + 1) * 65], c == 0, c == 2, [ptb, vb], [pob])
                pov = po[:, 0:260].rearrange("p (j d) -> p j d", j=4)
                o_tt(ph, "dve", dn[:, g * 4:(g + 1) * 4], pov[:, :, 64], es[:, g * 4:(g + 1) * 4], ALU.add, [pob, esb], [dnb])
                o_rcp(ph, dn[:, g * 4:(g + 1) * 4], dn[:, g * 4:(g + 1) * 4], [dnb], [dnb])
                o_tt(ph, "dve", oa[:, g * 256:(g + 1) * 256].rearrange("p (j d) -> p j d", j=4), pov[:, :, 0:64],
                     dn[:, g * 4:(g + 1) * 4].unsqueeze(2).to_broadcast([128, 4, 64]), ALU.mult, [pob, dnb], [oab])
            to, tob = tpq.next()
            for i in range(4):
                o_tp(ph, to[:, 512 + i * 128:512 + (i + 1) * 128], oa[:, i * 128:(i + 1) * 128], C.identb[:], [oab], [tob])
            if b % 4 == 0:
                ot, otb = otr.next()
            o_cp(ph, "act", ot[:, :, (b % 4) * 128:(b % 4 + 1) * 128], to[:, 512:1024].rearrange("p (i t) -> p i t", i=4), [tob], [otb])
            if b % 4 == 3:
                ph.dma("sp", OTv[:, :, (b // 4) * 512:(b // 4 + 1) * 512], ot[:], reads=[otb])
        ph.emit()


def phase_attn_b(C, l, gi):
    from contextlib import ExitStack
    nc = C.nc
    dil = B_CFG[gi][1]
    Ls = T // dil
    nb = Ls // 128
    NBK = 4
    with ExitStack() as st:
        A, P = mk_alloc(nc, st, f"pb_{l}_{gi}_")
        q_t = [A(f"q{i}", [128, 256], BF16) for i in range(NBK)]
        k_t = [A(f"k{i}", [128, 2, 256], BF16) for i in range(NBK)]
        v_t = [A(f"v{i}", [128, 2, 260], BF16) for i in range(NBK)]
        qT_t = [A(f"qT{i}", [128, 2, 128], BF16) for i in range(2)]
        kl_t = [A(f"kTl{i}", [128, 4, 128], BF16) for i in range(2)]
        kh_t = [A(f"kTh{i}", [128, 4, 128], BF16) for i in range(2)]
        pt_t = [A(f"pt{i}", [128, 1024], BF16) for i in range(NBK)]
        sb_t = [A(f"sb{i}", [128, 1024], F32) for i in range(2)]
        nb_t = [A(f"nb{i}", [128, 260], F32) for i in range(NBK)]
        tp_t = [P(f"tp{i}", [128, 1024], BF16) for i in range(2)]
        ps_t = [P("ps", [128, 1024], F32)]
        po_t = [P(f"po{i}", [128, 512], F32) for i in range(NBK)]
        QKv = C.QK.rearrange("(s d) c -> d s c", d=dil)
        VBv = C.VB[gi].rearrange("(s d) c -> d s c", d=dil)
        NBv = C.NB[gi].rearrange("(s d) c -> d s c", d=dil)
        sp0 = PADR // dil
        ph = Phase(C.S, f"attnB{l}_{gi}_init")
        zb = ph.buf("z")
        for i in range(2):
            o_memset(ph, "pool", kl_t[i][:], 0.0, [zb])
            o_memset(ph, "pool", kh_t[i][:], 0.0, [zb])
        ph.emit()
        blocks = [(r, b) for r in range(dil) for b in range(nb)]
        for g0 in range(0, len(blocks), NBK):
            grp = blocks[g0:g0 + NBK]
            ph = Phase(C.S, f"attnB{l}_{gi}_{g0}")
            tpr = Ring(ph, tp_t, "tp")
            psr = Ring(ph, ps_t, "ps")
            qTr = Ring(ph, qT_t, "qT")
            klr = Ring(ph, kl_t, "kl")
            khr = Ring(ph, kh_t, "kh")
            sbr = Ring(ph, sb_t, "sb")
            ld = []
            for i, (r, b) in enumerate(grp):
                qb, kb, vb = ph.buf(), ph.buf(), ph.buf()
                s0 = sp0 + 128 * b
                ph.dma("sp", q_t[i][:], QKv[r, s0:s0 + 128, QK_BQ[gi]:QK_BQ[gi] + 256], writes=[qb])
                ph.dma("sp", k_t[i][:], QKv[r, s0 - 64:s0 + 192, QK_BK[gi]:QK_BK[gi] + 256].rearrange("(j p) d -> p j d", p=128), writes=[kb])
                ph.dma("sp", v_t[i][:], VBv[r, s0 - 64:s0 + 192, :].rearrange("(j p) d -> p j d", p=128), writes=[vb])
                ld.append((qb, kb, vb))
            ptbs = []
            for i, (r, b) in enumerate(grp):
                q, k = q_t[i], k_t[i]
                qb, kb, vb = ld[i]
                tp, tpb = tpr.next()
                for a in range(2):
                    o_tp(ph, tp[:, a * 128:(a + 1) * 128], q[:, a * 128:(a + 1) * 128], C.identb[:], [qb], [tpb])
                for jp in range(2):
                    for a in range(2):
                        sl = 2 + 2 * jp + a
                        o_tp(ph, tp[:, sl * 128:(sl + 1) * 128], k[:, jp, a * 128:(a + 1) * 128], C.identb[:], [kb], [tpb])
                qT, qTb = qTr.next()
                kl, klb = klr.next()
                kh, khb = khr.next()
                o_cp(ph, "dve", qT[:].rearrange("p s t -> p (s t)"), tp[:, 0:256], [tpb], [qTb])
                o_cp(ph, "act", kl[0:64, :, :].rearrange("p s t -> p (s t)"), tp[0:64, 256:768], [tpb], [klb])
                o_cp(ph, "dve", kh[64:128, :, :].rearrange("p s t -> p (s t)"), tp[64:128, 256:768], [tpb], [khb])
                ps, psb = psr.next()
                for jp in range(2):
                    for h in range(4):
                        a, par = h // 2, h % 2
                        kk_, kkb = (kl, klb) if par == 0 else (kh, khb)
                        o_mm(ph, ps[:, (jp * 4 + h) * 128:(jp * 4 + h + 1) * 128], kk_[:, 2 * jp + a, :],
                             qT[:, a, :], True, True, [kkb, qTb], [psb])
                sb_, sbb = sbr.next()
                for jp in range(2):
                    o_stt(ph, sb_[:, jp * 512:(jp + 1) * 512], ps[:, jp * 512:(jp + 1) * 512], 0.125,
                          C.BB[:, gi * 1024 + jp * 512:gi * 1024 + (jp + 1) * 512], ALU.mult, ALU.add, [psb], [sbb])
                ptb = ph.buf()
                o_act(ph, pt_t[i][:], sb_[:], AF.Exp, [sbb], [ptb])
                ptbs.append(ptb)
            pobs = []
            for i, (r, b) in enumerate(grp):
                pt, v, po = pt_t[i], v_t[i], po_t[i]
                pob = ph.buf()
                for h in range(4):
                    for jp in range(2):
                        o_mm(ph, po[:, h * 65:(h + 1) * 65], pt[:, (jp * 4 + h) * 128:(jp * 4 + h + 1) * 128],
                             v[:, jp, h * 65:(h + 1) * 65], jp == 0, jp == 1, [ptbs[i], ld[i][2]], [pob])
                pobs.append(pob)
            for i, (r, b) in enumerate(grp):
                nbb = ph.buf()
                o_cp(ph, "act" if i % 2 == 0 else "dve", nb_t[i][:], po_t[i][:, 0:260], [pobs[i]], [nbb])
                ph.dma("sp", NBv[r, 128 * b:128 * (b + 1), :], nb_t[i][:], reads=[nbb])
            ph.emit()


def phase_attn_bc(C, l):
    from contextlib import ExitStack
    nc = C.nc
    with ExitStack() as st:
        A, P = mk_alloc(nc, st, f"pbc_{l}_")
        ph = Phase(C.S, f"attnBc{l}")
        nr = [Ring(ph, [A(f"n{g}_{i}", [128, 260], F32) for i in range(2)], f"n{g}") for g in range(3)]
        dnr = Ring(ph, [A(f"dn{i}", [128, 4], F32) for i in range(2)], "dn")
        obr = Ring(ph, [A(f"ob{i}", [128, 256], BF16) for i in range(2)], "ob")
        otr = Ring(ph, [A(f"ot{i}", [128, 2, 512], BF16) for i in range(2)], "ot")
        tpr = Ring(ph, [P(f"tp{i}", [128, 1024], BF16) for i in range(2)], "tp")
        OTv = C.OT[512:768, :].rearrange("(i p) t -> p i t", p=128)

        def loads(i):
            res = []
            for g in range(3):
                n, nb_ = nr[g].next()
                ph.dma("sp", n[:], C.NB[g][i * 128:(i + 1) * 128, :], writes=[nb_])
                res.append((n, nb_))
            return res

        nxt = loads(0)
        ot, otb = None, None
        for i in range(NT):
            (n0, b0), (n1, b1), (n2, b2) = nxt
            if i + 1 < NT:
                nxt = loads(i + 1)
            o_tt(ph, "pool", n0[:], n0[:], n1[:], ALU.add, [b0, b1], [b0])
            o_tt(ph, "pool", n0[:], n0[:], n2[:], ALU.add, [b0, b2], [b0])
            nv = n0[:].rearrange("p (h d) -> p h d", h=4)
            dn, dnb = dnr.next()
            o_rcp(ph, dn[:], nv[:, :, 64], [b0], [dnb])
            ob, obb = obr.next()
            o_tt(ph, "dve", ob[:].rearrange("p (h d) -> p h d", h=4), nv[:, :, 0:64],
                 dn[:].unsqueeze(2).to_broadcast([128, 4, 64]), ALU.mult, [b0, dnb], [obb])
            tp, tpb = tpr.next()
            for a in range(2):
                o_tp(ph, tp[:, a * 128:(a + 1) * 128], ob[:, a * 128:(a + 1) * 128], C.identb[:], [obb], [tpb])
            if i % 4 == 0:
                ot, otb = otr.next()
            o_cp(ph, "act", ot[:, :, (i % 4) * 128:(i % 4 + 1) * 128], tp[:, 0:256].rearrange("p (a t) -> p a t", a=2), [tpb], [otb])
            if i % 4 == 3:
                ph.dma("sp", OTv[:, :, (i // 4) * 512:(i // 4 + 1) * 512], ot[:], reads=[otb])
        ph.emit()


def phase_attn_c(C, l):
    from contextlib import ExitStack
    nc = C.nc
    NTL = 2
    with ExitStack() as st:
        A, P = mk_alloc(nc, st, f"pc_{l}_")
        EC = A("EC", [128, 5, 5120], BF16)
        tmp = A("ect", [128, 5120], F32)
        q_t = [A(f"q{i}", [128, 512], BF16) for i in range(NTL)]
        k_t = [A(f"k{i}", [128, 5, 512], BF16) for i in range(NTL)]
        v_t = [A(f"v{i}", [128, 5, 520], BF16) for i in range(NTL)]
        qT_t = [A(f"qT{i}", [128, 4, 128], BF16) for i in range(NTL)]
        kl_t = [A(f"kTl{i}", [128, 5, 4, 128], BF16) for i in range(NTL)]
        kh_t = [A(f"kTh{i}", [128, 5, 4, 128], BF16) for i in range(NTL)]
        C_pt = [A(f"pt{i}", [128, 640], BF16) for i in range(8 * NTL)]
        oc_t = [A(f"oc{i}", [128, 512], BF16) for i in range(NTL)]
        dn_t = [A(f"dn{i}", [128, 8], F32) for i in range(NTL)]
        tps = [P("tp0", [128, 1024], BF16)]
        psx = P("psx", [128, 1536], F32)
        po_t = [P(f"po{i}", [128, 512], F32) for i in range(2 * NTL)]
        OTv = C.OT[768:1280, :].rearrange("(i p) t -> p i t", p=128)
        ph = Phase(C.S, f"attnC{l}_init")
        zb = ph.buf("z")
        for i in range(NTL):
            o_memset(ph, "pool", kl_t[i][:], 0.0, [zb])
            o_memset(ph, "pool", kh_t[i][:], 0.0, [zb])
        ecb = ph.buf("EC")
        tb = ph.buf("tmp")
        for pi in range(5):
            ph.dma("sp", tmp[:], C.biasC[l, pi], writes=[tb])
            o_act(ph, EC[:, pi, :], tmp[:], AF.Exp, [tb], [ecb])
        ph.emit()
        for j0 in range(0, NT, NTL):
            ph = Phase(C.S, f"attnC{l}_{j0}")
            tpr = Ring(ph, tps, "tp")
            ps_b = [ph.buf("psA"), ph.buf("psB")]
            ps_v = [psx[:, 0:640], psx[:, 768:1408]]
            psk = 0
            info = []
            for t in range(NTL):
                j = j0 + t
                qb, kb, vb = ph.buf(), ph.buf(), ph.buf()
                cb0 = min(max(j - 2, 0), 59)
                r0 = PADR + 128 * j
                k0 = PADR + 128 * cb0
                ph.dma("sp", q_t[t][:], C.QK[r0:r0 + 128, QK_CQ:QK_CQ + 512], writes=[qb])
                ph.dma("sp", k_t[t][:], C.QK[k0:k0 + 640, QK_CK:QK_CK + 512].rearrange("(c p) d -> p c d", p=128), writes=[kb])
                ph.dma("sp", v_t[t][:], C.VC[k0:k0 + 640, :].rearrange("(c p) d -> p c d", p=128), writes=[vb])
                info.append((j, qb, kb, vb))
            ptbs = {}
            for t in range(NTL):
                j, qb, kb, vb = info[t]
                q, k, qT, kl, kh = q_t[t], k_t[t], qT_t[t], kl_t[t], kh_t[t]
                qTb, klb, khb = ph.buf(), ph.buf(), ph.buf()
                pat = c_pattern(j)
                tp, tpb = tpr.next()
                for i in range(4):
                    o_tp(ph, tp[:, i * 128:(i + 1) * 128], q[:, i * 128:(i + 1) * 128], C.identb[:], [qb], [tpb])
                    o_tp(ph, tp[:, (4 + i) * 128:(5 + i) * 128], k[:, 0, i * 128:(i + 1) * 128], C.identb[:], [kb], [tpb])
                o_cp(ph, "dve", qT[:].rearrange("p i t -> p (i t)"), tp[:, 0:512], [tpb], [qTb])
                o_cp(ph, "act", kl[0:64, 0, :, :].rearrange("p i t -> p (i t)"), tp[0:64, 512:1024], [tpb], [klb])
                o_cp(ph, "dve", kh[64:128, 0, :, :].rearrange("p i t -> p (i t)"), tp[64:128, 512:1024], [tpb], [khb])
                for f in range(2):
                    tp, tpb = tpr.next()
                    for cc in range(2):
                        c = 1 + 2 * f + cc
                        for i in range(4):
                            o_tp(ph, tp[:, (cc * 4 + i) * 128:(cc * 4 + i + 1) * 128], k[:, c, i * 128:(i + 1) * 128], C.identb[:], [kb], [tpb])
                    o_cp(ph, "act", kl[0:64, 1 + 2 * f:3 + 2 * f, :, :].rearrange("p c i t -> p (c i t)"), tp[0:64, :], [tpb], [klb])
                    o_cp(ph, "dve", kh[64:128, 1 + 2 * f:3 + 2 * f, :, :].rearrange("p c i t -> p (c i t)"), tp[64:128, :], [tpb], [khb])
                for h in range(8):
                    i, par = h // 2, h % 2
                    kk_, kkb = (kl, klb) if par == 0 else (kh, khb)
                    ps, psb = ps_v[psk % 2], ps_b[psk % 2]
                    psk += 1
                    for c in range(5):
                        o_mm(ph, ps[:, c * 128:(c + 1) * 128], kk_[:, c, i, :], qT[:, i, :], True, True, [kkb, qTb], [psb])
                    pt = C_pt[t * 8 + h]
                    ptb = ph.buf()
                    ptbs[(t, h)] = ptb
                    o_act(ph, pt[:], ps, AF.Exp, [psb], [ptb], scale=0.125)
                    o_tt(ph, "dve", pt[:].rearrange("p (c t) -> p c t", c=5), pt[:].rearrange("p (c t) -> p c t", c=5),
                         EC[:, pat, :].rearrange("p (c h t) -> p c h t", c=5, h=8)[:, :, h, :], ALU.mult, [ptb], [ptb])
            po_b = {}
            for t in range(NTL):
                j, qb, kb, vb = info[t]
                for h in range(8):
                    pt, ptb = C_pt[t * 8 + h], ptbs[(t, h)]
                    a = t * 2 + h // 4
                    if a not in po_b:
                        po_b[a] = ph.buf()
                    po, pob = po_t[a], po_b[a]
                    for c in range(5):
                        o_mm(ph, po[:, (h % 4) * 65:(h % 4 + 1) * 65], pt[:, c * 128:(c + 1) * 128],
                             v_t[t][:, c, h * 65:(h + 1) * 65], c == 0, c == 4, [ptb, vb], [pob])
            for t in range(NTL):
                j = info[t][0]
                oc, dn = oc_t[t], dn_t[t]
                ocb, dnb = ph.buf(), ph.buf()
                for a2 in range(2):
                    a = t * 2 + a2
                    pov = po_t[a][:, 0:260].rearrange("p (j d) -> p j d", j=4)
                    o_rcp(ph, dn[:, a2 * 4:(a2 + 1) * 4], pov[:, :, 64], [po_b[a]], [dnb])
                    o_tt(ph, "dve", oc[:, a2 * 256:(a2 + 1) * 256].rearrange("p (j d) -> p j d", j=4), pov[:, :, 0:64],
                         dn[:, a2 * 4:(a2 + 1) * 4].unsqueeze(2).to_broadcast([128, 4, 64]), ALU.mult, [po_b[a], dnb], [ocb])
                ph.dma("sp", C.OC[j * 128:(j + 1) * 128, :], oc[:], reads=[ocb])
            ph.emit()
        ph = Phase(C.S, f"attnC{l}_tr")
        ocr = Ring(ph, [A(f"oc2_{i}", [128, 512], BF16) for i in range(2)], "oc2")
        otr = Ring(ph, [A(f"ot2_{i}", [128, 4, 512], BF16) for i in range(2)], "ot2")
        tpr = Ring(ph, [tps[0]], "tp")
        nxt = ocr.next()
        ph.dma("sp", nxt[0][:], C.OC[0:128, :], writes=[nxt[1]])
        ot2, ot2b = None, None
        for j in range(NT):
            oc2, oc2b = nxt
            if j + 1 < NT:
                nxt = ocr.next()
                ph.dma("sp", nxt[0][:], C.OC[(j + 1) * 128:(j + 2) * 128, :], writes=[nxt[1]])
            tp, tpb = tpr.next()
            for i in range(4):
                o_tp(ph, tp[:, i * 128:(i + 1) * 128], oc2[:, i * 128:(i + 1) * 128], C.identb[:], [oc2b], [tpb])
            if j % 4 == 0:
                ot2, ot2b = otr.next()
            o_cp(ph, "act", ot2[:, :, (j % 4) * 128:(j % 4 + 1) * 128], tp[:, 0:512].rearrange("p (i t) -> p i t", i=4), [tpb], [ot2b])
            if j % 4 == 3:
                ph.dma("sp", OTv[:, :, (j // 4) * 512:(j // 4 + 1) * 512], ot2[:], reads=[ot2b])
        ph.emit()


def phase_merge(C, l):
    from contextlib import ExitStack
    nc = C.nc
    with ExitStack() as st:
        A, P = mk_alloc(nc, st, f"p3_{l}_")
        ph = Phase(C.S, f"merge{l}")
        ln_setup(ph, C, A)
        g, b, cb = load_gb(ph, A, C.ln1g[l], C.ln1b[l], "ln1")
        wbr = A("wbr", [128, 10, 1024], BF16)
        wo = A("wo", [128, 8, 1024], BF16)
        wb = ph.buf("w")
        srcs = [(C.w_bra[l], 4), (C.w_brb[l], 2), (C.w_brc[l], 4)]
        kk = 0
        wbs = []
        for src, nk in srcs:
            for k in range(nk):
                wbc = ph.buf()
                wbs.append(wbc)
                ph.dma("pool", wbr[:, kk, :], src[k * 128:(k + 1) * 128, :], writes=[wbc])
                kk += 1
        for k in range(8):
            wbc = ph.buf()
            wbs.append(wbc)
            ph.dma("pool", wo[:, k, :], C.w_out[l, k * 128:(k + 1) * 128, :], writes=[wbc])
        otr = Ring(ph, [A(f"ot{i}", [128, 10, 512], BF16) for i in range(2)], "ot")
        gtr = Ring(ph, [A(f"gt{i}", [128, 24, 512], BF16) for i in range(2)], "gt")
        t1r = Ring(ph, [A(f"t1_{i}", [128, 512], F32) for i in range(2)], "t1")
        t2r = Ring(ph, [A(f"t2_{i}", [128, 512], F32) for i in range(2)], "t2")
        t3r = Ring(ph, [A(f"t3_{i}", [128, 512], F32) for i in range(2)], "t3")
        mgr = Ring(ph, [A(f"mg{i}", [128, 8, 512], BF16) for i in range(2)], "mg")
        hr = Ring(ph, [A(f"h{i}", [128, D], F32) for i in range(2)], "h")
        zr = Ring(ph, [A(f"z{i}", [128, D], F32) for i in range(5)], "z")
        orr = Ring(ph, [A(f"o{i}", [128, D], F32) for i in range(3)], "o")
        obr = Ring(ph, [A(f"ob{i}", [128, D], BF16) for i in range(2)], "ob")
        par = Ring(ph, [P(f"pa{i}", [128, 512], F32) for i in range(2)], "pa")
        pbr = Ring(ph, [P(f"pb{i}", [128, 512], F32) for i in range(2)], "pb")
        pcr = Ring(ph, [P(f"pc{i}", [128, 512], F32) for i in range(2)], "pc")
        pyr = Ring(ph, [P("py", [128, 1024], F32)], "py")
        OTv = C.OT.rearrange("(i p) t -> p i t", p=128)
        GTv = C.GT.rearrange("(i p) t -> p i t", p=128)

        def loads(n):
            ot, otb = otr.next()
            gt, gtb = gtr.next()
            ph.dma("sp", ot[:], OTv[:, :, n * 512:(n + 1) * 512], writes=[otb])
            for a in range(3):
                ph.dma("sp", gt[:, a * 8:(a + 1) * 8, :], GTv[:, a * 8:(a + 1) * 8, n * 512:(n + 1) * 512], writes=[gtb])
            return ot, otb, gt, gtb

        nxt = loads(0)
        for n in range(T // 512):
            ot, otb, gt, gtb = nxt
            if n + 1 < T // 512:
                nxt = loads(n + 1)
            mg, mgb = mgr.next()
            for mc in range(8):
                pa, pab = par.next()
                pb, pbb = pbr.next()
                pc, pcb = pcr.next()
                for (pp, ppb, k0, nk) in ((pa, pab, 0, 4), (pb, pbb, 4, 2), (pc, pcb, 6, 4)):
                    for k in range(nk):
                        o_mm(ph, pp[:], wbr[:, k0 + k, mc * 128:(mc + 1) * 128], ot[:, k0 + k, :], k == 0, k == nk - 1, wbs + [otb], [ppb])
                t1, t1b = t1r.next()
                t2, t2b = t2r.next()
                t3, t3b = t3r.next()
                o_tt(ph, "dve", t1[:], pa[:], gt[:, mc, :], ALU.mult, [pab, gtb], [t1b])
                o_tt(ph, "dve", t2[:], pb[:], gt[:, 8 + mc, :], ALU.mult, [pbb, gtb], [t2b])
                o_tt(ph, "dve", t3[:], pc[:], gt[:, 16 + mc, :], ALU.mult, [pcb, gtb], [t3b])
                o_tt(ph, "pool", t1[:], t1[:], t2[:], ALU.add, [t1b, t2b], [t1b])
                o_tt(ph, "pool", mg[:, mc, :], t1[:], t3[:], ALU.add, [t1b, t3b], [mgb])
            subs = []
            for sub in range(4):
                i = 4 * n + sub
                h, hb = hr.next()
                ph.dma("sp", h[:], C.H[i * 128:(i + 1) * 128, :], writes=[hb])
                py, pyb = pyr.next()
                for half in range(2):
                    for k in range(8):
                        o_mm(ph, py[:, half * 512:(half + 1) * 512], mg[:, k, sub * 128:(sub + 1) * 128],
                             wo[:, k, half * 512:(half + 1) * 512], k == 0, k == 7, [mgb] + wbs, [pyb])
                z, zb = zr.next()
                o_stt(ph, z[:], h[:], ALPHA, py[:], ALU.mult, ALU.add, [hb, pyb], [zb])
                mv, mvb = ln_s1(ph, C, z[:], zb)
                subs.append((i, z, zb, mv, mvb))
            for (i, z, zb, mv, mvb) in subs:
                ln_s2(ph, C, mv, mvb)
            for (i, z, zb, mv, mvb) in subs:
                o, ob = orr.next()
                ln_s3(ph, C, z[:], zb, mv, mvb, o[:], ob, g[:], b[:], cb)
                ph.dma("sp", C.H1[i * 128:(i + 1) * 128, :], o[:], reads=[ob])
                o16, o16b = obr.next()
                o_cp(ph, "act", o16[:], o[:], [ob], [o16b])
                ph.dma("sp", C.H1b[i * 128:(i + 1) * 128, :], o16[:], reads=[o16b])
        ph.emit()


def phase_route(C, l):
    from contextlib import ExitStack
    nc = C.nc
    with ExitStack() as st:
        A, P = mk_alloc(nc, st, f"p3b_{l}_")
        ph = Phase(C.S, f"route{l}")
        wr = A("wr", [128, 8, 36], F32)
        brr = A("brr", [128, 36], F32)
        wb = ph.buf("w")
        ph.dma("sp", wr[:], C.wr[l].rearrange("(k p) e -> p k e", p=128), writes=[wb])
        ph.dma("sp", brr[:], C.br[l].to_broadcast([128, 36]), writes=[wb])
        LG = A("LG", [128, NT, 36], F32)
        lgb = ph.buf("LG")
        hr = Ring(ph, [A(f"h{i}", [128, D], F32) for i in range(2)], "h")
        hTr = Ring(ph, [A(f"hT{i}", [128, 8, 128], F32) for i in range(2)], "hT")
        tpr = Ring(ph, [P("tp", [128, 8, 128], F32)], "tp")
        plr = Ring(ph, [P("pl", [128, 512], F32)], "pl")
        ppr = Ring(ph, [P(f"pp{i}", [128, 512], F32) for i in range(2)], "pp")
        pcr = Ring(ph, [P(f"pcs{i}", [128, 512], F32) for i in range(2)], "pcs")
        nxt = hr.next()
        ph.dma("sp", nxt[0][:], C.H1[0:128, :], writes=[nxt[1]])
        for i in range(NT):
            h, hb = nxt
            if i + 1 < NT:
                nxt = hr.next()
                ph.dma("sp", nxt[0][:], C.H1[(i + 1) * 128:(i + 2) * 128, :], writes=[nxt[1]])
            tp, tpb = tpr.next()
            for k in range(8):
                o_tp(ph, tp[:, k, :], h[:, k * 128:(k + 1) * 128], C.identf[:], [hb], [tpb])
            hT, hTb = hTr.next()
            o_cp(ph, "act", hT[:], tp[:], [tpb], [hTb])
            pl, plb = plr.next()
            for k in range(8):
                o_mm(ph, pl[:, 0:36], hT[:, k, :], wr[:, k, :], k == 0, k == 7, [hTb, wb], [plb])
            o_tt(ph, "dve", LG[:, i, :], pl[:, 0:36], brr[:], ALU.add, [plb, wb], [lgb])
        def S_(name, shape, dt=F32):
            return A(name, shape, dt), ph.buf(name)
        lg = LG[:, :, 0:4]
        le = LG[:, :, 4:36].rearrange("p n (g e) -> p n g e", g=4)
        gmax, gmb = S_("gmax", [128, NT])
        o_red(ph, gmax[:], lg, ALU.max, [lgb], [gmb])
        goh, gohb = S_("goh", [128, NT, 4])
        o_tt(ph, "dve", goh[:], lg, gmax[:].unsqueeze(2).to_broadcast([128, NT, 4]), ALU.is_equal, [lgb, gmb], [gohb])
        gex, gexb = S_("gex", [128, NT, 4])
        o_tt(ph, "dve", gex[:], lg, gmax[:].unsqueeze(2).to_broadcast([128, NT, 4]), ALU.subtract, [lgb, gmb], [gexb])
        o_act(ph, gex[:], gex[:], AF.Exp, [gexb], [gexb])
        gw, gwb = S_("gw", [128, NT])
        o_red(ph, gw[:], gex[:], ALU.add, [gexb], [gwb])
        o_rcp(ph, gw[:], gw[:], [gwb], [gwb])
        esel, eselb = S_("esel", [128, NT, 8])
        etmp, etmpb = S_("etmp", [128, NT, 8])
        for g in range(4):
            dst, dstb = (esel, eselb) if g == 0 else (etmp, etmpb)
            o_tt(ph, "dve", dst[:], le[:, :, g, :], goh[:, :, g].unsqueeze(2).to_broadcast([128, NT, 8]), ALU.mult, [lgb, gohb], [dstb])
            if g > 0:
                o_tt(ph, "dve", esel[:], esel[:], etmp[:], ALU.add, [eselb, etmpb], [eselb])
        m1, m1b = S_("m1", [128, NT])
        o_red(ph, m1[:], esel[:], ALU.max, [eselb], [m1b])
        oh1, oh1b = S_("oh1", [128, NT, 8])
        o_tt(ph, "dve", oh1[:], esel[:], m1[:].unsqueeze(2).to_broadcast([128, NT, 8]), ALU.is_equal, [eselb, m1b], [oh1b])
        e2, e2b = S_("e2", [128, NT, 8])
        o_ts(ph, "dve", e2[:], oh1[:], -1e30, None, ALU.mult, None, [oh1b], [e2b])
        o_tt(ph, "dve", e2[:], e2[:], esel[:], ALU.add, [e2b, eselb], [e2b])
        m2, m2b = S_("m2", [128, NT])
        o_red(ph, m2[:], e2[:], ALU.max, [e2b], [m2b])
        oh2, oh2b = S_("oh2", [128, NT, 8])
        o_tt(ph, "dve", oh2[:], e2[:], m2[:].unsqueeze(2).to_broadcast([128, NT, 8]), ALU.is_equal, [e2b, m2b], [oh2b])
        ee, eeb = S_("ee", [128, NT])
        o_tt(ph, "dve", ee[:], m2[:], m1[:], ALU.subtract, [m1b, m2b], [eeb])
        o_act(ph, ee[:], ee[:], AF.Exp, [eeb], [eeb])
        p1, p1b = S_("p1", [128, NT])
        o_ts(ph, "dve", p1[:], ee[:], 1.0, None, ALU.add, None, [eeb], [p1b])
        o_rcp(ph, p1[:], p1[:], [p1b], [p1b])
        gwt = C.GW
        gwtb = ph.buf("GW")
        o_tt(ph, "dve", gwt[:, :, 1], ee[:], p1[:], ALU.mult, [eeb, p1b], [gwtb])
        o_tt(ph, "dve", gwt[:, :, 1], gwt[:, :, 1], gw[:], ALU.mult, [gwtb, gwb], [gwtb])
        o_tt(ph, "dve", gwt[:, :, 0], p1[:], gw[:], ALU.mult, [p1b, gwb], [gwtb])
        M1, M1b = S_("M1", [128, NT, 4, 8])
        M2, M2b = S_("M2", [128, NT, 4, 8])
        for g in range(4):
            o_tt(ph, "dve", M1[:, :, g, :], oh1[:], goh[:, :, g].unsqueeze(2).to_broadcast([128, NT, 8]), ALU.mult, [oh1b, gohb], [M1b])
            o_tt(ph, "dve", M2[:, :, g, :], oh2[:], goh[:, :, g].unsqueeze(2).to_broadcast([128, NT, 8]), ALU.mult, [oh2b, gohb], [M2b])
        Mb16, Mb16b = S_("Mb16", [128, NT * 32], BF16)
        o_tt(ph, "dve", Mb16[:], M1[:].rearrange("p n g e -> p (n g e)"), M2[:].rearrange("p n g e -> p (n g e)"), ALU.add, [M1b, M2b], [Mb16b])
        POS, POSb = S_("POS", [128, NT * 32])
        CS, CSb = S_("CS", [128, NT * 32])
        for ch in range(4):
            pp, ppb = ppr.next()
            o_mm(ph, pp[:], C.Umat[:], Mb16[:, ch * 512:(ch + 1) * 512], True, True, [Mb16b], [ppb])
            o_cp(ph, "dve", POS[:, ch * 512:(ch + 1) * 512], pp[:], [ppb], [POSb])
            pcs, pcsb = pcr.next()
            o_mm(ph, pcs[:], C.ones[:], Mb16[:, ch * 512:(ch + 1) * 512], True, True, [Mb16b], [pcsb])
            o_cp(ph, "act", CS[:, ch * 512:(ch + 1) * 512], pcs[:], [pcsb], [CSb])
        SA, SAb = S_("SA", [128, NT * 32])
        SB, SBb = S_("SB", [128, NT * 32])
        o_cp(ph, "dve", SA[:], CS[:], [CSb], [SAb])
        cur, curb, oth, othb = SA, SAb, SB, SBb
        s_ = 1
        while s_ < NT:
            o_cp(ph, "dve", oth[:, 0:s_ * 32], cur[:, 0:s_ * 32], [curb], [othb])
            o_tt(ph, "dve", oth[:, s_ * 32:], cur[:, s_ * 32:], cur[:, 0:(NT - s_) * 32], ALU.add, [curb], [othb])
            cur, curb, oth, othb = oth, othb, cur, curb
            s_ *= 2
        o_tt(ph, "dve", POS[:], POS[:], cur[:], ALU.add, [POSb, curb], [POSb])
        o_tt(ph, "dve", POS[:], POS[:], CS[:], ALU.subtract, [POSb, CSb], [POSb])
        o_ts(ph, "dve", POS[:], POS[:], float(CAP - 1), None, ALU.min, None, [POSb], [POSb])
        o_tt(ph, "dve", POS[:].rearrange("p (n e) -> p n e", e=32), POS[:].rearrange("p (n e) -> p n e", e=32),
             C.ecap[:].unsqueeze(1).to_broadcast([128, NT, 32]), ALU.add, [POSb], [POSb])
        DF, DFb = S_("DF", [128, NT, 2])
        for kx, (Mx, Mxb) in enumerate(((M1, M1b), (M2, M2b))):
            o_tt(ph, "dve", Mx[:].rearrange("p n g e -> p (n g e)"), Mx[:].rearrange("p n g e -> p (n g e)"), POS[:], ALU.mult, [Mxb, POSb], [Mxb])
            o_red(ph, DF[:, :, kx], Mx[:].rearrange("p n g e -> p n (g e)"), ALU.add, [Mxb], [DFb])
        dib = ph.buf("DI")
        o_cp(ph, "dve", C.DI[:], DF[:], [DFb], [dib])
        xr = Ring(ph, [A(f"x{i}", [128, D], BF16) for i in range(3)], "x")
        for i in range(NT):
            xt, xb = xr.next()
            ph.dma("sp", xt[:], C.H1b[i * 128:(i + 1) * 128, :], writes=[xb])
            for kx in range(2):
                ph.dma_fn("pool", (lambda e, xt=xt, i=i, kx=kx: e.indirect_dma_start(
                    out=C.XS, out_offset=bass.IndirectOffsetOnAxis(ap=C.DI[:, i, kx:kx + 1], axis=0), in_=xt[:], in_offset=None)),
                    reads=[xb, dib])
        ph.emit()


def phase_experts(C, l):
    from contextlib import ExitStack
    nc = C.nc
    NST = CAP // 128
    HN = CAP // 2
    with ExitStack() as st:
        A, P = mk_alloc(nc, st, f"p4_{l}_")
        ph = Phase(C.S, f"experts{l}")
        wgr = Ring(ph, [A(f"wg{i}", [128, 8, 512], BF16) for i in range(2)], "wg")
        wur = Ring(ph, [A(f"wu{i}", [128, 8, 512], BF16) for i in range(2)], "wu")
        wdr = Ring(ph, [A(f"wd{i}", [128, 4, 1024], BF16) for i in range(2)], "wd")
        wdsr = Ring(ph, [A(f"wds{i}", [128, 4, 1024], F32) for i in range(2)], "wds")
        xsr = Ring(ph, [A(f"xs{i}", [128, NST, D], BF16) for i in range(2)], "xs")
        xTr = Ring(ph, [A(f"xT{i}", [128, 8, CAP], BF16) for i in range(2)], "xT")
        sgr = Ring(ph, [A(f"sg{i}", [128, HN], F32) for i in range(2)], "sg")
        hdr = Ring(ph, [A(f"hd{i}", [128, 4, HN], BF16) for i in range(2)], "hd")
        yor = Ring(ph, [A(f"yo{i}", [128, D], BF16) for i in range(3)], "yo")
        tpr = Ring(ph, [P(f"tp{i}", [128, 1024], BF16) for i in range(2)], "tp")
        pgr = Ring(ph, [P(f"pg{i}", [128, 512], F32) for i in range(2)], "pg")
        pur = Ring(ph, [P(f"pu{i}", [128, 512], F32) for i in range(2)], "pu")
        pyr = Ring(ph, [P("py", [128, 1024], F32)], "py")

        def loads(e):
            wg, wgb = wgr.next()
            wu, wub = wur.next()
            wd, wdb = wdr.next()
            xs, xsb = xsr.next()
            ph.dma("pool", wg[:], C.w_eg[l, e].rearrange("(k p) f -> p k f", p=128), writes=[wgb])
            ph.dma("pool", wu[:], C.w_eu[l, e].rearrange("(k p) f -> p k f", p=128), writes=[wub])
            wds, wdsb = wdsr.next()
            ph.dma("sp", wds[:], C.w_ed[l, e].rearrange("(k p) f -> p k f", p=128), writes=[wdsb])
            o_cp(ph, "pool", wd[:], wds[:], [wdsb], [wdb])
            ph.dma("sp", xs[:], C.XS[e * CAP:(e + 1) * CAP, :].rearrange("(s p) d -> p s d", p=128), writes=[xsb])
            return wg, wgb, wu, wub, wd, wdb, xs, xsb

        nxt = loads(0)
        for e in range(32):
            wg, wgb, wu, wub, wd, wdb, xs, xsb = nxt
            if e + 1 < 32:
                nxt = loads(e + 1)
            xT, xTb = xTr.next()
            for s_ in range(NST):
                tp, tpb = tpr.next()
                for k in range(8):
                    o_tp(ph, tp[:, k * 128:(k + 1) * 128], xs[:, s_, k * 128:(k + 1) * 128], C.identb[:], [xsb], [tpb])
                o_cp(ph, "dve" if s_ % 2 == 0 else "act", xT[:, :, s_ * 128:(s_ + 1) * 128],
                     tp[:].rearrange("p (k t) -> p k t", k=8), [tpb], [xTb])
            for hh in range(2):
                hd, hdb = hdr.next()
                for fc in range(4):
                    pg, pgb = pgr.next()
                    pu, pub = pur.next()
                    for k in range(8):
                        o_mm(ph, pg[:, 0:HN], wg[:, k, fc * 128:(fc + 1) * 128], xT[:, k, hh * HN:(hh + 1) * HN], k == 0, k == 7, [wgb, xTb], [pgb])
                    for k in range(8):
                        o_mm(ph, pu[:, 0:HN], wu[:, k, fc * 128:(fc + 1) * 128], xT[:, k, hh * HN:(hh + 1) * HN], k == 0, k == 7, [wub, xTb], [pub])
                    sg, sgb = sgr.next()
                    o_act(ph, sg[:], pg[:, 0:HN], AF.Silu, [pgb], [sgb])
                    o_tt(ph, "dve", hd[:, fc, :], sg[:], pu[:, 0:HN], ALU.mult, [sgb, pub], [hdb])
                for s_ in range(NST // 2):
                    py, pyb = pyr.next()
                    for half in range(2):
                        for k in range(4):
                            o_mm(ph, py[:, half * 512:(half + 1) * 512], hd[:, k, s_ * 128:(s_ + 1) * 128],
                                 wd[:, k, half * 512:(half + 1) * 512], k == 0, k == 3, [hdb, wdb], [pyb])
                    yo, yob = yor.next()
                    o_cp(ph, "dve" if s_ % 2 == 0 else "act", yo[:], py[:], [pyb], [yob])
                    r0 = e * CAP + hh * HN + s_ * 128
                    ph.dma("sp", C.R[r0:r0 + 128, :], yo[:], reads=[yob])
        ph.emit()


def phase_combine(C, l, dst):
    from contextlib import ExitStack
    nc = C.nc
    with ExitStack() as st:
        A, P = mk_alloc(nc, st, f"p5_{l}_")
        ph = Phase(C.S, f"combine{l}")
        ln_setup(ph, C, A)
        g, b, cb = load_gb(ph, A, C.ln2g[l], C.ln2b[l], "ln2")
        r1r = Ring(ph, [A(f"r1_{i}", [128, D], BF16) for i in range(2)], "r1")
        r2r = Ring(ph, [A(f"r2_{i}", [128, D], BF16) for i in range(2)], "r2")
        hr = Ring(ph, [A(f"h{i}", [128, D], F32) for i in range(2)], "h")
        yr = Ring(ph, [A(f"y{i}", [128, D], F32) for i in range(2)], "y")
        zr = Ring(ph, [A(f"z{i}", [128, D], F32) for i in range(5)], "z")
        orr = Ring(ph, [A(f"o{i}", [128, D], F32) for i in range(3)], "o")

        def loads(i):
            r1, r1b = r1r.next()
            r2, r2b = r2r.next()
            h, hb = hr.next()
            for kx, (rt, rb) in enumerate(((r1, r1b), (r2, r2b))):
                ph.dma_fn("pool", (lambda e, rt=rt, i=i, kx=kx: e.indirect_dma_start(
                    out=rt[:], out_offset=None, in_=C.R, in_offset=bass.IndirectOffsetOnAxis(ap=C.DI[:, i, kx:kx + 1], axis=0))),
                    writes=[rb])
            ph.dma("sp", h[:], C.H1[i * 128:(i + 1) * 128, :], writes=[hb])
            return r1, r1b, r2, r2b, h, hb

        nxt = loads(0)
        grp = []
        for i in range(NT):
            r1, r1b, r2, r2b, h, hb = nxt
            if i + 1 < NT:
                nxt = loads(i + 1)
            y, yb = yr.next()
            o_act(ph, y[:], r1[:], AF.Copy, [r1b], [yb], scale=C.GW[:, i, 0:1])
            o_stt(ph, y[:], r2[:], C.GW[:, i, 1:2], y[:], ALU.mult, ALU.add, [r2b, yb], [yb])
            z, zb = zr.next()
            o_stt(ph, z[:], h[:], ALPHA, y[:], ALU.mult, ALU.add, [hb, yb], [zb])
            mv, mvb = ln_s1(ph, C, z[:], zb)
            grp.append((i, z, zb, mv, mvb))
            if len(grp) == 4 or i == NT - 1:
                for (ii, z_, zb_, mv_, mvb_) in grp:
                    ln_s2(ph, C, mv_, mvb_)
                for (ii, z_, zb_, mv_, mvb_) in grp:
                    o, ob = orr.next()
                    ln_s3(ph, C, z_[:], zb_, mv_, mvb_, o[:], ob, g[:], b[:], cb, geng="dve", beng="dve")
                    ph.dma("sp", dst[ii * 128:(ii + 1) * 128, :], o[:], reads=[ob])
                grp = []
        ph.emit()


def phase_pre(C):
    from contextlib import ExitStack
    nc = C.nc
    with ExitStack() as st:
        A, P = mk_alloc(nc, st, "pre_")
        ph = Phase(C.S, "pre")
        cb = ph.buf("const")
        o_memset(ph, "pool", C.identb[:], 0.0, [cb])
        ph.op("pool", lambda e: e.affine_select(out=C.identb[:], in_=C.identb[:], pattern=[[-1, 128]], compare_op=ALU.not_equal,
                                                 fill=1.0, base=0, channel_multiplier=1), [cb], [cb])
        o_memset(ph, "pool", C.identf[:], 0.0, [cb])
        ph.op("pool", lambda e: e.affine_select(out=C.identf[:], in_=C.identf[:], pattern=[[-1, 128]], compare_op=ALU.not_equal,
                                                 fill=1.0, base=0, channel_multiplier=1), [cb], [cb])
        o_memset(ph, "pool", C.Umat[:], 1.0, [cb])
        ph.op("pool", lambda e: e.affine_select(out=C.Umat[:], in_=C.Umat[:], pattern=[[1, 128]], compare_op=ALU.is_gt,
                                                 fill=0.0, base=0, channel_multiplier=-1), [cb], [cb])
        o_memset(ph, "pool", C.ones[:], 1.0, [cb])
        o_memset(ph, "pool", C.eps[:], LN_EPS, [cb])
        o_memset(ph, "pool", C.fence[:], 0.0, [cb])
        eci = A("eci", [128, 32], I32)
        ph.op("pool", lambda e: e.iota(eci[:], pattern=[[CAP, 32]], base=0, channel_multiplier=0), [cb], [cb])
        o_cp(ph, "pool", C.ecap[:], eci[:], [cb], [cb])
        tmpr = Ring(ph, [A(f"bt{i}", [128, 3072], F32) for i in range(2)], "bt")
        tmp, tb = tmpr.next()
        ph.dma("sp", tmp[:], C.biasA, writes=[tb])
        o_act(ph, C.EA[:], tmp[:], AF.Exp, [tb], [cb])
        ph.dma("sp", C.BB[:].rearrange("p (g c) -> p g c", g=3), C.biasB.rearrange("g p c -> p g c"), writes=[cb])
        zt = A("zt", [128, 8 * 520], BF16)
        zb = ph.buf("zt")
        o_memset(ph, "pool", zt[:], 0.0, [zb])
        na = PADR // 128
        for base in (0, PADR + T):
            for a in range(na):
                r0 = base + a * 128
                ph.dma("sp", C.QK[r0:r0 + 128, :], zt[:, 0:QK_W], reads=[zb])
            for arr, wd_ in [(C.VA, 130), (C.VB[0], 260), (C.VB[1], 260), (C.VB[2], 260), (C.VC, 520)]:
                ph.dma("sp", arr[base:base + PADR, :].rearrange("(p a) c -> p (a c)", a=na), zt[:, 0:na * wd_], reads=[zb])
        ph.emit()


def run_b(C, l):
    for gi in range(3):
        phase_attn_b(C, l, gi)
    phase_attn_bc(C, l)


INPUT_SPECS = [
    ("x", [T, D]), ("ln0_g", [1, D]), ("ln0_b", [1, D]),
    ("biasA", [128, 3072]), ("biasB", [3, 128, 1024]), ("biasC", [DEPTH, 5, 128, 5120]),
    ("w_in", [DEPTH, D, 7680]), ("bgT", [DEPTH, 128, 24]), ("sink", [DEPTH, 1, 8]),
    ("w_bra", [DEPTH, 512, D]), ("w_brb", [DEPTH, 256, D]), ("w_brc", [DEPTH, 512, D]), ("w_out", [DEPTH, D, D]),
    ("ln1g", [DEPTH, 1, D]), ("ln1b", [DEPTH, 1, D]), ("wr", [DEPTH, D, 36]), ("br", [DEPTH, 1, 36]),
    ("w_eg", [DEPTH, 32, D, 512]), ("w_eu", [DEPTH, 32, D, 512]), ("w_ed", [DEPTH, 32, 512, D]),
    ("ln2g", [DEPTH, 1, D]), ("ln2b", [DEPTH, 1, D]),
]


def build(debug=(), upto=None, only_inputs=None, only_steps=None):
    from contextlib import ExitStack
    nc = bass.Bass("TRN2", target_bir_lowering=False)
    C = Ctx()
    C.nc = nc
    for name, shape in INPUT_SPECS:
        if only_inputs is not None and name not in only_inputs:
            continue
        ap = nc.dram_tensor(name, list(shape), F32, kind="ExternalInput").ap()
        setattr(C, {"ln0_g": "ln0g", "ln0_b": "ln0b"}.get(name, name), ap)

    def scr(name, shape, dt):
        kind = "ExternalOutput" if name in debug else "Internal"
        return nc.dram_tensor(name, list(shape), dt, kind=kind).ap()

    C.out = nc.dram_tensor("out", [T, D], F32, kind="ExternalOutput").ap()
    C.H = scr("H", [T, D], F32)
    C.QK = scr("QK", [ROWS, QK_W], BF16)
    C.VA = scr("VA", [ROWS, 130], BF16)
    C.VB = [scr(f"VB{g}", [ROWS, 260], BF16) for g in range(3)]
    C.VC = scr("VC", [ROWS, 520], BF16)
    C.GT = scr("GT", [3072, T], BF16)
    C.NB = [scr(f"NB{g}", [T, 260], F32) for g in range(3)]
    C.OT = scr("OT", [1280, T], BF16)
    C.OC = scr("OC", [T, 512], BF16)
    C.H1 = scr("H1", [T, D], F32)
    C.H1b = scr("H1b", [T, D], BF16)
    C.XS = scr("XS", [NSLOT, D], BF16)
    C.R = scr("R", [NSLOT, D], BF16)
    with ExitStack() as st:
        C.S = Sched(nc, st)
        A, _ = mk_alloc(nc, st, "g_")
        C.identb = A("identb", [128, 128], BF16)
        C.identf = A("identf", [128, 128], F32)
        C.Umat = A("Umat", [128, 128], BF16)
        C.ones = A("ones", [128, 128], BF16)
        C.eps = A("eps", [128, 1], F32)
        C.ecap = A("ecap", [128, 32], F32)
        C.EA = A("EA", [128, 3072], BF16)
        C.BB = A("BB", [128, 3072], F32)
        C.fence = A("fence", [128, 2], F32)
        C.DI = A("DI", [128, NT, 2], I32)
        C.GW = A("GW", [128, NT, 2], F32)
        steps = [("pre", lambda: phase_pre(C)), ("ln0", lambda: phase_ln0(C))]
        for l in range(DEPTH):
            dst = C.H if l + 1 < DEPTH else C.out
            steps += [
                (f"proj{l}", lambda l=l: phase_proj(C, l)),
                (f"attnA{l}", lambda l=l: phase_attn_a(C, l)),
                (f"attnB{l}", lambda l=l: run_b(C, l)),
                (f"attnC{l}", lambda l=l: phase_attn_c(C, l)),
                (f"merge{l}", lambda l=l: phase_merge(C, l)),
                (f"route{l}", lambda l=l: phase_route(C, l)),
                (f"experts{l}", lambda l=l: phase_experts(C, l)),
                (f"combine{l}", lambda l=l, dst=dst: phase_combine(C, l, dst)),
            ]
        for name, fn in steps:
            if only_steps is not None and name not in only_steps:
                continue
            fn()
            if upto is not None and name == upto:
                break
    return nc


def host_inputs(inp):
    f = lambda a: np.ascontiguousarray(np.asarray(a), dtype=np.float32)
    rel_bias = f(inp["rel_bias"])
    rpb_c = f(inp["rpb_c"])
    ba, bb, bc = host_bias_tables(rel_bias, rpb_c)
    b_gate = f(inp["b_gate"])
    w_rg, w_re = f(inp["w_rg"]), f(inp["w_re"])
    wr = np.concatenate([w_rg, w_re.transpose(0, 2, 1, 3).reshape(DEPTH, D, 32)], axis=2)
    br = np.concatenate([f(inp["b_rg"]), f(inp["b_re"]).reshape(DEPTH, 32)], axis=1).reshape(DEPTH, 1, 36)
    shared = {
        "ln0_g": f(inp["ln0_g"]).reshape(1, D), "ln0_b": f(inp["ln0_b"]).reshape(1, D),
        "biasA": np.ascontiguousarray(ba.reshape(128, 3072)),
        "biasB": np.ascontiguousarray(bb.reshape(3, 128, 1024)),
        "biasC": np.ascontiguousarray(bc.reshape(DEPTH, 5, 128, 5120)),
        "w_in": f(inp["w_in"]),
        "bgT": np.ascontiguousarray(b_gate.reshape(DEPTH, 24, 128).transpose(0, 2, 1)),
        "sink": f(inp["sink_a"]).reshape(DEPTH, 1, 8),
        "w_bra": f(inp["w_br_a"]), "w_brb": f(inp["w_br_b"]), "w_brc": f(inp["w_br_c"]), "w_out": f(inp["w_out"]),
        "ln1g": f(inp["ln1_g"]).reshape(DEPTH, 1, D), "ln1b": f(inp["ln1_b"]).reshape(DEPTH, 1, D),
        "wr": np.ascontiguousarray(wr), "br": np.ascontiguousarray(br),
        "w_eg": f(inp["w_eg"]), "w_eu": f(inp["w_eu"]), "w_ed": f(inp["w_ed"]),
        "ln2g": f(inp["ln2_g"]).reshape(DEPTH, 1, D), "ln2b": f(inp["ln2_b"]).reshape(DEPTH, 1, D),
    }
    return shared


_NC_CACHE = {}


def kernel(**inputs):
    x = np.ascontiguousarray(np.asarray(inputs["x"]), dtype=np.float32)
    shared = host_inputs(inputs)
    if "nc" not in _NC_CACHE:
        _NC_CACHE["nc"] = build()
    nc = _NC_CACHE["nc"]
    in_maps = []
    for c in range(8):
        m = dict(shared)
        m["x"] = x[c]
        in_maps.append(m)
    res = run_bass_kernel_spmd(nc, in_maps, core_ids=list(range(8)))
    return np.stack([np.asarray(r["out"], dtype=np.float32) for r in res.results], axis=0)
```

```python
import numpy as np
import concourse.bass as bass
import concourse.mybir as mybir
from concourse.bass_utils import run_bass_kernel_spmd

F32 = mybir.dt.float32
BF16 = mybir.dt.bfloat16
I32 = mybir.dt.int32
U32 = mybir.dt.uint32
ALU = mybir.AluOpType
AF = mybir.ActivationFunctionType
AX = mybir.AxisListType

ENGS = ("pe", "act", "dve", "pool", "sp")


class Buf:
    __slots__ = ("name", "last_w", "readers")

    def __init__(self, name=""):
        self.name = name
        self.last_w = None
        self.readers = []


class Op:
    __slots__ = ("eng", "fn", "deps", "is_dma", "sem", "count", "has_dep", "prev_dma", "idx")

    def __init__(self, eng, fn, is_dma):
        self.eng = eng
        self.fn = fn
        self.deps = []
        self.is_dma = is_dma
        self.sem = None
        self.count = 0
        self.has_dep = False
        self.prev_dma = None


class Sched:
    NS = 4
    ND = 8

    def __init__(self, nc, stack):
        self.nc = nc
        self.esem = {e: [stack.enter_context(nc.semaphore(f"s_{e}{i}")) for i in range(self.NS)] for e in ENGS}
        self.ecnt = {e: [0] * self.NS for e in ENGS}
        self.ek = {e: 0 for e in ENGS}
        self.dsem = {e: [stack.enter_context(nc.semaphore(f"d_{e}{i}")) for i in range(self.ND)] for e in ("sp", "pool", "act")}
        self.dcnt = {e: [0] * self.ND for e in self.dsem}
        self.dk = {e: 0 for e in self.dsem}
        self.dlast = {e: [None] * self.ND for e in self.dsem}
        self.waited = {e: {} for e in ENGS}


class Phase:
    def __init__(self, sched, name):
        self.s = sched
        self.nc = sched.nc
        self.name = name
        self.ops = []
        self.bufs = []

    def buf(self, name=""):
        b = Buf(name)
        self.bufs.append(b)
        return b

    def _add(self, op, reads, writes):
        deps = []
        for b in reads:
            if b.last_w is not None:
                deps.append(b.last_w)
        for b in writes:
            if b.last_w is not None:
                deps.append(b.last_w)
            deps.extend(b.readers)
        for b in reads:
            b.readers.append(op)
        for b in writes:
            b.last_w = op
            b.readers = []
        seen = set()
        for d in deps:
            if d is op or id(d) in seen:
                continue
            seen.add(id(d))
            if d.eng == "pe" and op.eng == "pe" and not d.is_dma and not op.is_dma:
                continue
            op.deps.append(d)
            d.has_dep = True
        self.ops.append(op)
        return op

    def op(self, eng, fn, reads=(), writes=()):
        return self._add(Op(eng, fn, False), reads, writes)

    def dma(self, q, out, in_, reads=(), writes=(), **kw):
        def fn(e):
            return e.dma_start(out=out, in_=in_, **kw)
        return self._add(Op(q, fn, True), reads, writes)

    def dma_fn(self, q, fn, reads=(), writes=()):
        return self._add(Op(q, fn, True), reads, writes)

    def emit(self):
        s = self.s
        nc = self.nc
        for op in self.ops:
            e = op.eng
            if op.is_dma:
                k = s.dk[e] % s.ND
                s.dk[e] += 1
                op.prev_dma = s.dlast[e][k]
                s.dcnt[e][k] += 16
                op.sem = s.dsem[e][k]
                op.count = s.dcnt[e][k]
                s.dlast[e][k] = (op.sem, op.count)
            elif op.has_dep:
                k = s.ek[e] % s.NS
                s.ek[e] += 1
                s.ecnt[e][k] += 1
                op.sem = s.esem[e][k]
                op.count = s.ecnt[e][k]
        per = {e: [o for o in self.ops if o.eng == e] for e in ENGS}

        def run(e, eng):
            waited = s.waited[e]
            for op in per[e]:
                need = {}
                for d in op.deps:
                    key = id(d.sem)
                    if need.get(key, (None, 0))[1] < d.count:
                        need[key] = (d.sem, d.count)
                if op.prev_dma is not None:
                    sem, cnt = op.prev_dma
                    key = id(sem)
                    if need.get(key, (None, 0))[1] < cnt:
                        need[key] = (sem, cnt)
                for key, (sem, cnt) in need.items():
                    if waited.get(key, 0) < cnt:
                        eng.wait_ge(sem, cnt)
                        waited[key] = cnt
                ins = op.fn(eng)
                if op.sem is not None:
                    ins.then_inc(op.sem, 16 if op.is_dma else 1)
            if e in s.dsem:
                for k in range(s.ND):
                    if s.dlast[e][k] is not None:
                        sem, cnt = s.dlast[e][k]
                        if waited.get(id(sem), 0) < cnt:
                            eng.wait_ge(sem, cnt)
                            waited[id(sem)] = cnt

        with nc.Block() as block:
            @block.sync
            def _(eng):
                run("sp", eng)

            @block.tensor
            def _(eng):
                run("pe", eng)

            @block.scalar
            def _(eng):
                run("act", eng)

            @block.vector
            def _(eng):
                run("dve", eng)

            @block.gpsimd
            def _(eng):
                run("pool", eng)
        self.ops = []


T = 8192
D = 1024
NT = T // 128
PADR = 1024
ROWS = PADR + T + PADR
DEPTH = 2
ALPHA = (2 * DEPTH) ** 0.25
LN_EPS = 1e-5
CAP = 768
NSLOT = 32 * CAP
NEGB = -30000.0
B_CFG = ((128, 1), (512, 4), (2048, 16))
QK_AQ, QK_AK = 0, 512
QK_BQ = (640, 1152, 1664)
QK_BK = (896, 1408, 1920)
QK_CQ, QK_CK = 2176, 2688
QK_W = 3200
SEGS = [
    (0, 512, "qk", QK_AQ), (512, 128, "qk", QK_AK), (640, 128, "va", 0),
    (768, 512, "qk", QK_BQ[0]), (1280, 256, "vb", 0),
    (1536, 512, "qk", QK_BQ[1]), (2048, 256, "vb", 1),
    (2304, 512, "qk", QK_BQ[2]), (2816, 256, "vb", 2),
    (3072, 512, "qk", QK_CQ), (3584, 512, "qk", QK_CK), (4096, 512, "vc", 0),
]
GATE0 = 4608


def _t5_bucket(rel):
    half, max_exact = 16, 8
    ret = np.where(rel > 0, half, 0)
    n = np.abs(rel)
    large = max_exact + (np.log(np.maximum(n, max_exact) / max_exact) / np.log(2048 / max_exact) * (half - max_exact)).astype(np.int32)
    large = np.minimum(large, half - 1)
    return (ret + np.where(n < max_exact, n, large)).astype(np.int32)


def host_bias_tables(rel_bias, rpb_c):
    kk = np.arange(128)[:, None]
    qq = np.arange(128)[None, :]
    ba = np.full((128, 2, 3, 4, 128), NEGB, np.float32)
    for c in range(3):
        off = (c - 1) * 128 + kk - qq
        band = np.abs(off) <= 128
        bk = _t5_bucket(off)
        for h in range(8):
            ba[:, h // 4, c, h % 4, :] = np.where(band, rel_bias[bk, h], NEGB)
    bb = np.full((3, 128, 2, 4, 128), NEGB, np.float32)
    for g, (_, dil) in enumerate(B_CFG):
        for jp in range(2):
            off = jp * 128 + kk - 64 - qq
            band = np.abs(off) <= 64
            bk = _t5_bucket(off * dil)
            for h in range(4):
                bb[g, :, jp, h, :] = np.where(band, rel_bias[bk, 8 + 4 * g + h], NEGB)
    bc = np.full((rpb_c.shape[0], 5, 128, 5, 8, 128), NEGB, np.float32)
    for pi, j in enumerate((0, 1, 2, 62, 63)):
        cb0 = min(max(j - 2, 0), 59)
        qtok = j * 128 + np.arange(128)
        qi, qc = qtok // 64, qtok % 64
        rstart = np.clip(qi - 4, 0, 120)
        qstart = np.clip(qc - 8, 0, 48)
        for c in range(5):
            ktok = (cb0 + c) * 128 + np.arange(128)
            kr, kc = ktok // 64, ktok % 64
            valid = ((kr[:, None] >= rstart[None, :]) & (kr[:, None] < rstart[None, :] + 8)
                     & (kc[:, None] >= qstart[None, :]) & (kc[:, None] < qstart[None, :] + 16))
            ridx = np.clip(kr[:, None] - qi[None, :] + 7, 0, 14)
            cidx = np.clip(kc[:, None] - qc[None, :] + 15, 0, 30)
            for l in range(rpb_c.shape[0]):
                for h in range(8):
                    bc[l, pi, :, c, h, :] = np.where(valid, rpb_c[l, h][ridx, cidx], NEGB)
    return ba, bb, bc


def c_pattern(j):
    return {0: 0, 1: 1, 62: 3, 63: 4}.get(j, 2)


def o_mm(ph, out, lhsT, rhs, start, stop, reads, writes):
    return ph.op("pe", lambda e: e.matmul(out, lhsT=lhsT, rhs=rhs, start=start, stop=stop), reads, writes)


def o_tp(ph, out, in_, ident, reads, writes):
    return ph.op("pe", lambda e: e.transpose(out=out, in_=in_, identity=ident), reads, writes)


def o_act(ph, out, in_, func, reads, writes, scale=None, bias=None):
    kw = {}
    if scale is not None:
        kw["scale"] = scale
    if bias is not None:
        kw["bias"] = bias
    return ph.op("act", lambda e: e.activation(out=out, in_=in_, func=func, **kw), reads, writes)


def o_cp(ph, eng, out, in_, reads, writes):
    if eng == "act":
        return ph.op("act", lambda e: e.copy(out=out, in_=in_), reads, writes)
    return ph.op(eng, lambda e: e.tensor_copy(out=out, in_=in_), reads, writes)


def o_tt(ph, eng, out, in0, in1, op, reads, writes):
    return ph.op(eng, lambda e: e.tensor_tensor(out=out, in0=in0, in1=in1, op=op), reads, writes)


def o_ts(ph, eng, out, in0, s1, s2, op0, op1, reads, writes):
    if s2 is None:
        return ph.op(eng, lambda e: e.tensor_scalar(out=out, in0=in0, scalar1=s1, scalar2=None, op0=op0), reads, writes)
    return ph.op(eng, lambda e: e.tensor_scalar(out=out, in0=in0, scalar1=s1, scalar2=s2, op0=op0, op1=op1), reads, writes)


def o_stt(ph, out, in0, scalar, in1, op0, op1, reads, writes):
    return ph.op("dve", lambda e: e.scalar_tensor_tensor(out=out, in0=in0, scalar=scalar, in1=in1, op0=op0, op1=op1), reads, writes)


def o_red(ph, out, in_, op, reads, writes):
    return ph.op("dve", lambda e: e.tensor_reduce(out=out, in_=in_, axis=AX.X, op=op), reads, writes)


def o_rcp(ph, out, in_, reads, writes):
    return ph.op("dve", lambda e: e.reciprocal(out=out, in_=in_), reads, writes)


def o_memset(ph, eng, ap, val, writes):
    return ph.op(eng, lambda e: e.memset(ap, val), (), writes)


class Ring:
    def __init__(self, ph, tiles, name):
        self.tiles = tiles
        self.bufs = [ph.buf(f"{name}{i}") for i in range(len(tiles))]
        self.k = 0

    def next(self):
        i = self.k % len(self.tiles)
        self.k += 1
        return self.tiles[i], self.bufs[i]


class Ctx:
    pass


def ln_s1(ph, C, z, zb):
    st, stb = C.ln_st.next()
    mv, mvb = C.ln_mv.next()
    ph.op("dve", lambda e: e.bn_stats(out=st[:, 0, :], in_=z[:, 0:512]), [zb], [stb])
    ph.op("dve", lambda e: e.bn_stats(out=st[:, 1, :], in_=z[:, 512:1024]), [zb, stb], [stb])
    ph.op("dve", lambda e: e.bn_aggr(out=mv[:, 0:2], in_=st[:].rearrange("p a s -> p (a s)")), [stb], [mvb])
    return mv, mvb


def ln_s2(ph, C, mv, mvb):
    o_act(ph, mv[:, 2:3], mv[:, 1:2], AF.Sqrt, [mvb], [mvb], bias=C.eps[:, 0:1], scale=1.0)
    o_rcp(ph, mv[:, 3:4], mv[:, 2:3], [mvb], [mvb])
    o_ts(ph, "dve", mv[:, 4:5], mv[:, 0:1], mv[:, 3:4], -1.0, ALU.mult, ALU.mult, [mvb], [mvb])


def ln_s3(ph, C, z, zb, mv, mvb, out, outb, g, b, cb, geng="pool", beng="pool"):
    o_act(ph, out, z, AF.Identity, [zb, mvb], [outb], scale=mv[:, 3:4], bias=mv[:, 4:5])
    o_tt(ph, geng, out, out, g, ALU.mult, [outb, cb], [outb])
    o_tt(ph, beng, out, out, b, ALU.add, [outb, cb], [outb])


def ln_tile(ph, C, z, zb, out, outb, g, b, cb, geng="pool", beng="pool"):
    mv, mvb = ln_s1(ph, C, z, zb)
    ln_s2(ph, C, mv, mvb)
    ln_s3(ph, C, z, zb, mv, mvb, out, outb, g, b, cb, geng, beng)


def ln_setup(ph, C, A):
    C.ln_st = Ring(ph, [A(f"lnst{i}", [128, 2, 6], F32) for i in range(6)], "lnst")
    C.ln_mv = Ring(ph, [A(f"lnmv{i}", [128, 8], F32) for i in range(6)], "lnmv")


def load_gb(ph, A, gsrc, bsrc, name):
    g = A(name + "g", [128, D], F32)
    b = A(name + "b", [128, D], F32)
    cb = ph.buf(name)
    ph.dma("sp", g[:], gsrc.to_broadcast([128, D]), writes=[cb])
    ph.dma("sp", b[:], bsrc.to_broadcast([128, D]), writes=[cb])
    return g, b, cb


def mk_alloc(nc, st, prefix):
    def A(name, shape, dt):
        return st.enter_context(nc.sbuf_tensor(prefix + name, list(shape), dt))

    def P(name, shape, dt):
        return st.enter_context(nc.psum_tensor(prefix + name, list(shape), dt))
    return A, P


def phase_ln0(C):
    from contextlib import ExitStack
    nc = C.nc
    with ExitStack() as st:
        A, P = mk_alloc(nc, st, "p0_")
        ph = Phase(C.S, "ln0")
        ln_setup(ph, C, A)
        g, b, cb = load_gb(ph, A, C.ln0g, C.ln0b, "ln0")
        zr = Ring(ph, [A(f"z{i}", [128, D], F32) for i in range(3)], "z")
        orr = Ring(ph, [A(f"o{i}", [128, D], F32) for i in range(3)], "o")
        nxt = zr.next()
        ph.dma("sp", nxt[0][:], C.x[0:128, :], writes=[nxt[1]])
        for i in range(NT):
            z, zb = nxt
            if i + 1 < NT:
                nxt = zr.next()
                ph.dma("sp", nxt[0][:], C.x[(i + 1) * 128:(i + 2) * 128, :], writes=[nxt[1]])
            o, ob = orr.next()
            ln_tile(ph, C, z[:], zb, o[:], ob, g[:], b[:], cb, geng="dve", beng="pool")
            ph.dma("sp", C.H[i * 128:(i + 1) * 128, :], o[:], reads=[ob])
        ph.emit()


def phase_proj(C, l):
    from contextlib import ExitStack
    nc = C.nc
    with ExitStack() as st:
        A, P = mk_alloc(nc, st, f"p1_{l}_")
        ph = Phase(C.S, f"proj{l}")
        w = A("w", [128, 8, 7680], BF16)
        wb = ph.buf("w")
        wstr = Ring(ph, [A(f"wst{i}", [128, 1536], F32) for i in range(2)], "wst")
        wbs = []
        for k in range(8):
            for cbk in range(5):
                src = C.w_in[l, k * 128:(k + 1) * 128, cbk * 1536:(cbk + 1) * 1536]
                wbc = ph.buf(f"w{k}_{cbk}")
                wbs.append(wbc)
                if (k * 5 + cbk) % 3 == 2:
                    wst, wstb = wstr.next()
                    ph.dma("sp", wst[:], src, writes=[wstb])
                    o_cp(ph, "pool", w[:, k, cbk * 1536:(cbk + 1) * 1536], wst[:], [wstb], [wbc])
                else:
                    ph.dma("pool", w[:, k, cbk * 1536:(cbk + 1) * 1536], src, writes=[wbc])
        bg = A("bg", [128, 24], F32)
        ph.dma("sp", bg[:], C.bgT[l], writes=[wb])
        hin = Ring(ph, [A(f"hin{i}", [128, D], F32) for i in range(2)], "hin")
        hT_t = [A(f"hT{i}", [128, 8, 512], BF16) for i in range(2)]
        hT_b = [[ph.buf(f"hT{i}_{s_}") for s_ in range(4)] for i in range(2)]
        qks = Ring(ph, [A(f"qks{i}", [128, QK_W], BF16) for i in range(2)], "qks")
        vas_t = [A(f"vas{i}", [128, 2, 65], BF16) for i in range(2)]
        vbs_t = [A(f"vbs{i}", [128, 3, 4, 65], BF16) for i in range(2)]
        vcs_t = [A(f"vcs{i}", [128, 8, 65], BF16) for i in range(2)]
        vas = Ring(ph, vas_t, "vas")
        vbs = Ring(ph, vbs_t, "vbs")
        vcs = Ring(ph, vcs_t, "vcs")
        for i in range(2):
            o_memset(ph, "pool", vas_t[i][:], 1.0, [vas.bufs[i]])
            o_memset(ph, "pool", vbs_t[i][:], 1.0, [vbs.bufs[i]])
            o_memset(ph, "pool", vcs_t[i][:], 1.0, [vcs.bufs[i]])
        gst = Ring(ph, [A(f"gst{i}", [128, 6, 512], BF16) for i in range(2)], "gst")
        tpr = Ring(ph, [P(f"tp{i}", [128, 8, 128], F32) for i in range(2)], "tp")
        mmr = Ring(ph, [P(f"mm{i}", [128, 512], F32) for i in range(4)], "mm")
        GTv = C.GT
        ev = 0
        nxt = hin.next()
        ph.dma("sp", nxt[0][:], C.H[0:128, :], writes=[nxt[1]])
        for n in range(T // 512):
            ht, htbs = hT_t[n % 2], hT_b[n % 2]
            for sub in range(4):
                htb = htbs[sub]
                i = 4 * n + sub
                h_in, hib = nxt
                if i + 1 < NT:
                    nxt = hin.next()
                    ph.dma("sp", nxt[0][:], C.H[(i + 1) * 128:(i + 2) * 128, :], writes=[nxt[1]])
                tp, tpb = tpr.next()
                for k in range(8):
                    o_tp(ph, tp[:, k, :], h_in[:, k * 128:(k + 1) * 128], C.identf[:], [hib], [tpb])
                o_cp(ph, "dve", ht[:, :, sub * 128:(sub + 1) * 128], tp[:], [tpb], [htb])
                qk, qkb = qks.next()
                va, vab = vas.next()
                vb, vbb = vbs.next()
                vc, vcb = vcs.next()
                for (c0, wd, kind, dst) in SEGS:
                    mm, mmb = mmr.next()
                    for k in range(8):
                        o_mm(ph, mm[:, 0:wd], ht[:, k, sub * 128:(sub + 1) * 128], w[:, k, c0:c0 + wd],
                             k == 0, k == 7, [htb, wb] + wbs, [mmb])
                    eng = "dve" if ev % 2 == 0 else "act"
                    ev += 1
                    if kind == "qk":
                        o_cp(ph, eng, qk[:, dst:dst + wd], mm[:, 0:wd], [mmb], [qkb])
                    elif kind == "va":
                        o_cp(ph, eng, va[:, :, 0:64], mm[:, 0:128].rearrange("p (h d) -> p h d", h=2), [mmb], [vab])
                    elif kind == "vb":
                        o_cp(ph, eng, vb[:, dst, :, 0:64], mm[:, 0:256].rearrange("p (h d) -> p h d", h=4), [mmb], [vbb])
                    else:
                        o_cp(ph, eng, vc[:, :, 0:64], mm[:, 0:512].rearrange("p (h d) -> p h d", h=8), [mmb], [vcb])
                r0 = PADR + i * 128
                ph.dma("sp", C.QK[r0:r0 + 128, :], qk[:], reads=[qkb])
                ph.dma("sp", C.VA[r0:r0 + 128, :], va[:].rearrange("p h d -> p (h d)"), reads=[vab])
                for gi in range(3):
                    ph.dma("sp", C.VB[gi][r0:r0 + 128, :], vb[:, gi, :, :].rearrange("p h d -> p (h d)"), reads=[vbb])
                ph.dma("sp", C.VC[r0:r0 + 128, :], vc[:].rearrange("p h d -> p (h d)"), reads=[vcb])
            for cg in range(4):
                gs, gsb = gst.next()
                for a in range(6):
                    c = cg * 6 + a
                    mm, mmb = mmr.next()
                    for k in range(8):
                        o_mm(ph, mm[:], w[:, k, GATE0 + c * 128:GATE0 + (c + 1) * 128], ht[:, k, :],
                             k == 0, k == 7, htbs + [wb] + wbs, [mmb])
                    o_act(ph, gs[:, a, :], mm[:], AF.Sigmoid, [mmb, wb], [gsb], bias=bg[:, c:c + 1], scale=1.0)
                ph.dma("sp", GTv[cg * 768:(cg + 1) * 768, n * 512:(n + 1) * 512].rearrange("(a p) t -> p a t", p=128),
                       gs[:], reads=[gsb])
        ph.emit()


def phase_attn_a(C, l):
    from contextlib import ExitStack
    nc = C.nc
    with ExitStack() as st:
        A, P = mk_alloc(nc, st, f"pa_{l}_")
        ph = Phase(C.S, f"attnA{l}")
        es = A("es", [128, 8], F32)
        esb = ph.buf("es")
        ph.dma("sp", es[:], C.sink[l].to_broadcast([128, 8]), writes=[esb])
        o_act(ph, es[:], es[:], AF.Exp, [esb], [esb])
        qr = Ring(ph, [A(f"q{i}", [128, 512], BF16) for i in range(2)], "q")
        kdr = Ring(ph, [A(f"kd{i}", [128, 3, 2, 2, 64], BF16) for i in range(2)], "kd")
        vr = Ring(ph, [A(f"v{i}", [128, 3, 130], BF16) for i in range(2)], "v")
        qTr = Ring(ph, [A(f"qT{i}", [128, 4, 128], BF16) for i in range(2)], "qT")
        kl_t = [A(f"kTl{i}", [128, 6, 128], BF16) for i in range(2)]
        kh_t = [A(f"kTh{i}", [128, 6, 128], BF16) for i in range(2)]
        klr = Ring(ph, kl_t, "kTl")
        khr = Ring(ph, kh_t, "kTh")
        for i in range(2):
            o_memset(ph, "pool", kl_t[i][:], 0.0, [klr.bufs[i]])
            o_memset(ph, "pool", kh_t[i][:], 0.0, [khr.bufs[i]])
        ptr = Ring(ph, [A(f"pt{i}", [128, 1536], BF16) for i in range(2)], "pt")
        oar = Ring(ph, [A(f"oa{i}", [128, 512], BF16) for i in range(2)], "oa")
        dnr = Ring(ph, [A(f"dn{i}", [128, 8], F32) for i in range(2)], "dn")
        otr = Ring(ph, [A(f"ot{i}", [128, 4, 512], BF16) for i in range(2)], "ot")
        tpq = Ring(ph, [P("tpq", [128, 1024], BF16)], "tpq")
        tpk = Ring(ph, [P("tpk", [128, 1024], BF16)], "tpk")
        psr = Ring(ph, [P("ps", [128, 1536], F32)], "ps")
        por = Ring(ph, [P(f"po{i}", [128, 512], F32) for i in range(2)], "po")
        OTv = C.OT[0:512, :].rearrange("(i p) t -> p i t", p=128)

        def loads(b):
            q, qb = qr.next()
            kd, kdb = kdr.next()
            v, vb = vr.next()
            r0 = PADR + 128 * b
            ph.dma("sp", q[:], C.QK[r0:r0 + 128, QK_AQ:QK_AQ + 512], writes=[qb])
            for g in range(2):
                ksrc = C.QK[r0 - 128:r0 + 256, QK_AK + 64 * g:QK_AK + 64 * g + 64].rearrange("(c p) d -> p c d", p=128)
                for a in range(2):
                    ph.dma("sp", kd[:, :, g, a, :], ksrc, writes=[kdb])
            ph.dma("sp", v[:], C.VA[r0 - 128:r0 + 256, :].rearrange("(c p) d -> p c d", p=128), writes=[vb])
            return (q, qb, kd, kdb, v, vb)

        nxt = loads(0)
        ot, otb = None, None
        for b in range(NT):
            q, qb, kd, kdb, v, vb = nxt
            if b + 1 < NT:
                nxt = loads(b + 1)
            tq, tqb = tpq.next()
            for i in range(4):
                o_tp(ph, tq[:, i * 128:(i + 1) * 128], q[:, i * 128:(i + 1) * 128], C.identb[:], [qb], [tqb])
            qT, qTb = qTr.next()
            o_cp(ph, "dve", qT[:].rearrange("p i t -> p (i t)"), tq[:, 0:512], [tqb], [qTb])
            tk, tkb = tpk.next()
            for c in range(3):
                for g in range(2):
                    o_tp(ph, tk[:, (c * 2 + g) * 128:(c * 2 + g + 1) * 128],
                         kd[:, c, g, :, :].rearrange("p a d -> p (a d)"), C.identb[:], [kdb], [tkb])
            kl, klb = klr.next()
            kh, khb = khr.next()
            o_cp(ph, "act", kl[0:64, :, :].rearrange("p s t -> p (s t)"), tk[0:64, 0:768], [tkb], [klb])
            o_cp(ph, "dve", kh[64:128, :, :].rearrange("p s t -> p (s t)"), tk[64:128, 0:768], [tkb], [khb])
            oa, oab = oar.next()
            dn, dnb = dnr.next()
            for g in range(2):
                ps, psb = psr.next()
                for c in range(3):
                    for j in range(4):
                        h = 4 * g + j
                        i, par = h // 2, h % 2
                        kk_, kkb = (kl, klb) if par == 0 else (kh, khb)
                        o_mm(ph, ps[:, (c * 4 + j) * 128:(c * 4 + j + 1) * 128], kk_[:, c * 2 + g, :],
                             qT[:, i, :], True, True, [kkb, qTb], [psb])
                pt, ptb = ptr.next()
                for c in range(3):
                    o_act(ph, pt[:, c * 512:(c + 1) * 512], ps[:, c * 512:(c + 1) * 512], AF.Exp, [psb], [ptb], scale=0.125)
                o_tt(ph, "dve", pt[:], pt[:], C.EA[:, g * 1536:(g + 1) * 1536], ALU.mult, [ptb], [ptb])
                po, pob = por.next()
                for j in range(4):
                    for c in range(3):
                        o_mm(ph, po[:, j * 65:(j + 1) * 65], pt[:, (c * 4 + j) * 128:(c * 4 + j + 1) * 128],
                             v[:, c, g * 65:(g + 1) * 65], c == 0, c == 2, [ptb, vb], [pob])
                pov = po[:, 0:260].rearrange("p (j d) -> p j d", j=4)
                o_tt(ph, "dve", dn[:, g * 4:(g + 1) * 4], pov[:, :, 64], es[:, g * 4:(g + 1) * 4], ALU.add, [pob, esb], [dnb])
                o_rcp(ph, dn[:, g * 4:(g + 1) * 4], dn[:, g * 4:(g + 1) * 4], [dnb], [dnb])
                o_tt(ph, "dve", oa[:, g * 256:(g + 1) * 256].rearrange("p (j d) -> p j d", j=4), pov[:, :, 0:64],
                     dn[:, g * 4:(g + 1) * 4].unsqueeze(2).to_broadcast([128, 4, 64]), ALU.mult, [pob, dnb], [oab])
            to, tob = tpq.next()
            for i in range(4):
                o_tp(ph, to[:, 512 + i * 128:512 + (i + 1) * 128], oa[:, i * 128:(i + 1) * 128], C.identb[:], [oab], [tob])
            if b % 4 == 0:
                ot, otb = otr.next()
            o_cp(ph, "act", ot[:, :, (b % 4) * 128:(b % 4 + 1) * 128], to[:, 512:1024].rearrange("p (i t) -> p i t", i=4), [tob], [otb])
            if b % 4 == 3:
                ph.dma("sp", OTv[:, :, (b // 4) * 512:(b // 4 + 1) * 512], ot[:], reads=[otb])
        ph.emit()


def phase_attn_b(C, l, gi):
    from contextlib import ExitStack
    nc = C.nc
    dil = B_CFG[gi][1]
    Ls = T // dil
    nb = Ls // 128
    NBK = 4
    with ExitStack() as st:
        A, P = mk_alloc(nc, st, f"pb_{l}_{gi}_")
        q_t = [A(f"q{i}", [128, 256], BF16) for i in range(NBK)]
        k_t = [A(f"k{i}", [128, 2, 256], BF16) for i in range(NBK)]
        v_t = [A(f"v{i}", [128, 2, 260], BF16) for i in range(NBK)]
        qT_t = [A(f"qT{i}", [128, 2, 128], BF16) for i in range(2)]
        kl_t = [A(f"kTl{i}", [128, 4, 128], BF16) for i in range(2)]
        kh_t = [A(f"kTh{i}", [128, 4, 128], BF16) for i in range(2)]
        pt_t = [A(f"pt{i}", [128, 1024], BF16) for i in range(NBK)]
        sb_t = [A(f"sb{i}", [128, 1024], F32) for i in range(2)]
        nb_t = [A(f"nb{i}", [128, 260], F32) for i in range(NBK)]
        tp_t = [P(f"tp{i}", [128, 1024], BF16) for i in range(2)]
        ps_t = [P("ps", [128, 1024], F32)]
        po_t = [P(f"po{i}", [128, 512], F32) for i in range(NBK)]
        QKv = C.QK.rearrange("(s d) c -> d s c", d=dil)
        VBv = C.VB[gi].rearrange("(s d) c -> d s c", d=dil)
        NBv = C.NB[gi].rearrange("(s d) c -> d s c", d=dil)
        sp0 = PADR // dil
        ph = Phase(C.S, f"attnB{l}_{gi}_init")
        zb = ph.buf("z")
        for i in range(2):
            o_memset(ph, "pool", kl_t[i][:], 0.0, [zb])
            o_memset(ph, "pool", kh_t[i][:], 0.0, [zb])
        ph.emit()
        blocks = [(r, b) for r in range(dil) for b in range(nb)]
        for g0 in range(0, len(blocks), NBK):
            grp = blocks[g0:g0 + NBK]
            ph = Phase(C.S, f"attnB{l}_{gi}_{g0}")
            tpr = Ring(ph, tp_t, "tp")
            psr = Ring(ph, ps_t, "ps")
            qTr = Ring(ph, qT_t, "qT")
            klr = Ring(ph, kl_t, "kl")
            khr = Ring(ph, kh_t, "kh")
            sbr = Ring(ph, sb_t, "sb")
            ld = []
            for i, (r, b) in enumerate(grp):
                qb, kb, vb = ph.buf(), ph.buf(), ph.buf()
                s0 = sp0 + 128 * b
                ph.dma("sp", q_t[i][:], QKv[r, s0:s0 + 128, QK_BQ[gi]:QK_BQ[gi] + 256], writes=[qb])
                ph.dma("sp", k_t[i][:], QKv[r, s0 - 64:s0 + 192, QK_BK[gi]:QK_BK[gi] + 256].rearrange("(j p) d -> p j d", p=128), writes=[kb])
                ph.dma("sp", v_t[i][:], VBv[r, s0 - 64:s0 + 192, :].rearrange("(j p) d -> p j d", p=128), writes=[vb])
                ld.append((qb, kb, vb))
            ptbs = []
            for i, (r, b) in enumerate(grp):
                q, k = q_t[i], k_t[i]
                qb, kb, vb = ld[i]
                tp, tpb = tpr.next()
                for a in range(2):
                    o_tp(ph, tp[:, a * 128:(a + 1) * 128], q[:, a * 128:(a + 1) * 128], C.identb[:], [qb], [tpb])
                for jp in range(2):
                    for a in range(2):
                        sl = 2 + 2 * jp + a
                        o_tp(ph, tp[:, sl * 128:(sl + 1) * 128], k[:, jp, a * 128:(a + 1) * 128], C.identb[:], [kb], [tpb])
                qT, qTb = qTr.next()
                kl, klb = klr.next()
                kh, khb = khr.next()
                o_cp(ph, "dve", qT[:].rearrange("p s t -> p (s t)"), tp[:, 0:256], [tpb], [qTb])
                o_cp(ph, "act", kl[0:64, :, :].rearrange("p s t -> p (s t)"), tp[0:64, 256:768], [tpb], [klb])
                o_cp(ph, "dve", kh[64:128, :, :].rearrange("p s t -> p (s t)"), tp[64:128, 256:768], [tpb], [khb])
                ps, psb = psr.next()
                for jp in range(2):
                    for h in range(4):
                        a, par = h // 2, h % 2
                        kk_, kkb = (kl, klb) if par == 0 else (kh, khb)
                        o_mm(ph, ps[:, (jp * 4 + h) * 128:(jp * 4 + h + 1) * 128], kk_[:, 2 * jp + a, :],
                             qT[:, a, :], True, True, [kkb, qTb], [psb])
                sb_, sbb = sbr.next()
                for jp in range(2):
                    o_stt(ph, sb_[:, jp * 512:(jp + 1) * 512], ps[:, jp * 512:(jp + 1) * 512], 0.125,
                          C.BB[:, gi * 1024 + jp * 512:gi * 1024 + (jp + 1) * 512], ALU.mult, ALU.add, [psb], [sbb])
                ptb = ph.buf()
                o_act(ph, pt_t[i][:], sb_[:], AF.Exp, [sbb], [ptb])
                ptbs.append(ptb)
            pobs = []
            for i, (r, b) in enumerate(grp):
                pt, v, po = pt_t[i], v_t[i], po_t[i]
                pob = ph.buf()
                for h in range(4):
                    for jp in range(2):
                        o_mm(ph, po[:, h * 65:(h + 1) * 65], pt[:, (jp * 4 + h) * 128:(jp * 4 + h + 1) * 128],
                             v[:, jp, h * 65:(h + 1) * 65], jp == 0, jp == 1, [ptbs[i], ld[i][2]], [pob])
                pobs.append(pob)
            for i, (r, b) in enumerate(grp):
                nbb = ph.buf()
                o_cp(ph, "act" if i % 2 == 0 else "dve", nb_t[i][:], po_t[i][:, 0:260], [pobs[i]], [nbb])
                ph.dma("sp", NBv[r, 128 * b:128 * (b + 1), :], nb_t[i][:], reads=[nbb])
            ph.emit()


def phase_attn_bc(C, l):
    from contextlib import ExitStack
    nc = C.nc
    with ExitStack() as st:
        A, P = mk_alloc(nc, st, f"pbc_{l}_")
        ph = Phase(C.S, f"attnBc{l}")
        nr = [Ring(ph, [A(f"n{g}_{i}", [128, 260], F32) for i in range(2)], f"n{g}") for g in range(3)]
        dnr = Ring(ph, [A(f"dn{i}", [128, 4], F32) for i in range(2)], "dn")
        obr = Ring(ph, [A(f"ob{i}", [128, 256], BF16) for i in range(2)], "ob")
        otr = Ring(ph, [A(f"ot{i}", [128, 2, 512], BF16) for i in range(2)], "ot")
        tpr = Ring(ph, [P(f"tp{i}", [128, 1024], BF16) for i in range(2)], "tp")
        OTv = C.OT[512:768, :].rearrange("(i p) t -> p i t", p=128)

        def loads(i):
            res = []
            for g in range(3):
                n, nb_ = nr[g].next()
                ph.dma("sp", n[:], C.NB[g][i * 128:(i + 1) * 128, :], writes=[nb_])
                res.append((n, nb_))
            return res

        nxt = loads(0)
        ot, otb = None, None
        for i in range(NT):
            (n0, b0), (n1, b1), (n2, b2) = nxt
            if i + 1 < NT:
                nxt = loads(i + 1)
            o_tt(ph, "pool", n0[:], n0[:], n1[:], ALU.add, [b0, b1], [b0])
            o_tt(ph, "pool", n0[:], n0[:], n2[:], ALU.add, [b0, b2], [b0])
            nv = n0[:].rearrange("p (h d) -> p h d", h=4)
            dn, dnb = dnr.next()
            o_rcp(ph, dn[:], nv[:, :, 64], [b0], [dnb])
            ob, obb = obr.next()
            o_tt(ph, "dve", ob[:].rearrange("p (h d) -> p h d", h=4), nv[:, :, 0:64],
                 dn[:].unsqueeze(2).to_broadcast([128, 4, 64]), ALU.mult, [b0, dnb], [obb])
            tp, tpb = tpr.next()
            for a in range(2):
                o_tp(ph, tp[:, a * 128:(a + 1) * 128], ob[:, a * 128:(a + 1) * 128], C.identb[:], [obb], [tpb])
            if i % 4 == 0:
                ot, otb = otr.next()
            o_cp(ph, "act", ot[:, :, (i % 4) * 128:(i % 4 + 1) * 128], tp[:, 0:256].rearrange("p (a t) -> p a t", a=2), [tpb], [otb])
            if i % 4 == 3:
                ph.dma("sp", OTv[:, :, (i // 4) * 512:(i // 4 + 1) * 512], ot[:], reads=[otb])
        ph.emit()


def phase_attn_c(C, l):
    from contextlib import ExitStack
    nc = C.nc
    NTL = 2
    with ExitStack() as st:
        A, P = mk_alloc(nc, st, f"pc_{l}_")
        EC = A("EC", [128, 5, 5120], BF16)
        tmp = A("ect", [128, 5120], F32)
        q_t = [A(f"q{i}", [128, 512], BF16) for i in range(NTL)]
        k_t = [A(f"k{i}", [128, 5, 512], BF16) for i in range(NTL)]
        v_t = [A(f"v{i}", [128, 5, 520], BF16) for i in range(NTL)]
        qT_t = [A(f"qT{i}", [128, 4, 128], BF16) for i in range(NTL)]
        kl_t = [A(f"kTl{i}", [128, 5, 4, 128], BF16) for i in range(NTL)]
        kh_t = [A(f"kTh{i}", [128, 5, 4, 128], BF16) for i in range(NTL)]
        C_pt = [A(f"pt{i}", [128, 640], BF16) for i in range(8 * NTL)]
        oc_t = [A(f"oc{i}", [128, 512], BF16) for i in range(NTL)]
        dn_t = [A(f"dn{i}", [128, 8], F32) for i in range(NTL)]
        tps = [P("tp0", [128, 1024], BF16)]
        psx = P("psx", [128, 1536], F32)
        po_t = [P(f"po{i}", [128, 512], F32) for i in range(2 * NTL)]
        OTv = C.OT[768:1280, :].rearrange("(i p) t -> p i t", p=128)
        ph = Phase(C.S, f"attnC{l}_init")
        zb = ph.buf("z")
        for i in range(NTL):
            o_memset(ph, "pool", kl_t[i][:], 0.0, [zb])
            o_memset(ph, "pool", kh_t[i][:], 0.0, [zb])
        ecb = ph.buf("EC")
        tb = ph.buf("tmp")
        for pi in range(5):
            ph.dma("sp", tmp[:], C.biasC[l, pi], writes=[tb])
            o_act(ph, EC[:, pi, :], tmp[:], AF.Exp, [tb], [ecb])
        ph.emit()
        for j0 in range(0, NT, NTL):
            ph = Phase(C.S, f"attnC{l}_{j0}")
            tpr = Ring(ph, tps, "tp")
            ps_b = [ph.buf("psA"), ph.buf("psB")]
            ps_v = [psx[:, 0:640], psx[:, 768:1408]]
            psk = 0
            info = []
            for t in range(NTL):
                j = j0 + t
                qb, kb, vb = ph.buf(), ph.buf(), ph.buf()
                cb0 = min(max(j - 2, 0), 59)
                r0 = PADR + 128 * j
                k0 = PADR + 128 * cb0
                ph.dma("sp", q_t[t][:], C.QK[r0:r0 + 128, QK_CQ:QK_CQ + 512], writes=[qb])
                ph.dma("sp", k_t[t][:], C.QK[k0:k0 + 640, QK_CK:QK_CK + 512].rearrange("(c p) d -> p c d", p=128), writes=[kb])
                ph.dma("sp", v_t[t][:], C.VC[k0:k0 + 640, :].rearrange("(c p) d -> p c d", p=128), writes=[vb])
                info.append((j, qb, kb, vb))
            ptbs = {}
            for t in range(NTL):
                j, qb, kb, vb = info[t]
                q, k, qT, kl, kh = q_t[t], k_t[t], qT_t[t], kl_t[t], kh_t[t]
                qTb, klb, khb = ph.buf(), ph.buf(), ph.buf()
                pat = c_pattern(j)
                tp, tpb = tpr.next()
                for i in range(4):
                    o_tp(ph, tp[:, i * 128:(i + 1) * 128], q[:, i * 128:(i + 1) * 128], C.identb[:], [qb], [tpb])
                    o_tp(ph, tp[:, (4 + i) * 128:(5 + i) * 128], k[:, 0, i * 128:(i + 1) * 128], C.identb[:], [kb], [tpb])
                o_cp(ph, "dve", qT[:].rearrange("p i t -> p (i t)"), tp[:, 0:512], [tpb], [qTb])
                o_cp(ph, "act", kl[0:64, 0, :, :].rearrange("p i t -> p (i t)"), tp[0:64, 512:1024], [tpb], [klb])
                o_cp(ph, "dve", kh[64:128, 0, :, :].rearrange("p i t -> p (i t)"), tp[64:128, 512:1024], [tpb], [khb])
                for f in range(2):
                    tp, tpb = tpr.next()
                    for cc in range(2):
                        c = 1 + 2 * f + cc
                        for i in range(4):
                            o_tp(ph, tp[:, (cc * 4 + i) * 128:(cc * 4 + i + 1) * 128], k[:, c, i * 128:(i + 1) * 128], C.identb[:], [kb], [tpb])
                    o_cp(ph, "act", kl[0:64, 1 + 2 * f:3 + 2 * f, :, :].rearrange("p c i t -> p (c i t)"), tp[0:64, :], [tpb], [klb])
                    o_cp(ph, "dve", kh[64:128, 1 + 2 * f:3 + 2 * f, :, :].rearrange("p c i t -> p (c i t)"), tp[64:128, :], [tpb], [khb])
                for h in range(8):
                    i, par = h // 2, h % 2
                    kk_, kkb = (kl, klb) if par == 0 else (kh, khb)
                    ps, psb = ps_v[psk % 2], ps_b[psk % 2]
                    psk += 1
                    for c in range(5):
                        o_mm(ph, ps[:, c * 128:(c + 1) * 128], kk_[:, c, i, :], qT[:, i, :], True, True, [kkb, qTb], [psb])
                    pt = C_pt[t * 8 + h]
                    ptb = ph.buf()
                    ptbs[(t, h)] = ptb
                    o_act(ph, pt[:], ps, AF.Exp, [psb], [ptb], scale=0.125)
                    o_tt(ph, "dve", pt[:].rearrange("p (c t) -> p c t", c=5), pt[:].rearrange("p (c t) -> p c t", c=5),
                         EC[:, pat, :].rearrange("p (c h t) -> p c h t", c=5, h=8)[:, :, h, :], ALU.mult, [ptb], [ptb])
            po_b = {}
            for t in range(NTL):
                j, qb, kb, vb = info[t]
                for h in range(8):
                    pt, ptb = C_pt[t * 8 + h], ptbs[(t, h)]
                    a = t * 2 + h // 4
                    if a not in po_b:
                        po_b[a] = ph.buf()
                    po, pob = po_t[a], po_b[a]
                    for c in range(5):
                        o_mm(ph, po[:, (h % 4) * 65:(h % 4 + 1) * 65], pt[:, c * 128:(c + 1) * 128],
                             v_t[t][:, c, h * 65:(h + 1) * 65], c == 0, c == 4, [ptb, vb], [pob])
            for t in range(NTL):
                j = info[t][0]
                oc, dn = oc_t[t], dn_t[t]
                ocb, dnb = ph.buf(), ph.buf()
                for a2 in range(2):
                    a = t * 2 + a2
                    pov = po_t[a][:, 0:260].rearrange("p (j d) -> p j d", j=4)
                    o_rcp(ph, dn[:, a2 * 4:(a2 + 1) * 4], pov[:, :, 64], [po_b[a]], [dnb])
                    o_tt(ph, "dve", oc[:, a2 * 256:(a2 + 1) * 256].rearrange("p (j d) -> p j d", j=4), pov[:, :, 0:64],
                         dn[:, a2 * 4:(a2 + 1) * 4].unsqueeze(2).to_broadcast([128, 4, 64]), ALU.mult, [po_b[a], dnb], [ocb])
                ph.dma("sp", C.OC[j * 128:(j + 1) * 128, :], oc[:], reads=[ocb])
            ph.emit()
        ph = Phase(C.S, f"attnC{l}_tr")
        ocr = Ring(ph, [A(f"oc2_{i}", [128, 512], BF16) for i in range(2)], "oc2")
        otr = Ring(ph, [A(f"ot2_{i}", [128, 4, 512], BF16) for i in range(2)], "ot2")
        tpr = Ring(ph, [tps[0]], "tp")
        nxt = ocr.next()
        ph.dma("sp", nxt[0][:], C.OC[0:128, :], writes=[nxt[1]])
        ot2, ot2b = None, None
        for j in range(NT):
            oc2, oc2b = nxt
            if j + 1 < NT:
                nxt = ocr.next()
                ph.dma("sp", nxt[0][:], C.OC[(j + 1) * 128:(j + 2) * 128, :], writes=[nxt[1]])
            tp, tpb = tpr.next()
            for i in range(4):
                o_tp(ph, tp[:, i * 128:(i + 1) * 128], oc2[:, i * 128:(i + 1) * 128], C.identb[:], [oc2b], [tpb])
            if j % 4 == 0:
                ot2, ot2b = otr.next()
            o_cp(ph, "act", ot2[:, :, (j % 4) * 128:(j % 4 + 1) * 128], tp[:, 0:512].rearrange("p (i t) -> p i t", i=4), [tpb], [ot2b])
            if j % 4 == 3:
                ph.dma("sp", OTv[:, :, (j // 4) * 512:(j // 4 + 1) * 512], ot2[:], reads=[ot2b])
        ph.emit()


def phase_merge(C, l):
    from contextlib import ExitStack
    nc = C.nc
    with ExitStack() as st:
        A, P = mk_alloc(nc, st, f"p3_{l}_")
        ph = Phase(C.S, f"merge{l}")
        ln_setup(ph, C, A)
        g, b, cb = load_gb(ph, A, C.ln1g[l], C.ln1b[l], "ln1")
        wbr = A("wbr", [128, 10, 1024], BF16)
        wo = A("wo", [128, 8, 1024], BF16)
        wb = ph.buf("w")
        srcs = [(C.w_bra[l], 4), (C.w_brb[l], 2), (C.w_brc[l], 4)]
        kk = 0
        wbs = []
        for src, nk in srcs:
            for k in range(nk):
                wbc = ph.buf()
                wbs.append(wbc)
                ph.dma("pool", wbr[:, kk, :], src[k * 128:(k + 1) * 128, :], writes=[wbc])
                kk += 1
        for k in range(8):
            wbc = ph.buf()
            wbs.append(wbc)
            ph.dma("pool", wo[:, k, :], C.w_out[l, k * 128:(k + 1) * 128, :], writes=[wbc])
        otr = Ring(ph, [A(f"ot{i}", [128, 10, 512], BF16) for i in range(2)], "ot")
        gtr = Ring(ph, [A(f"gt{i}", [128, 24, 512], BF16) for i in range(2)], "gt")
        t1r = Ring(ph, [A(f"t1_{i}", [128, 512], F32) for i in range(2)], "t1")
        t2r = Ring(ph, [A(f"t2_{i}", [128, 512], F32) for i in range(2)], "t2")
        t3r = Ring(ph, [A(f"t3_{i}", [128, 512], F32) for i in range(2)], "t3")
        mgr = Ring(ph, [A(f"mg{i}", [128, 8, 512], BF16) for i in range(2)], "mg")
        hr = Ring(ph, [A(f"h{i}", [128, D], F32) for i in range(2)], "h")
        zr = Ring(ph, [A(f"z{i}", [128, D], F32) for i in range(5)], "z")
        orr = Ring(ph, [A(f"o{i}", [128, D], F32) for i in range(3)], "o")
        obr = Ring(ph, [A(f"ob{i}", [128, D], BF16) for i in range(2)], "ob")
        par = Ring(ph, [P(f"pa{i}", [128, 512], F32) for i in range(2)], "pa")
        pbr = Ring(ph, [P(f"pb{i}", [128, 512], F32) for i in range(2)], "pb")
        pcr = Ring(ph, [P(f"pc{i}", [128, 512], F32) for i in range(2)], "pc")
        pyr = Ring(ph, [P("py", [128, 1024], F32)], "py")
        OTv = C.OT.rearrange("(i p) t -> p i t", p=128)
        GTv = C.GT.rearrange("(i p) t -> p i t", p=128)

        def loads(n):
            ot, otb = otr.next()
            gt, gtb = gtr.next()
            ph.dma("sp", ot[:], OTv[:, :, n * 512:(n + 1) * 512], writes=[otb])
            for a in range(3):
                ph.dma("sp", gt[:, a * 8:(a + 1) * 8, :], GTv[:, a * 8:(a + 1) * 8, n * 512:(n + 1) * 512], writes=[gtb])
            return ot, otb, gt, gtb

        nxt = loads(0)
        for n in range(T // 512):
            ot, otb, gt, gtb = nxt
            if n + 1 < T // 512:
                nxt = loads(n + 1)
            mg, mgb = mgr.next()
            for mc in range(8):
                pa, pab = par.next()
                pb, pbb = pbr.next()
                pc, pcb = pcr.next()
                for (pp, ppb, k0, nk) in ((pa, pab, 0, 4), (pb, pbb, 4, 2), (pc, pcb, 6, 4)):
                    for k in range(nk):
                        o_mm(ph, pp[:], wbr[:, k0 + k, mc * 128:(mc + 1) * 128], ot[:, k0 + k, :], k == 0, k == nk - 1, wbs + [otb], [ppb])
                t1, t1b = t1r.next()
                t2, t2b = t2r.next()
                t3, t3b = t3r.next()
                o_tt(ph, "dve", t1[:], pa[:], gt[:, mc, :], ALU.mult, [pab, gtb], [t1b])
                o_tt(ph, "dve", t2[:], pb[:], gt[:, 8 + mc, :], ALU.mult, [pbb, gtb], [t2b])
                o_tt(ph, "dve", t3[:], pc[:], gt[:, 16 + mc, :], ALU.mult, [pcb, gtb], [t3b])
                o_tt(ph, "pool", t1[:], t1[:], t2[:], ALU.add, [t1b, t2b], [t1b])
                o_tt(ph, "pool", mg[:, mc, :], t1[:], t3[:], ALU.add, [t1b, t3b], [mgb])
            subs = []
            for sub in range(4):
                i = 4 * n + sub
                h, hb = hr.next()
                ph.dma("sp", h[:], C.H[i * 128:(i + 1) * 128, :], writes=[hb])
                py, pyb = pyr.next()
                for half in range(2):
                    for k in range(8):
                        o_mm(ph, py[:, half * 512:(half + 1) * 512], mg[:, k, sub * 128:(sub + 1) * 128],
                             wo[:, k, half * 512:(half + 1) * 512], k == 0, k == 7, [mgb] + wbs, [pyb])
                z, zb = zr.next()
                o_stt(ph, z[:], h[:], ALPHA, py[:], ALU.mult, ALU.add, [hb, pyb], [zb])
                mv, mvb = ln_s1(ph, C, z[:], zb)
                subs.append((i, z, zb, mv, mvb))
            for (i, z, zb, mv, mvb) in subs:
                ln_s2(ph, C, mv, mvb)
            for (i, z, zb, mv, mvb) in subs:
                o, ob = orr.next()
                ln_s3(ph, C, z[:], zb, mv, mvb, o[:], ob, g[:], b[:], cb)
                ph.dma("sp", C.H1[i * 128:(i + 1) * 128, :], o[:], reads=[ob])
                o16, o16b = obr.next()
                o_cp(ph, "act", o16[:], o[:], [ob], [o16b])
                ph.dma("sp", C.H1b[i * 128:(i + 1) * 128, :], o16[:], reads=[o16b])
        ph.emit()


def phase_route(C, l):
    from contextlib import ExitStack
    nc = C.nc
    with ExitStack() as st:
        A, P = mk_alloc(nc, st, f"p3b_{l}_")
        ph = Phase(C.S, f"route{l}")
        wr = A("wr", [128, 8, 36], F32)
        brr = A("brr", [128, 36], F32)
        wb = ph.buf("w")
        ph.dma("sp", wr[:], C.wr[l].rearrange("(k p) e -> p k e", p=128), writes=[wb])
        ph.dma("sp", brr[:], C.br[l].to_broadcast([128, 36]), writes=[wb])
        LG = A("LG", [128, NT, 36], F32)
        lgb = ph.buf("LG")
        hr = Ring(ph, [A(f"h{i}", [128, D], F32) for i in range(3)], "h")
        hTr = Ring(ph, [A(f"hT{i}", [128, 8, 128], F32) for i in range(3)], "hT")
        tpr = Ring(ph, [P(f"tp{i}", [128, 8, 128], F32) for i in range(2)], "tp")
        plr = Ring(ph, [P(f"pl{i}", [128, 512], F32) for i in range(2)], "pl")
        ppr = Ring(ph, [P("pp0", [128, 512], F32)], "pp")
        pcr = Ring(ph, [P("pcs0", [128, 512], F32)], "pcs")
        nxt = hr.next()
        ph.dma("sp", nxt[0][:], C.H1[0:128, :], writes=[nxt[1]])
        for i in range(NT):
            h, hb = nxt
            if i + 1 < NT:
                nxt = hr.next()
                ph.dma("sp", nxt[0][:], C.H1[(i + 1) * 128:(i + 2) * 128, :], writes=[nxt[1]])
            tp, tpb = tpr.next()
            for k in range(8):
                o_tp(ph, tp[:, k, :], h[:, k * 128:(k + 1) * 128], C.identf[:], [hb], [tpb])
            hT, hTb = hTr.next()
            o_cp(ph, "act", hT[:], tp[:], [tpb], [hTb])
            pl, plb = plr.next()
            for k in range(8):
                o_mm(ph, pl[:, 0:36], hT[:, k, :], wr[:, k, :], k == 0, k == 7, [hTb, wb], [plb])
            o_tt(ph, "dve", LG[:, i, :], pl[:, 0:36], brr[:], ALU.add, [plb, wb], [lgb])
        def S_(name, shape, dt=F32):
            return A(name, shape, dt), ph.buf(name)
        lg = LG[:, :, 0:4]
        le = LG[:, :, 4:36].rearrange("p n (g e) -> p n g e", g=4)
        gmax, gmb = S_("gmax", [128, NT])
        o_red(ph, gmax[:], lg, ALU.max, [lgb], [gmb])
        goh, gohb = S_("goh", [128, NT, 4])
        o_tt(ph, "dve", goh[:], lg, gmax[:].unsqueeze(2).to_broadcast([128, NT, 4]), ALU.is_equal, [lgb, gmb], [gohb])
        gex, gexb = S_("gex", [128, NT, 4])
        o_tt(ph, "dve", gex[:], lg, gmax[:].unsqueeze(2).to_broadcast([128, NT, 4]), ALU.subtract, [lgb, gmb], [gexb])
        o_act(ph, gex[:], gex[:], AF.Exp, [gexb], [gexb])
        gw, gwb = S_("gw", [128, NT])
        o_red(ph, gw[:], gex[:], ALU.add, [gexb], [gwb])
        o_rcp(ph, gw[:], gw[:], [gwb], [gwb])
        esel, eselb = S_("esel", [128, NT, 8])
        etmp, etmpb = S_("etmp", [128, NT, 8])
        for g in range(4):
            dst, dstb = (esel, eselb) if g == 0 else (etmp, etmpb)
            o_tt(ph, "dve", dst[:], le[:, :, g, :], goh[:, :, g].unsqueeze(2).to_broadcast([128, NT, 8]), ALU.mult, [lgb, gohb], [dstb])
            if g > 0:
                o_tt(ph, "dve", esel[:], esel[:], etmp[:], ALU.add, [eselb, etmpb], [eselb])
        m1, m1b = S_("m1", [128, NT])
        o_red(ph, m1[:], esel[:], ALU.max, [eselb], [m1b])
        oh1, oh1b = S_("oh1", [128, NT, 8])
        o_tt(ph, "dve", oh1[:], esel[:], m1[:].unsqueeze(2).to_broadcast([128, NT, 8]), ALU.is_equal, [eselb, m1b], [oh1b])
        e2, e2b = S_("e2", [128, NT, 8])
        o_ts(ph, "dve", e2[:], oh1[:], -1e30, None, ALU.mult, None, [oh1b], [e2b])
        o_tt(ph, "dve", e2[:], e2[:], esel[:], ALU.add, [e2b, eselb], [e2b])
        m2, m2b = S_("m2", [128, NT])
        o_red(ph, m2[:], e2[:], ALU.max, [e2b], [m2b])
        oh2, oh2b = S_("oh2", [128, NT, 8])
        o_tt(ph, "dve", oh2[:], e2[:], m2[:].unsqueeze(2).to_broadcast([128, NT, 8]), ALU.is_equal, [e2b, m2b], [oh2b])
        ee, eeb = S_("ee", [128, NT])
        o_tt(ph, "dve", ee[:], m2[:], m1[:], ALU.subtract, [m1b, m2b], [eeb])
        o_act(ph, ee[:], ee[:], AF.Exp, [eeb], [eeb])
        p1, p1b = S_("p1", [128, NT])
        o_ts(ph, "dve", p1[:], ee[:], 1.0, None, ALU.add, None, [eeb], [p1b])
        o_rcp(ph, p1[:], p1[:], [p1b], [p1b])
        gwt = C.GW
        gwtb = ph.buf("GW")
        o_tt(ph, "dve", gwt[:, :, 1], ee[:], p1[:], ALU.mult, [eeb, p1b], [gwtb])
        o_tt(ph, "dve", gwt[:, :, 1], gwt[:, :, 1], gw[:], ALU.mult, [gwtb, gwb], [gwtb])
        o_tt(ph, "dve", gwt[:, :, 0], p1[:], gw[:], ALU.mult, [p1b, gwb], [gwtb])
        M1, M1b = S_("M1", [128, NT, 4, 8])
        M2, M2b = S_("M2", [128, NT, 4, 8])
        for g in range(4):
            o_tt(ph, "dve", M1[:, :, g, :], oh1[:], goh[:, :, g].unsqueeze(2).to_broadcast([128, NT, 8]), ALU.mult, [oh1b, gohb], [M1b])
            o_tt(ph, "dve", M2[:, :, g, :], oh2[:], goh[:, :, g].unsqueeze(2).to_broadcast([128, NT, 8]), ALU.mult, [oh2b, gohb], [M2b])
        Mb16, Mb16b = S_("Mb16", [128, NT * 32], BF16)
        o_tt(ph, "dve", Mb16[:], M1[:].rearrange("p n g e -> p (n g e)"), M2[:].rearrange("p n g e -> p (n g e)"), ALU.add, [M1b, M2b], [Mb16b])
        POS, POSb = S_("POS", [128, NT * 32])
        CS, CSb = S_("CS", [128, NT * 32])
        for ch in range(4):
            pp, ppb = ppr.next()
            o_mm(ph, pp[:], C.Umat[:], Mb16[:, ch * 512:(ch + 1) * 512], True, True, [Mb16b], [ppb])
            o_cp(ph, "dve", POS[:, ch * 512:(ch + 1) * 512], pp[:], [ppb], [POSb])
            pcs, pcsb = pcr.next()
            o_mm(ph, pcs[:], C.ones[:], Mb16[:, ch * 512:(ch + 1) * 512], True, True, [Mb16b], [pcsb])
            o_cp(ph, "act", CS[:, ch * 512:(ch + 1) * 512], pcs[:], [pcsb], [CSb])
        SA, SAb = S_("SA", [128, NT * 32])
        SB, SBb = S_("SB", [128, NT * 32])
        o_cp(ph, "dve", SA[:], CS[:], [CSb], [SAb])
        cur, curb, oth, othb = SA, SAb, SB, SBb
        s_ = 1
        while s_ < NT:
            o_cp(ph, "dve", oth[:, 0:s_ * 32], cur[:, 0:s_ * 32], [curb], [othb])
            o_tt(ph, "dve", oth[:, s_ * 32:], cur[:, s_ * 32:], cur[:, 0:(NT - s_) * 32], ALU.add, [curb], [othb])
            cur, curb, oth, othb = oth, othb, cur, curb
            s_ *= 2
        o_tt(ph, "dve", POS[:], POS[:], cur[:], ALU.add, [POSb, curb], [POSb])
        o_tt(ph, "dve", POS[:], POS[:], CS[:], ALU.subtract, [POSb, CSb], [POSb])
        o_ts(ph, "dve", POS[:], POS[:], float(CAP - 1), None, ALU.min, None, [POSb], [POSb])
        o_tt(ph, "dve", POS[:].rearrange("p (n e) -> p n e", e=32), POS[:].rearrange("p (n e) -> p n e", e=32),
             C.ecap[:].unsqueeze(1).to_broadcast([128, NT, 32]), ALU.add, [POSb], [POSb])
        DF, DFb = S_("DF", [128, NT, 2])
        for kx, (Mx, Mxb) in enumerate(((M1, M1b), (M2, M2b))):
            o_tt(ph, "dve", Mx[:].rearrange("p n g e -> p (n g e)"), Mx[:].rearrange("p n g e -> p (n g e)"), POS[:], ALU.mult, [Mxb, POSb], [Mxb])
            o_red(ph, DF[:, :, kx], Mx[:].rearrange("p n g e -> p n (g e)"), ALU.add, [Mxb], [DFb])
        dib = ph.buf("DI")
        o_cp(ph, "dve", C.DI[:], DF[:], [DFb], [dib])
        xr = Ring(ph, [A(f"x{i}", [128, D], BF16) for i in range(3)], "x")
        for i in range(NT):
            xt, xb = xr.next()
            ph.dma("sp", xt[:], C.H1b[i * 128:(i + 1) * 128, :], writes=[xb])
            for kx in range(2):
                ph.dma_fn("pool", (lambda e, xt=xt, i=i, kx=kx: e.indirect_dma_start(
                    out=C.XS, out_offset=bass.IndirectOffsetOnAxis(ap=C.DI[:, i, kx:kx + 1], axis=0), in_=xt[:], in_offset=None)),
                    reads=[xb, dib])
        ph.emit()


def phase_experts(C, l):
    from contextlib import ExitStack
    nc = C.nc
    NST = CAP // 128
    HN = CAP // 2
    with ExitStack() as st:
        A, P = mk_alloc(nc, st, f"p4_{l}_")
        ph = Phase(C.S, f"experts{l}")
        wgr = Ring(ph, [A(f"wg{i}", [128, 8, 512], BF16) for i in range(2)], "wg")
        wur = Ring(ph, [A(f"wu{i}", [128, 8, 512], BF16) for i in range(2)], "wu")
        wdr = Ring(ph, [A(f"wd{i}", [128, 4, 1024], BF16) for i in range(2)], "wd")
        wdsr = Ring(ph, [A(f"wds{i}", [128, 4, 1024], F32) for i in range(2)], "wds")
        xsr = Ring(ph, [A(f"xs{i}", [128, NST, D], BF16) for i in range(2)], "xs")
        xTr = Ring(ph, [A(f"xT{i}", [128, 8, CAP], BF16) for i in range(2)], "xT")
        sgr = Ring(ph, [A(f"sg{i}", [128, HN], F32) for i in range(2)], "sg")
        hdr = Ring(ph, [A(f"hd{i}", [128, 4, HN], BF16) for i in range(2)], "hd")
        yor = Ring(ph, [A(f"yo{i}", [128, D], BF16) for i in range(3)], "yo")
        tpr = Ring(ph, [P(f"tp{i}", [128, 1024], BF16) for i in range(2)], "tp")
        pgr = Ring(ph, [P(f"pg{i}", [128, 512], F32) for i in range(2)], "pg")
        pur = Ring(ph, [P(f"pu{i}", [128, 512], F32) for i in range(2)], "pu")
        pyr = Ring(ph, [P("py", [128, 1024], F32)], "py")

        def loads(e):
            wg, wgb = wgr.next()
            wu, wub = wur.next()
            wd, wdb = wdr.next()
            xs, xsb = xsr.next()
            ph.dma("pool", wg[:], C.w_eg[l, e].rearrange("(k p) f -> p k f", p=128), writes=[wgb])
            ph.dma("pool", wu[:], C.w_eu[l, e].rearrange("(k p) f -> p k f", p=128), writes=[wub])
            wds, wdsb = wdsr.next()
            ph.dma("sp", wds[:], C.w_ed[l, e].rearrange("(k p) f -> p k f", p=128), writes=[wdsb])
            o_cp(ph, "pool", wd[:], wds[:], [wdsb], [wdb])
            ph.dma("sp", xs[:], C.XS[e * CAP:(e + 1) * CAP, :].rearrange("(s p) d -> p s d", p=128), writes=[xsb])
            return wg, wgb, wu, wub, wd, wdb, xs, xsb

        nxt = loads(0)
        for e in range(32):
            wg, wgb, wu, wub, wd, wdb, xs, xsb = nxt
            if e + 1 < 32:
                nxt = loads(e + 1)
            xT, xTb = xTr.next()
            for s_ in range(NST):
                tp, tpb = tpr.next()
                for k in range(8):
                    o_tp(ph, tp[:, k * 128:(k + 1) * 128], xs[:, s_, k * 128:(k + 1) * 128], C.identb[:], [xsb], [tpb])
                o_cp(ph, "dve" if s_ % 2 == 0 else "act", xT[:, :, s_ * 128:(s_ + 1) * 128],
                     tp[:].rearrange("p (k t) -> p k t", k=8), [tpb], [xTb])
            for hh in range(2):
                hd, hdb = hdr.next()
                for fc in range(4):
                    pg, pgb = pgr.next()
                    pu, pub = pur.next()
                    for k in range(8):
                        o_mm(ph, pg[:, 0:HN], wg[:, k, fc * 128:(fc + 1) * 128], xT[:, k, hh * HN:(hh + 1) * HN], k == 0, k == 7, [wgb, xTb], [pgb])
                    for k in range(8):
                        o_mm(ph, pu[:, 0:HN], wu[:, k, fc * 128:(fc + 1) * 128], xT[:, k, hh * HN:(hh + 1) * HN], k == 0, k == 7, [wub, xTb], [pub])
                    sg, sgb = sgr.next()
                    o_act(ph, sg[:], pg[:, 0:HN], AF.Silu, [pgb], [sgb])
                    o_tt(ph, "dve", hd[:, fc, :], sg[:], pu[:, 0:HN], ALU.mult, [sgb, pub], [hdb])
                for s_ in range(NST // 2):
                    py, pyb = pyr.next()
                    for half in range(2):
                        for k in range(4):
                            o_mm(ph, py[:, half * 512:(half + 1) * 512], hd[:, k, s_ * 128:(s_ + 1) * 128],
                                 wd[:, k, half * 512:(half + 1) * 512], k == 0, k == 3, [hdb, wdb], [pyb])
                    yo, yob = yor.next()
                    o_cp(ph, "dve" if s_ % 2 == 0 else "act", yo[:], py[:], [pyb], [yob])
                    r0 = e * CAP + hh * HN + s_ * 128
                    ph.dma("sp", C.R[r0:r0 + 128, :], yo[:], reads=[yob])
        ph.emit()


def phase_combine(C, l, dst):
    from contextlib import ExitStack
    nc = C.nc
    with ExitStack() as st:
        A, P = mk_alloc(nc, st, f"p5_{l}_")
        ph = Phase(C.S, f"combine{l}")
        ln_setup(ph, C, A)
        g, b, cb = load_gb(ph, A, C.ln2g[l], C.ln2b[l], "ln2")
        r1r = Ring(ph, [A(f"r1_{i}", [128, D], BF16) for i in range(2)], "r1")
        r2r = Ring(ph, [A(f"r2_{i}", [128, D], BF16) for i in range(2)], "r2")
        hr = Ring(ph, [A(f"h{i}", [128, D], F32) for i in range(2)], "h")
        yr = Ring(ph, [A(f"y{i}", [128, D], F32) for i in range(2)], "y")
        zr = Ring(ph, [A(f"z{i}", [128, D], F32) for i in range(5)], "z")
        orr = Ring(ph, [A(f"o{i}", [128, D], F32) for i in range(3)], "o")

        def loads(i):
            r1, r1b = r1r.next()
            r2, r2b = r2r.next()
            h, hb = hr.next()
            for kx, (rt, rb) in enumerate(((r1, r1b), (r2, r2b))):
                ph.dma_fn("pool", (lambda e, rt=rt, i=i, kx=kx: e.indirect_dma_start(
                    out=rt[:], out_offset=None, in_=C.R, in_offset=bass.IndirectOffsetOnAxis(ap=C.DI[:, i, kx:kx + 1], axis=0))),
                    writes=[rb])
            ph.dma("sp", h[:], C.H1[i * 128:(i + 1) * 128, :], writes=[hb])
            return r1, r1b, r2, r2b, h, hb

        nxt = loads(0)
        grp = []
        for i in range(NT):
            r1, r1b, r2, r2b, h, hb = nxt
            if i + 1 < NT:
                nxt = loads(i + 1)
            y, yb = yr.next()
            o_act(ph, y[:], r1[:], AF.Copy, [r1b], [yb], scale=C.GW[:, i, 0:1])
            o_stt(ph, y[:], r2[:], C.GW[:, i, 1:2], y[:], ALU.mult, ALU.add, [r2b, yb], [yb])
            z, zb = zr.next()
            o_stt(ph, z[:], h[:], ALPHA, y[:], ALU.mult, ALU.add, [hb, yb], [zb])
            mv, mvb = ln_s1(ph, C, z[:], zb)
            grp.append((i, z, zb, mv, mvb))
            if len(grp) == 4 or i == NT - 1:
                for (ii, z_, zb_, mv_, mvb_) in grp:
                    ln_s2(ph, C, mv_, mvb_)
                for (ii, z_, zb_, mv_, mvb_) in grp:
                    o, ob = orr.next()
                    ln_s3(ph, C, z_[:], zb_, mv_, mvb_, o[:], ob, g[:], b[:], cb, geng="dve", beng="dve")
                    ph.dma("sp", dst[ii * 128:(ii + 1) * 128, :], o[:], reads=[ob])
                grp = []
        ph.emit()


def phase_pre(C):
    from contextlib import ExitStack
    nc = C.nc
    with ExitStack() as st:
        A, P = mk_alloc(nc, st, "pre_")
        ph = Phase(C.S, "pre")
        cb = ph.buf("const")
        o_memset(ph, "pool", C.identb[:], 0.0, [cb])
        ph.op("pool", lambda e: e.affine_select(out=C.identb[:], in_=C.identb[:], pattern=[[-1, 128]], compare_op=ALU.not_equal,
                                                 fill=1.0, base=0, channel_multiplier=1), [cb], [cb])
        o_memset(ph, "pool", C.identf[:], 0.0, [cb])
        ph.op("pool", lambda e: e.affine_select(out=C.identf[:], in_=C.identf[:], pattern=[[-1, 128]], compare_op=ALU.not_equal,
                                                 fill=1.0, base=0, channel_multiplier=1), [cb], [cb])
        o_memset(ph, "pool", C.Umat[:], 1.0, [cb])
        ph.op("pool", lambda e: e.affine_select(out=C.Umat[:], in_=C.Umat[:], pattern=[[1, 128]], compare_op=ALU.is_gt,
                                                 fill=0.0, base=0, channel_multiplier=-1), [cb], [cb])
        o_memset(ph, "pool", C.ones[:], 1.0, [cb])
        o_memset(ph, "pool", C.eps[:], LN_EPS, [cb])
        o_memset(ph, "pool", C.fence[:], 0.0, [cb])
        eci = A("eci", [128, 32], I32)
        ph.op("pool", lambda e: e.iota(eci[:], pattern=[[CAP, 32]], base=0, channel_multiplier=0), [cb], [cb])
        o_cp(ph, "pool", C.ecap[:], eci[:], [cb], [cb])
        tmpr = Ring(ph, [A(f"bt{i}", [128, 3072], F32) for i in range(2)], "bt")
        tmp, tb = tmpr.next()
        ph.dma("sp", tmp[:], C.biasA, writes=[tb])
        o_act(ph, C.EA[:], tmp[:], AF.Exp, [tb], [cb])
        ph.dma("sp", C.BB[:].rearrange("p (g c) -> p g c", g=3), C.biasB.rearrange("g p c -> p g c"), writes=[cb])
        zt = A("zt", [128, 8 * 520], BF16)
        zb = ph.buf("zt")
        o_memset(ph, "pool", zt[:], 0.0, [zb])
        na = PADR // 128
        for base in (0, PADR + T):
            for a in range(na):
                r0 = base + a * 128
                ph.dma("sp", C.QK[r0:r0 + 128, :], zt[:, 0:QK_W], reads=[zb])
            for arr, wd_ in [(C.VA, 130), (C.VB[0], 260), (C.VB[1], 260), (C.VB[2], 260), (C.VC, 520)]:
                ph.dma("sp", arr[base:base + PADR, :].rearrange("(p a) c -> p (a c)", a=na), zt[:, 0:na * wd_], reads=[zb])
        ph.emit()


def run_b(C, l):
    for gi in range(3):
        phase_attn_b(C, l, gi)
    phase_attn_bc(C, l)


INPUT_SPECS = [
    ("x", [T, D]), ("ln0_g", [1, D]), ("ln0_b", [1, D]),
    ("biasA", [128, 3072]), ("biasB", [3, 128, 1024]), ("biasC", [DEPTH, 5, 128, 5120]),
    ("w_in", [DEPTH, D, 7680]), ("bgT", [DEPTH, 128, 24]), ("sink", [DEPTH, 1, 8]),
    ("w_bra", [DEPTH, 512, D]), ("w_brb", [DEPTH, 256, D]), ("w_brc", [DEPTH, 512, D]), ("w_out", [DEPTH, D, D]),
    ("ln1g", [DEPTH, 1, D]), ("ln1b", [DEPTH, 1, D]), ("wr", [DEPTH, D, 36]), ("br", [DEPTH, 1, 36]),
    ("w_eg", [DEPTH, 32, D, 512]), ("w_eu", [DEPTH, 32, D, 512]), ("w_ed", [DEPTH, 32, 512, D]),
    ("ln2g", [DEPTH, 1, D]), ("ln2b", [DEPTH, 1, D]),
]


def build(debug=(), upto=None, only_inputs=None, only_steps=None):
    from contextlib import ExitStack
    nc = bass.Bass("TRN2", target_bir_lowering=False)
    C = Ctx()
    C.nc = nc
    for name, shape in INPUT_SPECS:
        if only_inputs is not None and name not in only_inputs:
            continue
        ap = nc.dram_tensor(name, list(shape), F32, kind="ExternalInput").ap()
        setattr(C, {"ln0_g": "ln0g", "ln0_b": "ln0b"}.get(name, name), ap)

    def scr(name, shape, dt):
        kind = "ExternalOutput" if name in debug else "Internal"
        return nc.dram_tensor(name, list(shape), dt, kind=kind).ap()

    C.out = nc.dram_tensor("out", [T, D], F32, kind="ExternalOutput").ap()
    C.H = scr("H", [T, D], F32)
    C.QK = scr("QK", [ROWS, QK_W], BF16)
    C.VA = scr("VA", [ROWS, 130], BF16)
    C.VB = [scr(f"VB{g}", [ROWS, 260], BF16) for g in range(3)]
    C.VC = scr("VC", [ROWS, 520], BF16)
    C.GT = scr("GT", [3072, T], BF16)
    C.NB = [scr(f"NB{g}", [T, 260], F32) for g in range(3)]
    C.OT = scr("OT", [1280, T], BF16)
    C.OC = scr("OC", [T, 512], BF16)
    C.H1 = scr("H1", [T, D], F32)
    C.H1b = scr("H1b", [T, D], BF16)
    C.XS = scr("XS", [NSLOT, D], BF16)
    C.R = scr("R", [NSLOT, D], BF16)
    with ExitStack() as st:
        C.S = Sched(nc, st)
        A, _ = mk_alloc(nc, st, "g_")
        C.identb = A("identb", [128, 128], BF16)
        C.identf = A("identf", [128, 128], F32)
        C.Umat = A("Umat", [128, 128], BF16)
        C.ones = A("ones", [128, 128], BF16)
        C.eps = A("eps", [128, 1], F32)
        C.ecap = A("ecap", [128, 32], F32)
        C.EA = A("EA", [128, 3072], BF16)
        C.BB = A("BB", [128, 3072], F32)
        C.fence = A("fence", [128, 2], F32)
        C.DI = A("DI", [128, NT, 2], I32)
        C.GW = A("GW", [128, NT, 2], F32)
        steps = [("pre", lambda: phase_pre(C)), ("ln0", lambda: phase_ln0(C))]
        for l in range(DEPTH):
            dst = C.H if l + 1 < DEPTH else C.out
            steps += [
                (f"proj{l}", lambda l=l: phase_proj(C, l)),
                (f"attnA{l}", lambda l=l: phase_attn_a(C, l)),
                (f"attnB{l}", lambda l=l: run_b(C, l)),
                (f"attnC{l}", lambda l=l: phase_attn_c(C, l)),
                (f"merge{l}", lambda l=l: phase_merge(C, l)),
                (f"route{l}", lambda l=l: phase_route(C, l)),
                (f"experts{l}", lambda l=l: phase_experts(C, l)),
                (f"combine{l}", lambda l=l, dst=dst: phase_combine(C, l, dst)),
            ]
        for name, fn in steps:
            if only_steps is not None and name not in only_steps:
                continue
            fn()
            if upto is not None and name == upto:
                break
    return nc


def host_inputs(inp):
    f = lambda a: np.ascontiguousarray(np.asarray(a), dtype=np.float32)
    rel_bias = f(inp["rel_bias"])
    rpb_c = f(inp["rpb_c"])
    ba, bb, bc = host_bias_tables(rel_bias, rpb_c)
    b_gate = f(inp["b_gate"])
    w_rg, w_re = f(inp["w_rg"]), f(inp["w_re"])
    wr = np.concatenate([w_rg, w_re.transpose(0, 2, 1, 3).reshape(DEPTH, D, 32)], axis=2)
    br = np.concatenate([f(inp["b_rg"]), f(inp["b_re"]).reshape(DEPTH, 32)], axis=1).reshape(DEPTH, 1, 36)
    shared = {
        "ln0_g": f(inp["ln0_g"]).reshape(1, D), "ln0_b": f(inp["ln0_b"]).reshape(1, D),
        "biasA": np.ascontiguousarray(ba.reshape(128, 3072)),
        "biasB": np.ascontiguousarray(bb.reshape(3, 128, 1024)),
        "biasC": np.ascontiguousarray(bc.reshape(DEPTH, 5, 128, 5120)),
        "w_in": f(inp["w_in"]),
        "bgT": np.ascontiguousarray(b_gate.reshape(DEPTH, 24, 128).transpose(0, 2, 1)),
        "sink": f(inp["sink_a"]).reshape(DEPTH, 1, 8),
        "w_bra": f(inp["w_br_a"]), "w_brb": f(inp["w_br_b"]), "w_brc": f(inp["w_br_c"]), "w_out": f(inp["w_out"]),
        "ln1g": f(inp["ln1_g"]).reshape(DEPTH, 1, D), "ln1b": f(inp["ln1_b"]).reshape(DEPTH, 1, D),
        "wr": np.ascontiguousarray(wr), "br": np.ascontiguousarray(br),
        "w_eg": f(inp["w_eg"]), "w_eu": f(inp["w_eu"]), "w_ed": f(inp["w_ed"]),
        "ln2g": f(inp["ln2_g"]).reshape(DEPTH, 1, D), "ln2b": f(inp["ln2_b"]).reshape(DEPTH, 1, D),
    }
    return shared


_NC_CACHE = {}


def kernel(**inputs):
    x = np.ascontiguousarray(np.asarray(inputs["x"]), dtype=np.float32)
    shared = host_inputs(inputs)
    if "nc" not in _NC_CACHE:
        _NC_CACHE["nc"] = build()
    nc = _NC_CACHE["nc"]
    in_maps = []
    for c in range(8):
        m = dict(shared)
        m["x"] = x[c]
        in_maps.append(m)
    res = run_bass_kernel_spmd(nc, in_maps, core_ids=list(range(8)))
    return np.stack([np.asarray(r["out"], dtype=np.float32) for r in res.results], axis=0)
```

```python
import numpy as np
import concourse.bass as bass
import concourse.mybir as mybir
from concourse.bass_utils import run_bass_kernel_spmd

F32 = mybir.dt.float32
BF16 = mybir.dt.bfloat16
I32 = mybir.dt.int32
U32 = mybir.dt.uint32
ALU = mybir.AluOpType
AF = mybir.ActivationFunctionType
AX = mybir.AxisListType

ENGS = ("pe", "act", "dve", "pool", "sp")


class Buf:
    __slots__ = ("name", "last_w", "readers")

    def __init__(self, name=""):
        self.name = name
        self.last_w = None
        self.readers = []


class Op:
    __slots__ = ("eng", "fn", "deps", "is_dma", "sem", "count", "has_dep", "prev_dma", "idx")

    def __init__(self, eng, fn, is_dma):
        self.eng = eng
        self.fn = fn
        self.deps = []
        self.is_dma = is_dma
        self.sem = None
        self.count = 0
        self.has_dep = False
        self.prev_dma = None


class Sched:
    NS = 4
    ND = 8

    def __init__(self, nc, stack):
        self.nc = nc
        self.esem = {e: [stack.enter_context(nc.semaphore(f"s_{e}{i}")) for i in range(self.NS)] for e in ENGS}
        self.ecnt = {e: [0] * self.NS for e in ENGS}
        self.ek = {e: 0 for e in ENGS}
        self.dsem = {e: [stack.enter_context(nc.semaphore(f"d_{e}{i}")) for i in range(self.ND)] for e in ("sp", "pool", "act")}
        self.dcnt = {e: [0] * self.ND for e in self.dsem}
        self.dk = {e: 0 for e in self.dsem}
        self.dlast = {e: [None] * self.ND for e in self.dsem}
        self.waited = {e: {} for e in ENGS}


class Phase:
    def __init__(self, sched, name):
        self.s = sched
        self.nc = sched.nc
        self.name = name
        self.ops = []
        self.bufs = []

    def buf(self, name=""):
        b = Buf(name)
        self.bufs.append(b)
        return b

    def _add(self, op, reads, writes):
        deps = []
        for b in reads:
            if b.last_w is not None:
                deps.append(b.last_w)
        for b in writes:
            if b.last_w is not None:
                deps.append(b.last_w)
            deps.extend(b.readers)
        for b in reads:
            b.readers.append(op)
        for b in writes:
            b.last_w = op
            b.readers = []
        seen = set()
        for d in deps:
            if d is op or id(d) in seen:
                continue
            seen.add(id(d))
            if d.eng == "pe" and op.eng == "pe" and not d.is_dma and not op.is_dma:
                continue
            op.deps.append(d)
            d.has_dep = True
        self.ops.append(op)
        return op

    def op(self, eng, fn, reads=(), writes=()):
        return self._add(Op(eng, fn, False), reads, writes)

    def dma(self, q, out, in_, reads=(), writes=(), **kw):
        def fn(e):
            return e.dma_start(out=out, in_=in_, **kw)
        return self._add(Op(q, fn, True), reads, writes)

    def dma_fn(self, q, fn, reads=(), writes=()):
        return self._add(Op(q, fn, True), reads, writes)

    def emit(self):
        s = self.s
        nc = self.nc
        for op in self.ops:
            e = op.eng
            if op.is_dma:
                k = s.dk[e] % s.ND
                s.dk[e] += 1
                op.prev_dma = s.dlast[e][k]
                s.dcnt[e][k] += 16
                op.sem = s.dsem[e][k]
                op.count = s.dcnt[e][k]
                s.dlast[e][k] = (op.sem, op.count)
            elif op.has_dep:
                k = s.ek[e] % s.NS
                s.ek[e] += 1
                s.ecnt[e][k] += 1
                op.sem = s.esem[e][k]
                op.count = s.ecnt[e][k]
        per = {e: [o for o in self.ops if o.eng == e] for e in ENGS}

        def run(e, eng):
            waited = s.waited[e]
            for op in per[e]:
                need = {}
                for d in op.deps:
                    key = id(d.sem)
                    if need.get(key, (None, 0))[1] < d.count:
                        need[key] = (d.sem, d.count)
                if op.prev_dma is not None:
                    sem, cnt = op.prev_dma
                    key = id(sem)
                    if need.get(key, (None, 0))[1] < cnt:
                        need[key] = (sem, cnt)
                for key, (sem, cnt) in need.items():
                    if waited.get(key, 0) < cnt:
                        eng.wait_ge(sem, cnt)
                        waited[key] = cnt
                ins = op.fn(eng)
                if op.sem is not None:
                    ins.then_inc(op.sem, 16 if op.is_dma else 1)
            if e in s.dsem:
                for k in range(s.ND):
                    if s.dlast[e][k] is not None:
                        sem, cnt = s.dlast[e][k]
                        if waited.get(id(sem), 0) < cnt:
                            eng.wait_ge(sem, cnt)
                            waited[id(sem)] = cnt

        with nc.Block() as block:
            @block.sync
            def _(eng):
                run("sp", eng)

            @block.tensor
            def _(eng):
                run("pe", eng)

            @block.scalar
            def _(eng):
                run("act", eng)

            @block.vector
            def _(eng):
                run("dve", eng)

            @block.gpsimd
            def _(eng):
                run("pool", eng)
        self.ops = []


T = 8192
D = 1024
NT = T // 128
PADR = 1024
ROWS = PADR + T + PADR
DEPTH = 2
ALPHA = (2 * DEPTH) ** 0.25
LN_EPS = 1e-5
CAP = 768
NSLOT = 32 * CAP
NEGB = -30000.0
B_CFG = ((128, 1), (512, 4), (2048, 16))
QK_AQ, QK_AK = 0, 512
QK_BQ = (640, 1152, 1664)
QK_BK = (896, 1408, 1920)
QK_CQ, QK_CK = 2176, 2688
QK_W = 3200
SEGS = [
    (0, 512, "qk", QK_AQ), (512, 128, "qk", QK_AK), (640, 128, "va", 0),
    (768, 512, "qk", QK_BQ[0]), (1280, 256, "vb", 0),
    (1536, 512, "qk", QK_BQ[1]), (2048, 256, "vb", 1),
    (2304, 512, "qk", QK_BQ[2]), (2816, 256, "vb", 2),
    (3072, 512, "qk", QK_CQ), (3584, 512, "qk", QK_CK), (4096, 512, "vc", 0),
]
GATE0 = 4608


def _t5_bucket(rel):
    half, max_exact = 16, 8
    ret = np.where(rel > 0, half, 0)
    n = np.abs(rel)
    large = max_exact + (np.log(np.maximum(n, max_exact) / max_exact) / np.log(2048 / max_exact) * (half - max_exact)).astype(np.int32)
    large = np.minimum(large, half - 1)
    return (ret + np.where(n < max_exact, n, large)).astype(np.int32)


def host_bias_tables(rel_bias, rpb_c):
    kk = np.arange(128)[:, None]
    qq = np.arange(128)[None, :]
    ba = np.full((128, 2, 3, 4, 128), NEGB, np.float32)
    for c in range(3):
        off = (c - 1) * 128 + kk - qq
        band = np.abs(off) <= 128
        bk = _t5_bucket(off)
        for h in range(8):
            ba[:, h // 4, c, h % 4, :] = np.where(band, rel_bias[bk, h], NEGB)
    bb = np.full((3, 128, 2, 4, 128), NEGB, np.float32)
    for g, (_, dil) in enumerate(B_CFG):
        for jp in range(2):
            off = jp * 128 + kk - 64 - qq
            band = np.abs(off) <= 64
            bk = _t5_bucket(off * dil)
            for h in range(4):
                bb[g, :, jp, h, :] = np.where(band, rel_bias[bk, 8 + 4 * g + h], NEGB)
    bc = np.full((rpb_c.shape[0], 5, 128, 5, 8, 128), NEGB, np.float32)
    for pi, j in enumerate((0, 1, 2, 62, 63)):
        cb0 = min(max(j - 2, 0), 59)
        qtok = j * 128 + np.arange(128)
        qi, qc = qtok // 64, qtok % 64
        rstart = np.clip(qi - 4, 0, 120)
        qstart = np.clip(qc - 8, 0, 48)
        for c in range(5):
            ktok = (cb0 + c) * 128 + np.arange(128)
            kr, kc = ktok // 64, ktok % 64
            valid = ((kr[:, None] >= rstart[None, :]) & (kr[:, None] < rstart[None, :] + 8)
                     & (kc[:, None] >= qstart[None, :]) & (kc[:, None] < qstart[None, :] + 16))
            ridx = np.clip(kr[:, None] - qi[None, :] + 7, 0, 14)
            cidx = np.clip(kc[:, None] - qc[None, :] + 15, 0, 30)
            for l in range(rpb_c.shape[0]):
                for h in range(8):
                    bc[l, pi, :, c, h, :] = np.where(valid, rpb_c[l, h][ridx, cidx], NEGB)
    return ba, bb, bc


def c_pattern(j):
    return {0: 0, 1: 1, 62: 3, 63: 4}.get(j, 2)


def o_mm(ph, out, lhsT, rhs, start, stop, reads, writes):
    return ph.op("pe", lambda e: e.matmul(out, lhsT=lhsT, rhs=rhs, start=start, stop=stop), reads, writes)


def o_tp(ph, out, in_, ident, reads, writes):
    return ph.op("pe", lambda e: e.transpose(out=out, in_=in_, identity=ident), reads, writes)


def o_act(ph, out, in_, func, reads, writes, scale=None, bias=None):
    kw = {}
    if scale is not None:
        kw["scale"] = scale
    if bias is not None:
        kw["bias"] = bias
    return ph.op("act", lambda e: e.activation(out=out, in_=in_, func=func, **kw), reads, writes)


def o_cp(ph, eng, out, in_, reads, writes):
    if eng == "act":
        return ph.op("act", lambda e: e.copy(out=out, in_=in_), reads, writes)
    return ph.op(eng, lambda e: e.tensor_copy(out=out, in_=in_), reads, writes)


def o_tt(ph, eng, out, in0, in1, op, reads, writes):
    return ph.op(eng, lambda e: e.tensor_tensor(out=out, in0=in0, in1=in1, op=op), reads, writes)


def o_ts(ph, eng, out, in0, s1, s2, op0, op1, reads, writes):
    if s2 is None:
        return ph.op(eng, lambda e: e.tensor_scalar(out=out, in0=in0, scalar1=s1, scalar2=None, op0=op0), reads, writes)
    return ph.op(eng, lambda e: e.tensor_scalar(out=out, in0=in0, scalar1=s1, scalar2=s2, op0=op0, op1=op1), reads, writes)


def o_stt(ph, out, in0, scalar, in1, op0, op1, reads, writes):
    return ph.op("dve", lambda e: e.scalar_tensor_tensor(out=out, in0=in0, scalar=scalar, in1=in1, op0=op0, op1=op1), reads, writes)


def o_red(ph, out, in_, op, reads, writes):
    return ph.op("dve", lambda e: e.tensor_reduce(out=out, in_=in_, axis=AX.X, op=op), reads, writes)


def o_rcp(ph, out, in_, reads, writes):
    return ph.op("dve", lambda e: e.reciprocal(out=out, in_=in_), reads, writes)


def o_memset(ph, eng, ap, val, writes):
    return ph.op(eng, lambda e: e.memset(ap, val), (), writes)


class Ring:
    def __init__(self, ph, tiles, name):
        self.tiles = tiles
        self.bufs = [ph.buf(f"{name}{i}") for i in range(len(tiles))]
        self.k = 0

    def next(self):
        i = self.k % len(self.tiles)
        self.k += 1
        return self.tiles[i], self.bufs[i]


class Ctx:
    pass


def ln_s1(ph, C, z, zb):
    st, stb = C.ln_st.next()
    mv, mvb = C.ln_mv.next()
    ph.op("dve", lambda e: e.bn_stats(out=st[:, 0, :], in_=z[:, 0:512]), [zb], [stb])
    ph.op("dve", lambda e: e.bn_stats(out=st[:, 1, :], in_=z[:, 512:1024]), [zb, stb], [stb])
    ph.op("dve", lambda e: e.bn_aggr(out=mv[:, 0:2], in_=st[:].rearrange("p a s -> p (a s)")), [stb], [mvb])
    return mv, mvb


def ln_s2(ph, C, mv, mvb):
    o_act(ph, mv[:, 2:3], mv[:, 1:2], AF.Sqrt, [mvb], [mvb], bias=C.eps[:, 0:1], scale=1.0)
    o_rcp(ph, mv[:, 3:4], mv[:, 2:3], [mvb], [mvb])
    o_ts(ph, "dve", mv[:, 4:5], mv[:, 0:1], mv[:, 3:4], -1.0, ALU.mult, ALU.mult, [mvb], [mvb])


def ln_s3(ph, C, z, zb, mv, mvb, out, outb, g, b, cb, geng="pool", beng="pool"):
    o_act(ph, out, z, AF.Identity, [zb, mvb], [outb], scale=mv[:, 3:4], bias=mv[:, 4:5])
    o_tt(ph, geng, out, out, g, ALU.mult, [outb, cb], [outb])
    o_tt(ph, beng, out, out, b, ALU.add, [outb, cb], [outb])


def ln_tile(ph, C, z, zb, out, outb, g, b, cb, geng="pool", beng="pool"):
    mv, mvb = ln_s1(ph, C, z, zb)
    ln_s2(ph, C, mv, mvb)
    ln_s3(ph, C, z, zb, mv, mvb, out, outb, g, b, cb, geng, beng)


def ln_setup(ph, C, A):
    C.ln_st = Ring(ph, [A(f"lnst{i}", [128, 2, 6], F32) for i in range(6)], "lnst")
    C.ln_mv = Ring(ph, [A(f"lnmv{i}", [128, 8], F32) for i in range(6)], "lnmv")


def load_gb(ph, A, gsrc, bsrc, name):
    g = A(name + "g", [128, D], F32)
    b = A(name + "b", [128, D], F32)
    cb = ph.buf(name)
    ph.dma("sp", g[:], gsrc.to_broadcast([128, D]), writes=[cb])
    ph.dma("sp", b[:], bsrc.to_broadcast([128, D]), writes=[cb])
    return g, b, cb


def mk_alloc(nc, st, prefix):
    def A(name, shape, dt):
        return st.enter_context(nc.sbuf_tensor(prefix + name, list(shape), dt))

    def P(name, shape, dt):
        return st.enter_context(nc.psum_tensor(prefix + name, list(shape), dt))
    return A, P


def phase_ln0(C):
    from contextlib import ExitStack
    nc = C.nc
    with ExitStack() as st:
        A, P = mk_alloc(nc, st, "p0_")
        ph = Phase(C.S, "ln0")
        ln_setup(ph, C, A)
        g, b, cb = load_gb(ph, A, C.ln0g, C.ln0b, "ln0")
        zr = Ring(ph, [A(f"z{i}", [128, D], F32) for i in range(6)], "z")
        orr = Ring(ph, [A(f"o{i}", [128, D], F32) for i in range(3)], "o")
        nxt = zr.next()
        ph.dma("sp", nxt[0][:], C.x[0:128, :], writes=[nxt[1]])
        grp = []
        for i in range(NT):
            z, zb = nxt
            if i + 1 < NT:
                nxt = zr.next()
                ph.dma("sp", nxt[0][:], C.x[(i + 1) * 128:(i + 2) * 128, :], writes=[nxt[1]])
            mv, mvb = ln_s1(ph, C, z[:], zb)
            grp.append((i, z, zb, mv, mvb))
            if len(grp) == 4 or i == NT - 1:
                for (ii, z_, zb_, mv_, mvb_) in grp:
                    ln_s2(ph, C, mv_, mvb_)
                for (ii, z_, zb_, mv_, mvb_) in grp:
                    o, ob = orr.next()
                    ln_s3(ph, C, z_[:], zb_, mv_, mvb_, o[:], ob, g[:], b[:], cb, geng="dve", beng="pool")
                    ph.dma("sp", C.H[ii * 128:(ii + 1) * 128, :], o[:], reads=[ob])
                grp = []
        ph.emit()


def phase_proj(C, l):
    from contextlib import ExitStack
    nc = C.nc
    with ExitStack() as st:
        A, P = mk_alloc(nc, st, f"p1_{l}_")
        ph = Phase(C.S, f"proj{l}")
        w = A("w", [128, 8, 7680], BF16)
        wb = ph.buf("w")
        wstr = Ring(ph, [A(f"wst{i}", [128, 1536], F32) for i in range(2)], "wst")
        wbs = []
        for k in range(8):
            for cbk in range(5):
                src = C.w_in[l, k * 128:(k + 1) * 128, cbk * 1536:(cbk + 1) * 1536]
                wbc = ph.buf(f"w{k}_{cbk}")
                wbs.append(wbc)
                if (k * 5 + cbk) % 3 == 2:
                    wst, wstb = wstr.next()
                    ph.dma("sp", wst[:], src, writes=[wstb])
                    o_cp(ph, "pool", w[:, k, cbk * 1536:(cbk + 1) * 1536], wst[:], [wstb], [wbc])
                else:
                    ph.dma("pool", w[:, k, cbk * 1536:(cbk + 1) * 1536], src, writes=[wbc])
        bg = A("bg", [128, 24], F32)
        ph.dma("sp", bg[:], C.bgT[l], writes=[wb])
        hin = Ring(ph, [A(f"hin{i}", [128, D], F32) for i in range(2)], "hin")
        hT_t = [A(f"hT{i}", [128, 8, 512], BF16) for i in range(2)]
        hT_b = [[ph.buf(f"hT{i}_{s_}") for s_ in range(4)] for i in range(2)]
        qks = Ring(ph, [A(f"qks{i}", [128, QK_W], BF16) for i in range(2)], "qks")
        vas_t = [A(f"vas{i}", [128, 2, 65], BF16) for i in range(2)]
        vbs_t = [A(f"vbs{i}", [128, 3, 4, 65], BF16) for i in range(2)]
        vcs_t = [A(f"vcs{i}", [128, 8, 65], BF16) for i in range(2)]
        vas = Ring(ph, vas_t, "vas")
        vbs = Ring(ph, vbs_t, "vbs")
        vcs = Ring(ph, vcs_t, "vcs")
        for i in range(2):
            o_memset(ph, "pool", vas_t[i][:], 1.0, [vas.bufs[i]])
            o_memset(ph, "pool", vbs_t[i][:], 1.0, [vbs.bufs[i]])
            o_memset(ph, "pool", vcs_t[i][:], 1.0, [vcs.bufs[i]])
        gst = Ring(ph, [A(f"gst{i}", [128, 6, 512], BF16) for i in range(2)], "gst")
        tpr = Ring(ph, [P(f"tp{i}", [128, 8, 128], F32) for i in range(2)], "tp")
        mmr = Ring(ph, [P(f"mm{i}", [128, 512], F32) for i in range(4)], "mm")
        GTv = C.GT
        ev = 0
        nxt = hin.next()
        ph.dma("sp", nxt[0][:], C.H[0:128, :], writes=[nxt[1]])
        for n in range(T // 512):
            ht, htbs = hT_t[n % 2], hT_b[n % 2]
            for sub in range(4):
                htb = htbs[sub]
                i = 4 * n + sub
                h_in, hib = nxt
                if i + 1 < NT:
                    nxt = hin.next()
                    ph.dma("sp", nxt[0][:], C.H[(i + 1) * 128:(i + 2) * 128, :], writes=[nxt[1]])
                tp, tpb = tpr.next()
                for k in range(8):
                    o_tp(ph, tp[:, k, :], h_in[:, k * 128:(k + 1) * 128], C.identf[:], [hib], [tpb])
                o_cp(ph, "dve", ht[:, :, sub * 128:(sub + 1) * 128], tp[:], [tpb], [htb])
                qk, qkb = qks.next()
                va, vab = vas.next()
                vb, vbb = vbs.next()
                vc, vcb = vcs.next()
                for (c0, wd, kind, dst) in SEGS:
                    mm, mmb = mmr.next()
                    for k in range(8):
                        o_mm(ph, mm[:, 0:wd], ht[:, k, sub * 128:(sub + 1) * 128], w[:, k, c0:c0 + wd],
                             k == 0, k == 7, [htb, wb] + wbs, [mmb])
                    eng = "dve" if ev % 2 == 0 else "act"
                    ev += 1
                    if kind == "qk":
                        o_cp(ph, eng, qk[:, dst:dst + wd], mm[:, 0:wd], [mmb], [qkb])
                    elif kind == "va":
                        o_cp(ph, eng, va[:, :, 0:64], mm[:, 0:128].rearrange("p (h d) -> p h d", h=2), [mmb], [vab])
                    elif kind == "vb":
                        o_cp(ph, eng, vb[:, dst, :, 0:64], mm[:, 0:256].rearrange("p (h d) -> p h d", h=4), [mmb], [vbb])
                    else:
                        o_cp(ph, eng, vc[:, :, 0:64], mm[:, 0:512].rearrange("p (h d) -> p h d", h=8), [mmb], [vcb])
                r0 = PADR + i * 128
                ph.dma("sp", C.QK[r0:r0 + 128, :], qk[:], reads=[qkb])
                ph.dma("sp", C.VA[r0:r0 + 128, :], va[:].rearrange("p h d -> p (h d)"), reads=[vab])
                for gi in range(3):
                    ph.dma("sp", C.VB[gi][r0:r0 + 128, :], vb[:, gi, :, :].rearrange("p h d -> p (h d)"), reads=[vbb])
                ph.dma("sp", C.VC[r0:r0 + 128, :], vc[:].rearrange("p h d -> p (h d)"), reads=[vcb])
            for cg in range(4):
                gs, gsb = gst.next()
                for a in range(6):
                    c = cg * 6 + a
                    mm, mmb = mmr.next()
                    for k in range(8):
                        o_mm(ph, mm[:], w[:, k, GATE0 + c * 128:GATE0 + (c + 1) * 128], ht[:, k, :],
                             k == 0, k == 7, htbs + [wb] + wbs, [mmb])
                    o_act(ph, gs[:, a, :], mm[:], AF.Sigmoid, [mmb, wb], [gsb], bias=bg[:, c:c + 1], scale=1.0)
                ph.dma("sp", GTv[cg * 768:(cg + 1) * 768, n * 512:(n + 1) * 512].rearrange("(a p) t -> p a t", p=128),
                       gs[:], reads=[gsb])
        ph.emit()


def phase_attn_a(C, l):
    from contextlib import ExitStack
    nc = C.nc
    with ExitStack() as st:
        A, P = mk_alloc(nc, st, f"pa_{l}_")
        ph = Phase(C.S, f"attnA{l}")
        es = A("es", [128, 8], F32)
        esb = ph.buf("es")
        ph.dma("sp", es[:], C.sink[l].to_broadcast([128, 8]), writes=[esb])
        o_act(ph, es[:], es[:], AF.Exp, [esb], [esb])
        qr = Ring(ph, [A(f"q{i}", [128, 512], BF16) for i in range(2)], "q")
        kdr = Ring(ph, [A(f"kd{i}", [128, 3, 2, 2, 64], BF16) for i in range(2)], "kd")
        vr = Ring(ph, [A(f"v{i}", [128, 3, 130], BF16) for i in range(2)], "v")
        qTr = Ring(ph, [A(f"qT{i}", [128, 4, 128], BF16) for i in range(2)], "qT")
        kl_t = [A(f"kTl{i}", [128, 6, 128], BF16) for i in range(2)]
        kh_t = [A(f"kTh{i}", [128, 6, 128], BF16) for i in range(2)]
        klr = Ring(ph, kl_t, "kTl")
        khr = Ring(ph, kh_t, "kTh")
        for i in range(2):
            o_memset(ph, "pool", kl_t[i][:], 0.0, [klr.bufs[i]])
            o_memset(ph, "pool", kh_t[i][:], 0.0, [khr.bufs[i]])
        ptr = Ring(ph, [A(f"pt{i}", [128, 1536], BF16) for i in range(2)], "pt")
        oar = Ring(ph, [A(f"oa{i}", [128, 512], BF16) for i in range(2)], "oa")
        dnr = Ring(ph, [A(f"dn{i}", [128, 8], F32) for i in range(2)], "dn")
        otr = Ring(ph, [A(f"ot{i}", [128, 4, 512], BF16) for i in range(2)], "ot")
        tpq = Ring(ph, [P("tpq", [128, 1024], BF16)], "tpq")
        tpk = Ring(ph, [P("tpk", [128, 1024], BF16)], "tpk")
        psr = Ring(ph, [P("ps", [128, 1536], F32)], "ps")
        por = Ring(ph, [P(f"po{i}", [128, 512], F32) for i in range(2)], "po")
        OTv = C.OT[0:512, :].rearrange("(i p) t -> p i t", p=128)

        def loads(b):
            q, qb = qr.next()
            kd, kdb = kdr.next()
            v, vb = vr.next()
            r0 = PADR + 128 * b
            ph.dma("sp", q[:], C.QK[r0:r0 + 128, QK_AQ:QK_AQ + 512], writes=[qb])
            for g in range(2):
                ksrc = C.QK[r0 - 128:r0 + 256, QK_AK + 64 * g:QK_AK + 64 * g + 64].rearrange("(c p) d -> p c d", p=128)
                for a in range(2):
                    ph.dma("sp", kd[:, :, g, a, :], ksrc, writes=[kdb])
            ph.dma("sp", v[:], C.VA[r0 - 128:r0 + 256, :].rearrange("(c p) d -> p c d", p=128), writes=[vb])
            return (q, qb, kd, kdb, v, vb)

        nxt = loads(0)
        ot, otb = None, None
        for b in range(NT):
            q, qb, kd, kdb, v, vb = nxt
            if b + 1 < NT:
                nxt = loads(b + 1)
            tq, tqb = tpq.next()
            for i in range(4):
                o_tp(ph, tq[:, i * 128:(i + 1) * 128], q[:, i * 128:(i + 1) * 128], C.identb[:], [qb], [tqb])
            qT, qTb = qTr.next()
            o_cp(ph, "dve", qT[:].rearrange("p i t -> p (i t)"), tq[:, 0:512], [tqb], [qTb])
            tk, tkb = tpk.next()
            for c in range(3):
                for g in range(2):
                    o_tp(ph, tk[:, (c * 2 + g) * 128:(c * 2 + g + 1) * 128],
                         kd[:, c, g, :, :].rearrange("p a d -> p (a d)"), C.identb[:], [kdb], [tkb])
            kl, klb = klr.next()
            kh, khb = khr.next()
            o_cp(ph, "act", kl[0:64, :, :].rearrange("p s t -> p (s t)"), tk[0:64, 0:768], [tkb], [klb])
            o_cp(ph, "dve", kh[64:128, :, :].rearrange("p s t -> p (s t)"), tk[64:128, 0:768], [tkb], [khb])
            oa, oab = oar.next()
            dn, dnb = dnr.next()
            for g in range(2):
                ps, psb = psr.next()
                for c in range(3):
                    for j in range(4):
                        h = 4 * g + j
                        i, par = h // 2, h % 2
                        kk_, kkb = (kl, klb) if par == 0 else (kh, khb)
                        o_mm(ph, ps[:, (c * 4 + j) * 128:(c * 4 + j + 1) * 128], kk_[:, c * 2 + g, :],
                             qT[:, i, :], True, True, [kkb, qTb], [psb])
                pt, ptb = ptr.next()
                for c in range(3):
                    o_act(ph, pt[:, c * 512:(c + 1) * 512], ps[:, c * 512:(c + 1) * 512], AF.Exp, [psb], [ptb], scale=0.125)
                o_tt(ph, "dve", pt[:], pt[:], C.EA[:, g * 1536:(g + 1) * 1536], ALU.mult, [ptb], [ptb])
                po, pob = por.next()
                for j in range(4):
                    for c in range(3):
                        o_mm(ph, po[:, j * 65:(j + 1) * 65], pt[:, (c * 4 + j) * 128:(c * 4 + j + 1) * 128],
                             v[:, c, g * 65:(g + 1) * 65], c == 0, c == 2, [ptb, vb], [pob])
                pov = po[:, 0:260].rearrange("p (j d) -> p j d", j=4)
                o_tt(ph, "dve", dn[:, g * 4:(g + 1) * 4], pov[:, :, 64], es[:, g * 4:(g + 1) * 4], ALU.add, [pob, esb], [dnb])
                o_rcp(ph, dn[:, g * 4:(g + 1) * 4], dn[:, g * 4:(g + 1) * 4], [dnb], [dnb])
                o_tt(ph, "dve", oa[:, g * 256:(g + 1) * 256].rearrange("p (j d) -> p j d", j=4), pov[:, :, 0:64],
                     dn[:, g * 4:(g + 1) * 4].unsqueeze(2).to_broadcast([128, 4, 64]), ALU.mult, [pob, dnb], [oab])
            to, tob = tpq.next()
            for i in range(4):
                o_tp(ph, to[:, 512 + i * 128:512 + (i + 1) * 128], oa[:, i * 128:(i + 1) * 128], C.identb[:], [oab], [tob])
            if b % 4 == 0:
                ot, otb = otr.next()
            o_cp(ph, "act", ot[:, :, (b % 4) * 128:(b % 4 + 1) * 128], to[:, 512:1024].rearrange("p (i t) -> p i t", i=4), [tob], [otb])
            if b % 4 == 3:
                ph.dma("sp", OTv[:, :, (b // 4) * 512:(b // 4 + 1) * 512], ot[:], reads=[otb])
        ph.emit()


def phase_attn_b(C, l, gi):
    from contextlib import ExitStack
    nc = C.nc
    dil = B_CFG[gi][1]
    Ls = T // dil
    nb = Ls // 128
    NBK = 4
    with ExitStack() as st:
        A, P = mk_alloc(nc, st, f"pb_{l}_{gi}_")
        q_t = [A(f"q{i}", [128, 256], BF16) for i in range(NBK)]
        k_t = [A(f"k{i}", [128, 2, 256], BF16) for i in range(NBK)]
        v_t = [A(f"v{i}", [128, 2, 260], BF16) for i in range(NBK)]
        qT_t = [A(f"qT{i}", [128, 2, 128], BF16) for i in range(2)]
        kl_t = [A(f"kTl{i}", [128, 4, 128], BF16) for i in range(2)]
        kh_t = [A(f"kTh{i}", [128, 4, 128], BF16) for i in range(2)]
        pt_t = [A(f"pt{i}", [128, 1024], BF16) for i in range(NBK)]
        sb_t = [A(f"sb{i}", [128, 1024], F32) for i in range(2)]
        nb_t = [A(f"nb{i}", [128, 260], F32) for i in range(NBK)]
        tp_t = [P(f"tp{i}", [128, 1024], BF16) for i in range(2)]
        ps_t = [P("ps", [128, 1024], F32)]
        po_t = [P(f"po{i}", [128, 512], F32) for i in range(NBK)]
        QKv = C.QK.rearrange("(s d) c -> d s c", d=dil)
        VBv = C.VB[gi].rearrange("(s d) c -> d s c", d=dil)
        NBv = C.NB[gi].rearrange("(s d) c -> d s c", d=dil)
        sp0 = PADR // dil
        ph = Phase(C.S, f"attnB{l}_{gi}_init")
        zb = ph.buf("z")
        for i in range(2):
            o_memset(ph, "pool", kl_t[i][:], 0.0, [zb])
            o_memset(ph, "pool", kh_t[i][:], 0.0, [zb])
        ph.emit()
        blocks = [(r, b) for r in range(dil) for b in range(nb)]
        for g0 in range(0, len(blocks), NBK):
            grp = blocks[g0:g0 + NBK]
            ph = Phase(C.S, f"attnB{l}_{gi}_{g0}")
            tpr = Ring(ph, tp_t, "tp")
            psr = Ring(ph, ps_t, "ps")
            qTr = Ring(ph, qT_t, "qT")
            klr = Ring(ph, kl_t, "kl")
            khr = Ring(ph, kh_t, "kh")
            sbr = Ring(ph, sb_t, "sb")
            ld = []
            for i, (r, b) in enumerate(grp):
                qb, kb, vb = ph.buf(), ph.buf(), ph.buf()
                s0 = sp0 + 128 * b
                ph.dma("sp", q_t[i][:], QKv[r, s0:s0 + 128, QK_BQ[gi]:QK_BQ[gi] + 256], writes=[qb])
                ph.dma("sp", k_t[i][:], QKv[r, s0 - 64:s0 + 192, QK_BK[gi]:QK_BK[gi] + 256].rearrange("(j p) d -> p j d", p=128), writes=[kb])
                ph.dma("sp", v_t[i][:], VBv[r, s0 - 64:s0 + 192, :].rearrange("(j p) d -> p j d", p=128), writes=[vb])
                ld.append((qb, kb, vb))
            ptbs = []
            for i, (r, b) in enumerate(grp):
                q, k = q_t[i], k_t[i]
                qb, kb, vb = ld[i]
                tp, tpb = tpr.next()
                for a in range(2):
                    o_tp(ph, tp[:, a * 128:(a + 1) * 128], q[:, a * 128:(a + 1) * 128], C.identb[:], [qb], [tpb])
                for jp in range(2):
                    for a in range(2):
                        sl = 2 + 2 * jp + a
                        o_tp(ph, tp[:, sl * 128:(sl + 1) * 128], k[:, jp, a * 128:(a + 1) * 128], C.identb[:], [kb], [tpb])
                qT, qTb = qTr.next()
                kl, klb = klr.next()
                kh, khb = khr.next()
                o_cp(ph, "dve", qT[:].rearrange("p s t -> p (s t)"), tp[:, 0:256], [tpb], [qTb])
                o_cp(ph, "act", kl[0:64, :, :].rearrange("p s t -> p (s t)"), tp[0:64, 256:768], [tpb], [klb])
                o_cp(ph, "dve", kh[64:128, :, :].rearrange("p s t -> p (s t)"), tp[64:128, 256:768], [tpb], [khb])
                ps, psb = psr.next()
                for jp in range(2):
                    for h in range(4):
                        a, par = h // 2, h % 2
                        kk_, kkb = (kl, klb) if par == 0 else (kh, khb)
                        o_mm(ph, ps[:, (jp * 4 + h) * 128:(jp * 4 + h + 1) * 128], kk_[:, 2 * jp + a, :],
                             qT[:, a, :], True, True, [kkb, qTb], [psb])
                sb_, sbb = sbr.next()
                for jp in range(2):
                    o_stt(ph, sb_[:, jp * 512:(jp + 1) * 512], ps[:, jp * 512:(jp + 1) * 512], 0.125,
                          C.BB[:, gi * 1024 + jp * 512:gi * 1024 + (jp + 1) * 512], ALU.mult, ALU.add, [psb], [sbb])
                ptb = ph.buf()
                o_act(ph, pt_t[i][:], sb_[:], AF.Exp, [sbb], [ptb])
                ptbs.append(ptb)
            pobs = []
            for i, (r, b) in enumerate(grp):
                pt, v, po = pt_t[i], v_t[i], po_t[i]
                pob = ph.buf()
                for h in range(4):
                    for jp in range(2):
                        o_mm(ph, po[:, h * 65:(h + 1) * 65], pt[:, (jp * 4 + h) * 128:(jp * 4 + h + 1) * 128],
                             v[:, jp, h * 65:(h + 1) * 65], jp == 0, jp == 1, [ptbs[i], ld[i][2]], [pob])
                pobs.append(pob)
            for i, (r, b) in enumerate(grp):
                nbb = ph.buf()
                o_cp(ph, "act" if i % 2 == 0 else "dve", nb_t[i][:], po_t[i][:, 0:260], [pobs[i]], [nbb])
                ph.dma("sp", NBv[r, 128 * b:128 * (b + 1), :], nb_t[i][:], reads=[nbb])
            ph.emit()


def phase_attn_bc(C, l):
    from contextlib import ExitStack
    nc = C.nc
    with ExitStack() as st:
        A, P = mk_alloc(nc, st, f"pbc_{l}_")
        ph = Phase(C.S, f"attnBc{l}")
        nr = [Ring(ph, [A(f"n{g}_{i}", [128, 260], F32) for i in range(2)], f"n{g}") for g in range(3)]
        dnr = Ring(ph, [A(f"dn{i}", [128, 4], F32) for i in range(2)], "dn")
        obr = Ring(ph, [A(f"ob{i}", [128, 256], BF16) for i in range(2)], "ob")
        otr = Ring(ph, [A(f"ot{i}", [128, 2, 512], BF16) for i in range(2)], "ot")
        tpr = Ring(ph, [P(f"tp{i}", [128, 1024], BF16) for i in range(2)], "tp")
        OTv = C.OT[512:768, :].rearrange("(i p) t -> p i t", p=128)

        def loads(i):
            res = []
            for g in range(3):
                n, nb_ = nr[g].next()
                ph.dma("sp", n[:], C.NB[g][i * 128:(i + 1) * 128, :], writes=[nb_])
                res.append((n, nb_))
            return res

        nxt = loads(0)
        ot, otb = None, None
        for i in range(NT):
            (n0, b0), (n1, b1), (n2, b2) = nxt
            if i + 1 < NT:
                nxt = loads(i + 1)
            o_tt(ph, "pool", n0[:], n0[:], n1[:], ALU.add, [b0, b1], [b0])
            o_tt(ph, "pool", n0[:], n0[:], n2[:], ALU.add, [b0, b2], [b0])
            nv = n0[:].rearrange("p (h d) -> p h d", h=4)
            dn, dnb = dnr.next()
            o_rcp(ph, dn[:], nv[:, :, 64], [b0], [dnb])
            ob, obb = obr.next()
            o_tt(ph, "dve", ob[:].rearrange("p (h d) -> p h d", h=4), nv[:, :, 0:64],
                 dn[:].unsqueeze(2).to_broadcast([128, 4, 64]), ALU.mult, [b0, dnb], [obb])
            tp, tpb = tpr.next()
            for a in range(2):
                o_tp(ph, tp[:, a * 128:(a + 1) * 128], ob[:, a * 128:(a + 1) * 128], C.identb[:], [obb], [tpb])
            if i % 4 == 0:
                ot, otb = otr.next()
            o_cp(ph, "act", ot[:, :, (i % 4) * 128:(i % 4 + 1) * 128], tp[:, 0:256].rearrange("p (a t) -> p a t", a=2), [tpb], [otb])
            if i % 4 == 3:
                ph.dma("sp", OTv[:, :, (i // 4) * 512:(i // 4 + 1) * 512], ot[:], reads=[otb])
        ph.emit()


def phase_attn_c(C, l):
    from contextlib import ExitStack
    nc = C.nc
    NTL = 2
    with ExitStack() as st:
        A, P = mk_alloc(nc, st, f"pc_{l}_")
        EC = A("EC", [128, 5, 5120], BF16)
        tmp = A("ect", [128, 5120], F32)
        q_t = [A(f"q{i}", [128, 512], BF16) for i in range(NTL)]
        k_t = [A(f"k{i}", [128, 5, 512], BF16) for i in range(NTL)]
        v_t = [A(f"v{i}", [128, 5, 520], BF16) for i in range(NTL)]
        qT_t = [A(f"qT{i}", [128, 4, 128], BF16) for i in range(NTL)]
        kl_t = [A(f"kTl{i}", [128, 5, 4, 128], BF16) for i in range(NTL)]
        kh_t = [A(f"kTh{i}", [128, 5, 4, 128], BF16) for i in range(NTL)]
        C_pt = [A(f"pt{i}", [128, 640], BF16) for i in range(8 * NTL)]
        oc_t = [A(f"oc{i}", [128, 512], BF16) for i in range(NTL)]
        dn_t = [A(f"dn{i}", [128, 8], F32) for i in range(NTL)]
        tps = [P("tp0", [128, 1024], BF16)]
        psx = P("psx", [128, 1536], F32)
        po_t = [P(f"po{i}", [128, 512], F32) for i in range(2 * NTL)]
        OTv = C.OT[768:1280, :].rearrange("(i p) t -> p i t", p=128)
        ph = Phase(C.S, f"attnC{l}_init")
        zb = ph.buf("z")
        for i in range(NTL):
            o_memset(ph, "pool", kl_t[i][:], 0.0, [zb])
            o_memset(ph, "pool", kh_t[i][:], 0.0, [zb])
        ecb = ph.buf("EC")
        tb = ph.buf("tmp")
        for pi in range(5):
            ph.dma("sp", tmp[:], C.biasC[l, pi], writes=[tb])
            o_act(ph, EC[:, pi, :], tmp[:], AF.Exp, [tb], [ecb])
        ph.emit()
        for j0 in range(0, NT, NTL):
            ph = Phase(C.S, f"attnC{l}_{j0}")
            tpr = Ring(ph, tps, "tp")
            ps_b = [ph.buf("psA"), ph.buf("psB")]
            ps_v = [psx[:, 0:640], psx[:, 768:1408]]
            psk = 0
            info = []
            for t in range(NTL):
                j = j0 + t
                qb, kb, vb = ph.buf(), ph.buf(), ph.buf()
                cb0 = min(max(j - 2, 0), 59)
                r0 = PADR + 128 * j
                k0 = PADR + 128 * cb0
                ph.dma("sp", q_t[t][:], C.QK[r0:r0 + 128, QK_CQ:QK_CQ + 512], writes=[qb])
                ph.dma("sp", k_t[t][:], C.QK[k0:k0 + 640, QK_CK:QK_CK + 512].rearrange("(c p) d -> p c d", p=128), writes=[kb])
                ph.dma("sp", v_t[t][:], C.VC[k0:k0 + 640, :].rearrange("(c p) d -> p c d", p=128), writes=[vb])
                info.append((j, qb, kb, vb))
            ptbs = {}
            for t in range(NTL):
                j, qb, kb, vb = info[t]
                q, k, qT, kl, kh = q_t[t], k_t[t], qT_t[t], kl_t[t], kh_t[t]
                qTb, klb, khb = ph.buf(), ph.buf(), ph.buf()
                pat = c_pattern(j)
                tp, tpb = tpr.next()
                for i in range(4):
                    o_tp(ph, tp[:, i * 128:(i + 1) * 128], q[:, i * 128:(i + 1) * 128], C.identb[:], [qb], [tpb])
                    o_tp(ph, tp[:, (4 + i) * 128:(5 + i) * 128], k[:, 0, i * 128:(i + 1) * 128], C.identb[:], [kb], [tpb])
                o_cp(ph, "dve", qT[:].rearrange("p i t -> p (i t)"), tp[:, 0:512], [tpb], [qTb])
                o_cp(ph, "act", kl[0:64, 0, :, :].rearrange("p i t -> p (i t)"), tp[0:64, 512:1024], [tpb], [klb])
                o_cp(ph, "dve", kh[64:128, 0, :, :].rearrange("p i t -> p (i t)"), tp[64:128, 512:1024], [tpb], [khb])
                for f in range(2):
                    tp, tpb = tpr.next()
                    for cc in range(2):
                        c = 1 + 2 * f + cc
                        for i in range(4):
                            o_tp(ph, tp[:, (cc * 4 + i) * 128:(cc * 4 + i + 1) * 128], k[:, c, i * 128:(i + 1) * 128], C.identb[:], [kb], [tpb])
                    o_cp(ph, "act", kl[0:64, 1 + 2 * f:3 + 2 * f, :, :].rearrange("p c i t -> p (c i t)"), tp[0:64, :], [tpb], [klb])
                    o_cp(ph, "dve", kh[64:128, 1 + 2 * f:3 + 2 * f, :, :].rearrange("p c i t -> p (c i t)"), tp[64:128, :], [tpb], [khb])
                for h in range(8):
                    i, par = h // 2, h % 2
                    kk_, kkb = (kl, klb) if par == 0 else (kh, khb)
                    ps, psb = ps_v[psk % 2], ps_b[psk % 2]
                    psk += 1
                    for c in range(5):
                        o_mm(ph, ps[:, c * 128:(c + 1) * 128], kk_[:, c, i, :], qT[:, i, :], True, True, [kkb, qTb], [psb])
                    pt = C_pt[t * 8 + h]
                    ptb = ph.buf()
                    ptbs[(t, h)] = ptb
                    o_act(ph, pt[:], ps, AF.Exp, [psb], [ptb], scale=0.125)
                    o_tt(ph, "dve", pt[:].rearrange("p (c t) -> p c t", c=5), pt[:].rearrange("p (c t) -> p c t", c=5),
                         EC[:, pat, :].rearrange("p (c h t) -> p c h t", c=5, h=8)[:, :, h, :], ALU.mult, [ptb], [ptb])
            po_b = {}
            for t in range(NTL):
                j, qb, kb, vb = info[t]
                for h in range(8):
                    pt, ptb = C_pt[t * 8 + h], ptbs[(t, h)]
                    a = t * 2 + h // 4
                    if a not in po_b:
                        po_b[a] = ph.buf()
                    po, pob = po_t[a], po_b[a]
                    for c in range(5):
                        o_mm(ph, po[:, (h % 4) * 65:(h % 4 + 1) * 65], pt[:, c * 128:(c + 1) * 128],
                             v_t[t][:, c, h * 65:(h + 1) * 65], c == 0, c == 4, [ptb, vb], [pob])
            for t in range(NTL):
                j = info[t][0]
                oc, dn = oc_t[t], dn_t[t]
                ocb, dnb = ph.buf(), ph.buf()
                for a2 in range(2):
                    a = t * 2 + a2
                    pov = po_t[a][:, 0:260].rearrange("p (j d) -> p j d", j=4)
                    o_rcp(ph, dn[:, a2 * 4:(a2 + 1) * 4], pov[:, :, 64], [po_b[a]], [dnb])
                    o_tt(ph, "dve", oc[:, a2 * 256:(a2 + 1) * 256].rearrange("p (j d) -> p j d", j=4), pov[:, :, 0:64],
                         dn[:, a2 * 4:(a2 + 1) * 4].unsqueeze(2).to_broadcast([128, 4, 64]), ALU.mult, [po_b[a], dnb], [ocb])
                ph.dma("sp", C.OC[j * 128:(j + 1) * 128, :], oc[:], reads=[ocb])
            ph.emit()
        ph = Phase(C.S, f"attnC{l}_tr")
        ocr = Ring(ph, [A(f"oc2_{i}", [128, 512], BF16) for i in range(2)], "oc2")
        otr = Ring(ph, [A(f"ot2_{i}", [128, 4, 512], BF16) for i in range(2)], "ot2")
        tpr = Ring(ph, [tps[0]], "tp")
        nxt = ocr.next()
        ph.dma("sp", nxt[0][:], C.OC[0:128, :], writes=[nxt[1]])
        ot2, ot2b = None, None
        for j in range(NT):
            oc2, oc2b = nxt
            if j + 1 < NT:
                nxt = ocr.next()
                ph.dma("sp", nxt[0][:], C.OC[(j + 1) * 128:(j + 2) * 128, :], writes=[nxt[1]])
            tp, tpb = tpr.next()
            for i in range(4):
                o_tp(ph, tp[:, i * 128:(i + 1) * 128], oc2[:, i * 128:(i + 1) * 128], C.identb[:], [oc2b], [tpb])
            if j % 4 == 0:
                ot2, ot2b = otr.next()
            o_cp(ph, "act", ot2[:, :, (j % 4) * 128:(j % 4 + 1) * 128], tp[:, 0:512].rearrange("p (i t) -> p i t", i=4), [tpb], [ot2b])
            if j % 4 == 3:
                ph.dma("sp", OTv[:, :, (j // 4) * 512:(j // 4 + 1) * 512], ot2[:], reads=[ot2b])
        ph.emit()


def phase_merge(C, l):
    from contextlib import ExitStack
    nc = C.nc
    with ExitStack() as st:
        A, P = mk_alloc(nc, st, f"p3_{l}_")
        ph = Phase(C.S, f"merge{l}")
        ln_setup(ph, C, A)
        g, b, cb = load_gb(ph, A, C.ln1g[l], C.ln1b[l], "ln1")
        wbr = A("wbr", [128, 10, 1024], BF16)
        wo = A("wo", [128, 8, 1024], BF16)
        wb = ph.buf("w")
        srcs = [(C.w_bra[l], 4), (C.w_brb[l], 2), (C.w_brc[l], 4)]
        kk = 0
        wbs = []
        for src, nk in srcs:
            for k in range(nk):
                wbc = ph.buf()
                wbs.append(wbc)
                ph.dma("pool", wbr[:, kk, :], src[k * 128:(k + 1) * 128, :], writes=[wbc])
                kk += 1
        for k in range(8):
            wbc = ph.buf()
            wbs.append(wbc)
            ph.dma("pool", wo[:, k, :], C.w_out[l, k * 128:(k + 1) * 128, :], writes=[wbc])
        otr = Ring(ph, [A(f"ot{i}", [128, 10, 512], BF16) for i in range(2)], "ot")
        gtr = Ring(ph, [A(f"gt{i}", [128, 24, 512], BF16) for i in range(2)], "gt")
        t1r = Ring(ph, [A(f"t1_{i}", [128, 512], F32) for i in range(2)], "t1")
        t2r = Ring(ph, [A(f"t2_{i}", [128, 512], F32) for i in range(2)], "t2")
        t3r = Ring(ph, [A(f"t3_{i}", [128, 512], F32) for i in range(2)], "t3")
        mgr = Ring(ph, [A(f"mg{i}", [128, 8, 512], BF16) for i in range(2)], "mg")
        hr = Ring(ph, [A(f"h{i}", [128, D], F32) for i in range(2)], "h")
        zr = Ring(ph, [A(f"z{i}", [128, D], F32) for i in range(5)], "z")
        orr = Ring(ph, [A(f"o{i}", [128, D], F32) for i in range(3)], "o")
        obr = Ring(ph, [A(f"ob{i}", [128, D], BF16) for i in range(2)], "ob")
        par = Ring(ph, [P(f"pa{i}", [128, 512], F32) for i in range(2)], "pa")
        pbr = Ring(ph, [P(f"pb{i}", [128, 512], F32) for i in range(2)], "pb")
        pcr = Ring(ph, [P(f"pc{i}", [128, 512], F32) for i in range(2)], "pc")
        pyr = Ring(ph, [P("py", [128, 1024], F32)], "py")
        OTv = C.OT.rearrange("(i p) t -> p i t", p=128)
        GTv = C.GT.rearrange("(i p) t -> p i t", p=128)

        def loads(n):
            ot, otb = otr.next()
            gt, gtb = gtr.next()
            ph.dma("sp", ot[:], OTv[:, :, n * 512:(n + 1) * 512], writes=[otb])
            for a in range(3):
                ph.dma("sp", gt[:, a * 8:(a + 1) * 8, :], GTv[:, a * 8:(a + 1) * 8, n * 512:(n + 1) * 512], writes=[gtb])
            return ot, otb, gt, gtb

        nxt = loads(0)
        for n in range(T // 512):
            ot, otb, gt, gtb = nxt
            if n + 1 < T // 512:
                nxt = loads(n + 1)
            mg, mgb = mgr.next()
            for mc in range(8):
                pa, pab = par.next()
                pb, pbb = pbr.next()
                pc, pcb = pcr.next()
                for (pp, ppb, k0, nk) in ((pa, pab, 0, 4), (pb, pbb, 4, 2), (pc, pcb, 6, 4)):
                    for k in range(nk):
                        o_mm(ph, pp[:], wbr[:, k0 + k, mc * 128:(mc + 1) * 128], ot[:, k0 + k, :], k == 0, k == nk - 1, wbs + [otb], [ppb])
                t1, t1b = t1r.next()
                t2, t2b = t2r.next()
                t3, t3b = t3r.next()
                o_tt(ph, "dve", t1[:], pa[:], gt[:, mc, :], ALU.mult, [pab, gtb], [t1b])
                o_tt(ph, "dve", t2[:], pb[:], gt[:, 8 + mc, :], ALU.mult, [pbb, gtb], [t2b])
                o_tt(ph, "dve", t3[:], pc[:], gt[:, 16 + mc, :], ALU.mult, [pcb, gtb], [t3b])
                o_tt(ph, "pool", t1[:], t1[:], t2[:], ALU.add, [t1b, t2b], [t1b])
                o_tt(ph, "pool", mg[:, mc, :], t1[:], t3[:], ALU.add, [t1b, t3b], [mgb])
            subs = []
            for sub in range(4):
                i = 4 * n + sub
                h, hb = hr.next()
                ph.dma("sp", h[:], C.H[i * 128:(i + 1) * 128, :], writes=[hb])
                py, pyb = pyr.next()
                for half in range(2):
                    for k in range(8):
                        o_mm(ph, py[:, half * 512:(half + 1) * 512], mg[:, k, sub * 128:(sub + 1) * 128],
                             wo[:, k, half * 512:(half + 1) * 512], k == 0, k == 7, [mgb] + wbs, [pyb])
                z, zb = zr.next()
                o_stt(ph, z[:], h[:], ALPHA, py[:], ALU.mult, ALU.add, [hb, pyb], [zb])
                mv, mvb = ln_s1(ph, C, z[:], zb)
                subs.append((i, z, zb, mv, mvb))
            for (i, z, zb, mv, mvb) in subs:
                ln_s2(ph, C, mv, mvb)
            for (i, z, zb, mv, mvb) in subs:
                o, ob = orr.next()
                ln_s3(ph, C, z[:], zb, mv, mvb, o[:], ob, g[:], b[:], cb, geng="dve", beng="pool")
                ph.dma("sp", C.H1[i * 128:(i + 1) * 128, :], o[:], reads=[ob])
                o16, o16b = obr.next()
                o_cp(ph, "act", o16[:], o[:], [ob], [o16b])
                ph.dma("sp", C.H1b[i * 128:(i + 1) * 128, :], o16[:], reads=[o16b])
        ph.emit()


def phase_route(C, l):
    from contextlib import ExitStack
    nc = C.nc
    with ExitStack() as st:
        A, P = mk_alloc(nc, st, f"p3b_{l}_")
        ph = Phase(C.S, f"route{l}")
        wr = A("wr", [128, 8, 36], F32)
        brr = A("brr", [128, 36], F32)
        wb = ph.buf("w")
        ph.dma("sp", wr[:], C.wr[l].rearrange("(k p) e -> p k e", p=128), writes=[wb])
        ph.dma("sp", brr[:], C.br[l].to_broadcast([128, 36]), writes=[wb])
        LG = A("LG", [128, NT, 36], F32)
        lgb = ph.buf("LG")
        hr = Ring(ph, [A(f"h{i}", [128, D], F32) for i in range(3)], "h")
        hTr = Ring(ph, [A(f"hT{i}", [128, 8, 128], F32) for i in range(3)], "hT")
        tpr = Ring(ph, [P(f"tp{i}", [128, 8, 128], F32) for i in range(2)], "tp")
        plr = Ring(ph, [P(f"pl{i}", [128, 512], F32) for i in range(2)], "pl")
        ppr = Ring(ph, [P("pp0", [128, 512], F32)], "pp")
        pcr = Ring(ph, [P("pcs0", [128, 512], F32)], "pcs")
        nxt = hr.next()
        ph.dma("sp", nxt[0][:], C.H1[0:128, :], writes=[nxt[1]])
        for i in range(NT):
            h, hb = nxt
            if i + 1 < NT:
                nxt = hr.next()
                ph.dma("sp", nxt[0][:], C.H1[(i + 1) * 128:(i + 2) * 128, :], writes=[nxt[1]])
            tp, tpb = tpr.next()
            for k in range(8):
                o_tp(ph, tp[:, k, :], h[:, k * 128:(k + 1) * 128], C.identf[:], [hb], [tpb])
            hT, hTb = hTr.next()
            o_cp(ph, "act", hT[:], tp[:], [tpb], [hTb])
            pl, plb = plr.next()
            for k in range(8):
                o_mm(ph, pl[:, 0:36], hT[:, k, :], wr[:, k, :], k == 0, k == 7, [hTb, wb], [plb])
            o_tt(ph, "dve", LG[:, i, :], pl[:, 0:36], brr[:], ALU.add, [plb, wb], [lgb])
        def S_(name, shape, dt=F32):
            return A(name, shape, dt), ph.buf(name)
        lg = LG[:, :, 0:4]
        le = LG[:, :, 4:36].rearrange("p n (g e) -> p n g e", g=4)
        gmax, gmb = S_("gmax", [128, NT])
        o_red(ph, gmax[:], lg, ALU.max, [lgb], [gmb])
        goh, gohb = S_("goh", [128, NT, 4])
        o_tt(ph, "dve", goh[:], lg, gmax[:].unsqueeze(2).to_broadcast([128, NT, 4]), ALU.is_equal, [lgb, gmb], [gohb])
        gex, gexb = S_("gex", [128, NT, 4])
        o_tt(ph, "dve", gex[:], lg, gmax[:].unsqueeze(2).to_broadcast([128, NT, 4]), ALU.subtract, [lgb, gmb], [gexb])
        o_act(ph, gex[:], gex[:], AF.Exp, [gexb], [gexb])
        gw, gwb = S_("gw", [128, NT])
        o_red(ph, gw[:], gex[:], ALU.add, [gexb], [gwb])
        o_rcp(ph, gw[:], gw[:], [gwb], [gwb])
        esel, eselb = S_("esel", [128, NT, 8])
        etmp, etmpb = S_("etmp", [128, NT, 8])
        for g in range(4):
            dst, dstb = (esel, eselb) if g == 0 else (etmp, etmpb)
            o_tt(ph, "dve", dst[:], le[:, :, g, :], goh[:, :, g].unsqueeze(2).to_broadcast([128, NT, 8]), ALU.mult, [lgb, gohb], [dstb])
            if g > 0:
                o_tt(ph, "dve", esel[:], esel[:], etmp[:], ALU.add, [eselb, etmpb], [eselb])
        m1, m1b = S_("m1", [128, NT])
        o_red(ph, m1[:], esel[:], ALU.max, [eselb], [m1b])
        oh1, oh1b = S_("oh1", [128, NT, 8])
        o_tt(ph, "dve", oh1[:], esel[:], m1[:].unsqueeze(2).to_broadcast([128, NT, 8]), ALU.is_equal, [eselb, m1b], [oh1b])
        e2, e2b = S_("e2", [128, NT, 8])
        o_ts(ph, "dve", e2[:], oh1[:], -1e30, None, ALU.mult, None, [oh1b], [e2b])
        o_tt(ph, "dve", e2[:], e2[:], esel[:], ALU.add, [e2b, eselb], [e2b])
        m2, m2b = S_("m2", [128, NT])
        o_red(ph, m2[:], e2[:], ALU.max, [e2b], [m2b])
        oh2, oh2b = S_("oh2", [128, NT, 8])
        o_tt(ph, "dve", oh2[:], e2[:], m2[:].unsqueeze(2).to_broadcast([128, NT, 8]), ALU.is_equal, [e2b, m2b], [oh2b])
        ee, eeb = S_("ee", [128, NT])
        o_tt(ph, "dve", ee[:], m2[:], m1[:], ALU.subtract, [m1b, m2b], [eeb])
        o_act(ph, ee[:], ee[:], AF.Exp, [eeb], [eeb])
        p1, p1b = S_("p1", [128, NT])
        o_ts(ph, "dve", p1[:], ee[:], 1.0, None, ALU.add, None, [eeb], [p1b])
        o_rcp(ph, p1[:], p1[:], [p1b], [p1b])
        gwt = C.GW
        gwtb = ph.buf("GW")
        o_tt(ph, "dve", gwt[:, :, 1], ee[:], p1[:], ALU.mult, [eeb, p1b], [gwtb])
        o_tt(ph, "dve", gwt[:, :, 1], gwt[:, :, 1], gw[:], ALU.mult, [gwtb, gwb], [gwtb])
        o_tt(ph, "dve", gwt[:, :, 0], p1[:], gw[:], ALU.mult, [p1b, gwb], [gwtb])
        M1, M1b = S_("M1", [128, NT, 4, 8])
        M2, M2b = S_("M2", [128, NT, 4, 8])
        for g in range(4):
            o_tt(ph, "dve", M1[:, :, g, :], oh1[:], goh[:, :, g].unsqueeze(2).to_broadcast([128, NT, 8]), ALU.mult, [oh1b, gohb], [M1b])
            o_tt(ph, "dve", M2[:, :, g, :], oh2[:], goh[:, :, g].unsqueeze(2).to_broadcast([128, NT, 8]), ALU.mult, [oh2b, gohb], [M2b])
        Mb16, Mb16b = S_("Mb16", [128, NT * 32], BF16)
        o_tt(ph, "dve", Mb16[:], M1[:].rearrange("p n g e -> p (n g e)"), M2[:].rearrange("p n g e -> p (n g e)"), ALU.add, [M1b, M2b], [Mb16b])
        POS, POSb = S_("POS", [128, NT * 32])
        CS, CSb = S_("CS", [128, NT * 32])
        for ch in range(4):
            pp, ppb = ppr.next()
            o_mm(ph, pp[:], C.Umat[:], Mb16[:, ch * 512:(ch + 1) * 512], True, True, [Mb16b], [ppb])
            o_cp(ph, "dve", POS[:, ch * 512:(ch + 1) * 512], pp[:], [ppb], [POSb])
            pcs, pcsb = pcr.next()
            o_mm(ph, pcs[:], C.ones[:], Mb16[:, ch * 512:(ch + 1) * 512], True, True, [Mb16b], [pcsb])
            o_cp(ph, "act", CS[:, ch * 512:(ch + 1) * 512], pcs[:], [pcsb], [CSb])
        SA, SAb = S_("SA", [128, NT * 32])
        SB, SBb = S_("SB", [128, NT * 32])
        o_cp(ph, "dve", SA[:], CS[:], [CSb], [SAb])
        cur, curb, oth, othb = SA, SAb, SB, SBb
        s_ = 1
        while s_ < NT:
            o_cp(ph, "dve", oth[:, 0:s_ * 32], cur[:, 0:s_ * 32], [curb], [othb])
            o_tt(ph, "dve", oth[:, s_ * 32:], cur[:, s_ * 32:], cur[:, 0:(NT - s_) * 32], ALU.add, [curb], [othb])
            cur, curb, oth, othb = oth, othb, cur, curb
            s_ *= 2
        o_tt(ph, "dve", POS[:], POS[:], cur[:], ALU.add, [POSb, curb], [POSb])
        o_tt(ph, "dve", POS[:], POS[:], CS[:], ALU.subtract, [POSb, CSb], [POSb])
        o_ts(ph, "dve", POS[:], POS[:], float(CAP - 1), None, ALU.min, None, [POSb], [POSb])
        o_tt(ph, "dve", POS[:].rearrange("p (n e) -> p n e", e=32), POS[:].rearrange("p (n e) -> p n e", e=32),
             C.ecap[:].unsqueeze(1).to_broadcast([128, NT, 32]), ALU.add, [POSb], [POSb])
        DF, DFb = S_("DF", [128, NT, 2])
        for kx, (Mx, Mxb) in enumerate(((M1, M1b), (M2, M2b))):
            o_tt(ph, "dve", Mx[:].rearrange("p n g e -> p (n g e)"), Mx[:].rearrange("p n g e -> p (n g e)"), POS[:], ALU.mult, [Mxb, POSb], [Mxb])
            o_red(ph, DF[:, :, kx], Mx[:].rearrange("p n g e -> p n (g e)"), ALU.add, [Mxb], [DFb])
        dib = ph.buf("DI")
        o_cp(ph, "dve", C.DI[:], DF[:], [DFb], [dib])
        xr = Ring(ph, [A(f"x{i}", [128, D], BF16) for i in range(3)], "x")
        for i in range(NT):
            xt, xb = xr.next()
            ph.dma("sp", xt[:], C.H1b[i * 128:(i + 1) * 128, :], writes=[xb])
            for kx in range(2):
                ph.dma_fn("pool", (lambda e, xt=xt, i=i, kx=kx: e.indirect_dma_start(
                    out=C.XS, out_offset=bass.IndirectOffsetOnAxis(ap=C.DI[:, i, kx:kx + 1], axis=0), in_=xt[:], in_offset=None)),
                    reads=[xb, dib])
        ph.emit()


def phase_experts(C, l):
    from contextlib import ExitStack
    nc = C.nc
    NST = CAP // 128
    HN = CAP // 2
    with ExitStack() as st:
        A, P = mk_alloc(nc, st, f"p4_{l}_")
        ph = Phase(C.S, f"experts{l}")
        wgr = Ring(ph, [A(f"wg{i}", [128, 8, 512], BF16) for i in range(2)], "wg")
        wur = Ring(ph, [A(f"wu{i}", [128, 8, 512], BF16) for i in range(2)], "wu")
        wdr = Ring(ph, [A(f"wd{i}", [128, 4, 1024], BF16) for i in range(2)], "wd")
        wdsr = Ring(ph, [A(f"wds{i}", [128, 4, 1024], F32) for i in range(2)], "wds")
        xsr = Ring(ph, [A(f"xs{i}", [128, NST, D], BF16) for i in range(2)], "xs")
        xTr = Ring(ph, [A(f"xT{i}", [128, 8, CAP], BF16) for i in range(2)], "xT")
        sgr = Ring(ph, [A(f"sg{i}", [128, HN], F32) for i in range(2)], "sg")
        hdr = Ring(ph, [A(f"hd{i}", [128, 4, HN], BF16) for i in range(2)], "hd")
        yor = Ring(ph, [A(f"yo{i}", [128, D], BF16) for i in range(3)], "yo")
        tpr = Ring(ph, [P(f"tp{i}", [128, 1024], BF16) for i in range(2)], "tp")
        pgr = Ring(ph, [P(f"pg{i}", [128, 512], F32) for i in range(2)], "pg")
        pur = Ring(ph, [P(f"pu{i}", [128, 512], F32) for i in range(2)], "pu")
        pyr = Ring(ph, [P("py", [128, 1024], F32)], "py")

        def loads(e):
            wg, wgb = wgr.next()
            wu, wub = wur.next()
            wd, wdb = wdr.next()
            xs, xsb = xsr.next()
            ph.dma("pool", wg[:], C.w_eg[l, e].rearrange("(k p) f -> p k f", p=128), writes=[wgb])
            ph.dma("pool", wu[:], C.w_eu[l, e].rearrange("(k p) f -> p k f", p=128), writes=[wub])
            wds, wdsb = wdsr.next()
            ph.dma("sp", wds[:], C.w_ed[l, e].rearrange("(k p) f -> p k f", p=128), writes=[wdsb])
            o_cp(ph, "pool", wd[:], wds[:], [wdsb], [wdb])
            ph.dma("sp", xs[:], C.XS[e * CAP:(e + 1) * CAP, :].rearrange("(s p) d -> p s d", p=128), writes=[xsb])
            return wg, wgb, wu, wub, wd, wdb, xs, xsb

        nxt = loads(0)
        for e in range(32):
            wg, wgb, wu, wub, wd, wdb, xs, xsb = nxt
            if e + 1 < 32:
                nxt = loads(e + 1)
            xT, xTb = xTr.next()
            for s_ in range(NST):
                tp, tpb = tpr.next()
                for k in range(8):
                    o_tp(ph, tp[:, k * 128:(k + 1) * 128], xs[:, s_, k * 128:(k + 1) * 128], C.identb[:], [xsb], [tpb])
                o_cp(ph, "dve" if s_ % 2 == 0 else "act", xT[:, :, s_ * 128:(s_ + 1) * 128],
                     tp[:].rearrange("p (k t) -> p k t", k=8), [tpb], [xTb])
            for hh in range(2):
                hd, hdb = hdr.next()
                for fc in range(4):
                    pg, pgb = pgr.next()
                    pu, pub = pur.next()
                    for k in range(8):
                        o_mm(ph, pg[:, 0:HN], wg[:, k, fc * 128:(fc + 1) * 128], xT[:, k, hh * HN:(hh + 1) * HN], k == 0, k == 7, [wgb, xTb], [pgb])
                    for k in range(8):
                        o_mm(ph, pu[:, 0:HN], wu[:, k, fc * 128:(fc + 1) * 128], xT[:, k, hh * HN:(hh + 1) * HN], k == 0, k == 7, [wub, xTb], [pub])
                    sg, sgb = sgr.next()
                    o_act(ph, sg[:], pg[:, 0:HN], AF.Silu, [pgb], [sgb])
                    o_tt(ph, "dve", hd[:, fc, :], sg[:], pu[:, 0:HN], ALU.mult, [sgb, pub], [hdb])
                for s_ in range(NST // 2):
                    py, pyb = pyr.next()
                    for half in range(2):
                        for k in range(4):
                            o_mm(ph, py[:, half * 512:(half + 1) * 512], hd[:, k, s_ * 128:(s_ + 1) * 128],
                                 wd[:, k, half * 512:(half + 1) * 512], k == 0, k == 3, [hdb, wdb], [pyb])
                    yo, yob = yor.next()
                    o_cp(ph, "dve" if s_ % 2 == 0 else "act", yo[:], py[:], [pyb], [yob])
                    r0 = e * CAP + hh * HN + s_ * 128
                    ph.dma("sp", C.R[r0:r0 + 128, :], yo[:], reads=[yob])
        ph.emit()


def phase_combine(C, l, dst):
    from contextlib import ExitStack
    nc = C.nc
    with ExitStack() as st:
        A, P = mk_alloc(nc, st, f"p5_{l}_")
        ph = Phase(C.S, f"combine{l}")
        ln_setup(ph, C, A)
        g, b, cb = load_gb(ph, A, C.ln2g[l], C.ln2b[l], "ln2")
        r1r = Ring(ph, [A(f"r1_{i}", [128, D], BF16) for i in range(2)], "r1")
        r2r = Ring(ph, [A(f"r2_{i}", [128, D], BF16) for i in range(2)], "r2")
        hr = Ring(ph, [A(f"h{i}", [128, D], F32) for i in range(2)], "h")
        yr = Ring(ph, [A(f"y{i}", [128, D], F32) for i in range(2)], "y")
        zr = Ring(ph, [A(f"z{i}", [128, D], F32) for i in range(5)], "z")
        orr = Ring(ph, [A(f"o{i}", [128, D], F32) for i in range(3)], "o")

        def loads(i):
            r1, r1b = r1r.next()
            r2, r2b = r2r.next()
            h, hb = hr.next()
            for kx, (rt, rb) in enumerate(((r1, r1b), (r2, r2b))):
                ph.dma_fn("pool", (lambda e, rt=rt, i=i, kx=kx: e.indirect_dma_start(
                    out=rt[:], out_offset=None, in_=C.R, in_offset=bass.IndirectOffsetOnAxis(ap=C.DI[:, i, kx:kx + 1], axis=0))),
                    writes=[rb])
            ph.dma("sp", h[:], C.H1[i * 128:(i + 1) * 128, :], writes=[hb])
            return r1, r1b, r2, r2b, h, hb

        nxt = loads(0)
        grp = []
        for i in range(NT):
            r1, r1b, r2, r2b, h, hb = nxt
            if i + 1 < NT:
                nxt = loads(i + 1)
            y, yb = yr.next()
            o_act(ph, y[:], r1[:], AF.Copy, [r1b], [yb], scale=C.GW[:, i, 0:1])
            o_stt(ph, y[:], r2[:], C.GW[:, i, 1:2], y[:], ALU.mult, ALU.add, [r2b, yb], [yb])
            z, zb = zr.next()
            o_stt(ph, z[:], h[:], ALPHA, y[:], ALU.mult, ALU.add, [hb, yb], [zb])
            mv, mvb = ln_s1(ph, C, z[:], zb)
            grp.append((i, z, zb, mv, mvb))
            if len(grp) == 4 or i == NT - 1:
                for (ii, z_, zb_, mv_, mvb_) in grp:
                    ln_s2(ph, C, mv_, mvb_)
                for (ii, z_, zb_, mv_, mvb_) in grp:
                    o, ob = orr.next()
                    ln_s3(ph, C, z_[:], zb_, mv_, mvb_, o[:], ob, g[:], b[:], cb, geng="dve", beng="dve")
                    ph.dma("sp", dst[ii * 128:(ii + 1) * 128, :], o[:], reads=[ob])
                grp = []
        ph.emit()


def phase_pre(C):
    from contextlib import ExitStack
    nc = C.nc
    with ExitStack() as st:
        A, P = mk_alloc(nc, st, "pre_")
        ph = Phase(C.S, "pre")
        cb = ph.buf("const")
        o_memset(ph, "pool", C.identb[:], 0.0, [cb])
        ph.op("pool", lambda e: e.affine_select(out=C.identb[:], in_=C.identb[:], pattern=[[-1, 128]], compare_op=ALU.not_equal,
                                                 fill=1.0, base=0, channel_multiplier=1), [cb], [cb])
        o_memset(ph, "pool", C.identf[:], 0.0, [cb])
        ph.op("pool", lambda e: e.affine_select(out=C.identf[:], in_=C.identf[:], pattern=[[-1, 128]], compare_op=ALU.not_equal,
                                                 fill=1.0, base=0, channel_multiplier=1), [cb], [cb])
        o_memset(ph, "pool", C.Umat[:], 1.0, [cb])
        ph.op("pool", lambda e: e.affine_select(out=C.Umat[:], in_=C.Umat[:], pattern=[[1, 128]], compare_op=ALU.is_gt,
                                                 fill=0.0, base=0, channel_multiplier=-1), [cb], [cb])
        o_memset(ph, "pool", C.ones[:], 1.0, [cb])
        o_memset(ph, "pool", C.eps[:], LN_EPS, [cb])
        o_memset(ph, "pool", C.fence[:], 0.0, [cb])
        eci = A("eci", [128, 32], I32)
        ph.op("pool", lambda e: e.iota(eci[:], pattern=[[CAP, 32]], base=0, channel_multiplier=0), [cb], [cb])
        o_cp(ph, "pool", C.ecap[:], eci[:], [cb], [cb])
        tmpr = Ring(ph, [A(f"bt{i}", [128, 3072], F32) for i in range(2)], "bt")
        tmp, tb = tmpr.next()
        ph.dma("sp", tmp[:], C.biasA, writes=[tb])
        o_act(ph, C.EA[:], tmp[:], AF.Exp, [tb], [cb])
        ph.dma("sp", C.BB[:].rearrange("p (g c) -> p g c", g=3), C.biasB.rearrange("g p c -> p g c"), writes=[cb])
        zt = A("zt", [128, 8 * 520], BF16)
        zb = ph.buf("zt")
        o_memset(ph, "pool", zt[:], 0.0, [zb])
        na = PADR // 128
        for base in (0, PADR + T):
            for a in range(na):
                r0 = base + a * 128
                ph.dma("sp", C.QK[r0:r0 + 128, :], zt[:, 0:QK_W], reads=[zb])
            for arr, wd_ in [(C.VA, 130), (C.VB[0], 260), (C.VB[1], 260), (C.VB[2], 260), (C.VC, 520)]:
                ph.dma("sp", arr[base:base + PADR, :].rearrange("(p a) c -> p (a c)", a=na), zt[:, 0:na * wd_], reads=[zb])
        ph.emit()


def run_b(C, l):
    for gi in range(3):
        phase_attn_b(C, l, gi)
    phase_attn_bc(C, l)


INPUT_SPECS = [
    ("x", [T, D]), ("ln0_g", [1, D]), ("ln0_b", [1, D]),
    ("biasA", [128, 3072]), ("biasB", [3, 128, 1024]), ("biasC", [DEPTH, 5, 128, 5120]),
    ("w_in", [DEPTH, D, 7680]), ("bgT", [DEPTH, 128, 24]), ("sink", [DEPTH, 1, 8]),
    ("w_bra", [DEPTH, 512, D]), ("w_brb", [DEPTH, 256, D]), ("w_brc", [DEPTH, 512, D]), ("w_out", [DEPTH, D, D]),
    ("ln1g", [DEPTH, 1, D]), ("ln1b", [DEPTH, 1, D]), ("wr", [DEPTH, D, 36]), ("br", [DEPTH, 1, 36]),
    ("w_eg", [DEPTH, 32, D, 512]), ("w_eu", [DEPTH, 32, D, 512]), ("w_ed", [DEPTH, 32, 512, D]),
    ("ln2g", [DEPTH, 1, D]), ("ln2b", [DEPTH, 1, D]),
]


def build(debug=(), upto=None, only_inputs=None, only_steps=None):
    from contextlib import ExitStack
    nc = bass.Bass("TRN2", target_bir_lowering=False)
    C = Ctx()
    C.nc = nc
    for name, shape in INPUT_SPECS:
        if only_inputs is not None and name not in only_inputs:
            continue
        ap = nc.dram_tensor(name, list(shape), F32, kind="ExternalInput").ap()
        setattr(C, {"ln0_g": "ln0g", "ln0_b": "ln0b"}.get(name, name), ap)

    def scr(name, shape, dt):
        kind = "ExternalOutput" if name in debug else "Internal"
        return nc.dram_tensor(name, list(shape), dt, kind=kind).ap()

    C.out = nc.dram_tensor("out", [T, D], F32, kind="ExternalOutput").ap()
    C.H = scr("H", [T, D], F32)
    C.QK = scr("QK", [ROWS, QK_W], BF16)
    C.VA = scr("VA", [ROWS, 130], BF16)
    C.VB = [scr(f"VB{g}", [ROWS, 260], BF16) for g in range(3)]
    C.VC = scr("VC", [ROWS, 520], BF16)
    C.GT = scr("GT", [3072, T], BF16)
    C.NB = [scr(f"NB{g}", [T, 260], F32) for g in range(3)]
    C.OT = scr("OT", [1280, T], BF16)
    C.OC = scr("OC", [T, 512], BF16)
    C.H1 = scr("H1", [T, D], F32)
    C.H1b = scr("H1b", [T, D], BF16)
    C.XS = scr("XS", [NSLOT, D], BF16)
    C.R = scr("R", [NSLOT, D], BF16)
    with ExitStack() as st:
        C.S = Sched(nc, st)
        A, _ = mk_alloc(nc, st, "g_")
        C.identb = A("identb", [128, 128], BF16)
        C.identf = A("identf", [128, 128], F32)
        C.Umat = A("Umat", [128, 128], BF16)
        C.ones = A("ones", [128, 128], BF16)
        C.eps = A("eps", [128, 1], F32)
        C.ecap = A("ecap", [128, 32], F32)
        C.EA = A("EA", [128, 3072], BF16)
        C.BB = A("BB", [128, 3072], F32)
        C.fence = A("fence", [128, 2], F32)
        C.DI = A("DI", [128, NT, 2], I32)
        C.GW = A("GW", [128, NT, 2], F32)
        steps = [("pre", lambda: phase_pre(C)), ("ln0", lambda: phase_ln0(C))]
        for l in range(DEPTH):
            dst = C.H if l + 1 < DEPTH else C.out
            steps += [
                (f"proj{l}", lambda l=l: phase_proj(C, l)),
                (f"attnA{l}", lambda l=l: phase_attn_a(C, l)),
                (f"attnB{l}", lambda l=l: run_b(C, l)),
                (f"attnC{l}", lambda l=l: phase_attn_c(C, l)),
                (f"merge{l}", lambda l=l: phase_merge(C, l)),
                (f"route{l}", lambda l=l: phase_route(C, l)),
                (f"experts{l}", lambda l=l: phase_experts(C, l)),
                (f"combine{l}", lambda l=l, dst=dst: phase_combine(C, l, dst)),
            ]
        for name, fn in steps:
            if only_steps is not None and name not in only_steps:
                continue
            fn()
            if upto is not None and name == upto:
                break
    return nc


def host_inputs(inp):
    f = lambda a: np.ascontiguousarray(np.asarray(a), dtype=np.float32)
    rel_bias = f(inp["rel_bias"])
    rpb_c = f(inp["rpb_c"])
    ba, bb, bc = host_bias_tables(rel_bias, rpb_c)
    b_gate = f(inp["b_gate"])
    w_rg, w_re = f(inp["w_rg"]), f(inp["w_re"])
    wr = np.concatenate([w_rg, w_re.transpose(0, 2, 1, 3).reshape(DEPTH, D, 32)], axis=2)
    br = np.concatenate([f(inp["b_rg"]), f(inp["b_re"]).reshape(DEPTH, 32)], axis=1).reshape(DEPTH, 1, 36)
    shared = {
        "ln0_g": f(inp["ln0_g"]).reshape(1, D), "ln0_b": f(inp["ln0_b"]).reshape(1, D),
        "biasA": np.ascontiguousarray(ba.reshape(128, 3072)),
        "biasB": np.ascontiguousarray(bb.reshape(3, 128, 1024)),
        "biasC": np.ascontiguousarray(bc.reshape(DEPTH, 5, 128, 5120)),
        "w_in": f(inp["w_in"]),
        "bgT": np.ascontiguousarray(b_gate.reshape(DEPTH, 24, 128).transpose(0, 2, 1)),
        "sink": f(inp["sink_a"]).reshape(DEPTH, 1, 8),
        "w_bra": f(inp["w_br_a"]), "w_brb": f(inp["w_br_b"]), "w_brc": f(inp["w_br_c"]), "w_out": f(inp["w_out"]),
        "ln1g": f(inp["ln1_g"]).reshape(DEPTH, 1, D), "ln1b": f(inp["ln1_b"]).reshape(DEPTH, 1, D),
        "wr": np.ascontiguousarray(wr), "br": np.ascontiguousarray(br),
        "w_eg": f(inp["w_eg"]), "w_eu": f(inp["w_eu"]), "w_ed": f(inp["w_ed"]),
        "ln2g": f(inp["ln2_g"]).reshape(DEPTH, 1, D), "ln2b": f(inp["ln2_b"]).reshape(DEPTH, 1, D),
    }
    return shared


_NC_CACHE = {}


def kernel(**inputs):
    x = np.ascontiguousarray(np.asarray(inputs["x"]), dtype=np.float32)
    shared = host_inputs(inputs)
    if "nc" not in _NC_CACHE:
        _NC_CACHE["nc"] = build()
    nc = _NC_CACHE["nc"]
    in_maps = []
    for c in range(8):
        m = dict(shared)
        m["x"] = x[c]
        in_maps.append(m)
    res = run_bass_kernel_spmd(nc, in_maps, core_ids=list(range(8)))
    return np.stack([np.asarray(r["out"], dtype=np.float32) for r in res.results], axis=0)
```
